# Optimizing a Trainium2 kernel written in Bass

```python
import jax, jax.numpy as jnp
from jax import lax
import numpy as np

D_MODEL = 1024
BATCH = 4
SEQ = 4096
DEPTH = 1

D_MIX = D_MODEL
MLSTM_WIDTH = D_MIX // 2
MLSTM_HEADS = 4
MLSTM_HEAD_DIM = MLSTM_WIDTH // MLSTM_HEADS
MLSTM_CHUNK = 64
CONV_WIDTH = 4
POOL_WIDTH = D_MIX - MLSTM_WIDTH
POOL_WINDOWS = (2, 4, 8, 16)
POOL_GROUPS = len(POOL_WINDOWS)
POOL_GROUP_DIM = POOL_WIDTH // POOL_GROUPS
N_IN = 4 * MLSTM_WIDTH + 2 * MLSTM_HEADS + POOL_WIDTH
N_EXPERTS = 32
TOP_K = 4
D_FF = D_MODEL
SWIGLU_LIMIT = 7.0
SWIGLU_ALPHA = 1.702
EXPERT_BLOCK = 128
EPS = 1e-5

kernel_name = "hybrid_mlstm_pool_moe_block"


def rmsnorm(x, g):
    xf = x.astype(jnp.float32)
    y = xf * lax.rsqrt(jnp.mean(xf * xf, axis=-1, keepdims=True) + EPS)
    return (y * g.astype(jnp.float32)).astype(x.dtype)


def causal_depthwise_conv(u, w):
    k = w.shape[0]
    return lax.conv_general_dilated(
        u, w[:, None, :].astype(u.dtype), window_strides=(1,), padding=[(k - 1, 0)],
        dimension_numbers=("NWC", "WIO", "NWC"), feature_group_count=u.shape[-1])


def mlstm_chunkwise(q, k, v, ig, lf):
    B, H, S, Dh = q.shape
    L = MLSTM_CHUNK
    NC = S // L
    q = q.reshape(B, H, NC, L, Dh)
    k = k.reshape(B, H, NC, L, Dh)
    v = v.reshape(B, H, NC, L, Dh)
    ig = ig.reshape(B, H, NC, L)
    lf = lf.reshape(B, H, NC, L)
    b = jnp.cumsum(lf, axis=-1)
    b_tot = b[..., -1]

    g = b_tot[..., None] - b + ig
    m_loc = jnp.max(g, axis=-1)
    w_loc = jnp.exp(g - m_loc[..., None])
    C_loc = jnp.einsum("bhclk,bhclv->bhckv", w_loc[..., None] * k, v)
    n_loc = jnp.einsum("bhcl,bhclk->bhck", w_loc, k)

    def step(carry, inp):
        C, n, m = carry
        C_l, n_l, m_l, bt = inp
        m_new = jnp.maximum(bt + m, m_l)
        s_old = jnp.exp(bt + m - m_new)
        s_loc = jnp.exp(m_l - m_new)
        C_new = s_old[..., None, None] * C + s_loc[..., None, None] * C_l
        n_new = s_old[..., None] * n + s_loc[..., None] * n_l
        return (C_new, n_new, m_new), (C, n, m)

    init = (jnp.zeros((B, H, Dh, Dh), jnp.float32),
            jnp.zeros((B, H, Dh), jnp.float32),
            jnp.zeros((B, H), jnp.float32))
    xs = (jnp.moveaxis(C_loc, 2, 0), jnp.moveaxis(n_loc, 2, 0),
          jnp.moveaxis(m_loc, 2, 0), jnp.moveaxis(b_tot, 2, 0))
    _, (C_in, n_in, m_in) = lax.scan(step, init, xs)
    C_in = jnp.moveaxis(C_in, 0, 2)
    n_in = jnp.moveaxis(n_in, 0, 2)
    m_in = jnp.moveaxis(m_in, 0, 2)

    causal = jnp.tril(jnp.ones((L, L), dtype=bool))
    log_d = b[..., :, None] - b[..., None, :] + ig[..., None, :]
    log_d = jnp.where(causal, log_d, -jnp.inf)
    a = b + m_in[..., None]
    m_out = jnp.maximum(a, jnp.max(log_d, axis=-1))
    s = jnp.einsum("bhcld,bhcrd->bhclr", q, k) * jnp.exp(log_d - m_out[..., None])
    inter = jnp.exp(a - m_out)
    num = (jnp.einsum("bhclr,bhcrv->bhclv", s, v)
           + inter[..., None] * jnp.einsum("bhcld,bhcdv->bhclv", q, C_in))
    den = jnp.sum(s, axis=-1) + inter * jnp.einsum("bhcld,bhcd->bhcl", q, n_in)
    h = num / jnp.maximum(jnp.abs(den), jnp.exp(-m_out))[..., None]
    return h.reshape(B, H, S, Dh)


def multiscale_pool(u, pool_w, pool_scale):
    B, S, _ = u.shape
    uf = u.astype(jnp.float32).reshape(B, S, POOL_GROUPS, POOL_GROUP_DIM)
    cs = jnp.concatenate([jnp.zeros((B, 1, POOL_GROUPS, POOL_GROUP_DIM), jnp.float32),
                          jnp.cumsum(uf, axis=1)], axis=1)
    t = jnp.arange(S)
    pooled = []
    for gi, w in enumerate(POOL_WINDOWS):
        c = cs[:, :, gi]
        c_lo = jnp.concatenate([jnp.zeros((B, w - 1, POOL_GROUP_DIM), jnp.float32),
                                c[:, :S - w + 1]], axis=1)
        cnt = jnp.minimum(t + 1, w).astype(jnp.float32)
        pooled.append((c[:, 1:] - c_lo) / cnt[None, :, None])
    pooled = jnp.stack(pooled, axis=2) - uf
    mixed = jnp.einsum("bsgc,gcd->bsgd", pooled, pool_w.astype(jnp.float32))
    mixed = mixed.reshape(B, S, POOL_WIDTH) * pool_scale.astype(jnp.float32)
    return mixed.astype(u.dtype)


def hybrid_mixer(h, w_in, ig_b, fg_b, conv_w, head_norm_g, pool_w, pool_scale, w_out):
    B, S, _ = h.shape
    W, H, Dh = MLSTM_WIDTH, MLSTM_HEADS, MLSTM_HEAD_DIM
    p = h @ w_in
    qk = p[..., :2 * W]
    v = p[..., 2 * W:3 * W]
    o = p[..., 3 * W:4 * W]
    gates = p[..., 4 * W:4 * W + 2 * H].astype(jnp.float32)
    u = p[..., 4 * W + 2 * H:]

    qk = jax.nn.silu(causal_depthwise_conv(qk, conv_w))

    def to_heads(t):
        return t.reshape(B, S, H, Dh).transpose(0, 2, 1, 3).astype(jnp.float32)

    q = to_heads(qk[..., :W])
    k = to_heads(qk[..., W:]) * (Dh ** -0.5)
    vh = to_heads(v)
    ig = (gates[..., :H] + ig_b.astype(jnp.float32)).transpose(0, 2, 1)
    lf = jax.nn.log_sigmoid(gates[..., H:] + fg_b.astype(jnp.float32)).transpose(0, 2, 1)
    hm = mlstm_chunkwise(q, k, vh, ig, lf)
    mu = jnp.mean(hm, axis=-1, keepdims=True)
    var = jnp.mean(jnp.square(hm - mu), axis=-1, keepdims=True)
    hm = (hm - mu) * lax.rsqrt(var + EPS)
    hm = hm.transpose(0, 2, 1, 3).reshape(B, S, W) * head_norm_g.astype(jnp.float32)
    y_m = (jax.nn.sigmoid(o.astype(jnp.float32)) * hm).astype(h.dtype)

    y_p = multiscale_pool(u, pool_w, pool_scale)

    return jnp.concatenate([y_m, y_p], axis=-1) @ w_out


def moe_ffn(h, w_router, b_router, w_gate, b_gate, w_up, b_up, w_down, b_down):
    B, S, D = h.shape
    T = B * S
    xt = h.reshape(T, D)
    logits = (xt @ w_router + b_router).astype(jnp.float32)
    top_vals, top_idx = lax.top_k(logits, TOP_K)
    top_gates = jax.nn.softmax(top_vals, axis=-1)

    A = T * TOP_K
    flat_expert = top_idx.reshape(A)
    flat_token = jnp.repeat(jnp.arange(T, dtype=jnp.int32), TOP_K)
    flat_gate = top_gates.reshape(A)
    order = jnp.argsort(flat_expert)
    s_expert = flat_expert[order]
    s_token = flat_token[order]
    s_gate = flat_gate[order]

    counts = jax.ops.segment_sum(jnp.ones((A,), jnp.int32), flat_expert, num_segments=N_EXPERTS)
    group_start = jnp.cumsum(counts) - counts
    padded = ((counts + EXPERT_BLOCK - 1) // EXPERT_BLOCK) * EXPERT_BLOCK
    padded_end = jnp.cumsum(padded)
    padded_start = padded_end - padded
    rank = jnp.arange(A, dtype=jnp.int32) - group_start[s_expert]
    dest = padded_start[s_expert] + rank

    n_blocks = -(-A // EXPERT_BLOCK) + N_EXPERTS
    P = n_blocks * EXPERT_BLOCK
    tok_buf = jnp.full((P,), T, jnp.int32).at[dest].set(s_token)
    gate_buf = jnp.zeros((P,), jnp.float32).at[dest].set(s_gate)
    block_start = jnp.arange(n_blocks, dtype=jnp.int32) * EXPERT_BLOCK
    block_expert = jnp.minimum(jnp.searchsorted(padded_end, block_start, side="right"),
                               N_EXPERTS - 1).astype(jnp.int32)

    x_pad = jnp.concatenate([xt, jnp.zeros((1, D), xt.dtype)], axis=0)
    x_buf = x_pad[tok_buf].reshape(n_blocks, EXPERT_BLOCK, D)

    def expert_block(args):
        xb, e = args
        gate = xb @ w_gate[e] + b_gate[e]
        up = xb @ w_up[e] + b_up[e]
        gate = jnp.minimum(gate, SWIGLU_LIMIT)
        up = jnp.clip(up, -SWIGLU_LIMIT, SWIGLU_LIMIT)
        glu = gate * jax.nn.sigmoid(SWIGLU_ALPHA * gate)
        return (glu * (up + 1.0)) @ w_down[e] + b_down[e]

    y_buf = lax.map(expert_block, (x_buf, block_expert)).reshape(P, D)
    y_buf = y_buf * gate_buf[:, None].astype(y_buf.dtype)
    out = jax.ops.segment_sum(y_buf, tok_buf, num_segments=T + 1)[:T]
    return out.reshape(B, S, D).astype(h.dtype)


def setup_inputs(seed: int = 0) -> dict:
    key = jax.random.key(seed)
    ks = jax.random.split(key, 20)
    f32 = jnp.float32
    L_, D, E, F, H = DEPTH, D_MODEL, N_EXPERTS, D_FF, MLSTM_HEADS
    nrm = lambda k, shape, s: jax.random.normal(k, shape, f32) * s
    x = jax.random.normal(ks[0], (BATCH, SEQ, D), f32)
    norm1_g = 1.0 + nrm(ks[1], (L_, D), 0.02)
    w_in = nrm(ks[2], (L_, D, N_IN), D ** -0.5)
    ig_b = nrm(ks[3], (L_, H), 0.1)
    fg_b = jnp.broadcast_to(jnp.linspace(3.0, 6.0, H, dtype=f32), (L_, H)) + nrm(ks[4], (L_, H), 0.1)
    conv_w = nrm(ks[5], (L_, CONV_WIDTH, 2 * MLSTM_WIDTH), CONV_WIDTH ** -0.5)
    head_norm_g = 1.0 + nrm(ks[6], (L_, MLSTM_WIDTH), 0.02)
    pool_w = nrm(ks[7], (L_, POOL_GROUPS, POOL_GROUP_DIM, POOL_GROUP_DIM), POOL_GROUP_DIM ** -0.5)
    pool_scale = 1.0 + nrm(ks[8], (L_, POOL_WIDTH), 0.02)
    w_out = nrm(ks[9], (L_, D_MIX, D), D_MIX ** -0.5)
    norm2_g = 1.0 + nrm(ks[10], (L_, D), 0.02)
    w_router = nrm(ks[11], (L_, D, E), D ** -0.5)
    b_router = nrm(ks[12], (L_, E), 0.01)
    w_gate = nrm(ks[13], (L_, E, D, F), D ** -0.5)
    b_gate = nrm(ks[14], (L_, E, F), 0.01)
    w_up = nrm(ks[15], (L_, E, D, F), D ** -0.5)
    b_up = nrm(ks[16], (L_, E, F), 0.01)
    w_down = nrm(ks[17], (L_, E, F, D), F ** -0.5)
    b_down = nrm(ks[18], (L_, E, D), 0.01)
    normf_g = 1.0 + nrm(ks[19], (D,), 0.02)
    return {"x": x, "norm1_g": norm1_g, "w_in": w_in, "ig_b": ig_b, "fg_b": fg_b,
            "conv_w": conv_w, "head_norm_g": head_norm_g, "pool_w": pool_w,
            "pool_scale": pool_scale, "w_out": w_out, "norm2_g": norm2_g,
            "w_router": w_router, "b_router": b_router, "w_gate": w_gate,
            "b_gate": b_gate, "w_up": w_up, "b_up": b_up, "w_down": w_down,
            "b_down": b_down, "normf_g": normf_g}


def reference(x, norm1_g, w_in, ig_b, fg_b, conv_w, head_norm_g, pool_w, pool_scale,
              w_out, norm2_g, w_router, b_router, w_gate, b_gate, w_up, b_up,
              w_down, b_down, normf_g):
    for l in range(DEPTH):
        h = rmsnorm(x, norm1_g[l])
        x = x + hybrid_mixer(h, w_in[l], ig_b[l], fg_b[l], conv_w[l], head_norm_g[l],
                             pool_w[l], pool_scale[l], w_out[l])
        h = rmsnorm(x, norm2_g[l])
        x = x + moe_ffn(h, w_router[l], b_router[l], w_gate[l], b_gate[l], w_up[l],
                        b_up[l], w_down[l], b_down[l])
    return rmsnorm(x, normf_g)
```

```python
import contextlib
import math
import os
import numpy as np
import concourse.bass as bass
import concourse.mybir as mybir
from concourse.alu_op_type import AluOpType as ALU
from concourse.bass_utils import run_bass_kernel_spmd

F32 = mybir.dt.float32
BF16 = mybir.dt.bfloat16
AF = mybir.ActivationFunctionType
AX = mybir.AxisListType

D = 1024
TOK = 2048
NT = TOK // 128
NE = 32
NIN = 2568
EPS = 1e-5
LNSC = math.log(128.0 ** -0.5)


class Op:
    __slots__ = ("eng", "fn", "deps", "odeps", "signal", "sem", "val", "is_dma", "idx", "cost", "lat", "fin", "nrem", "users")


class Sched:
    ENG = ("pe", "dve", "act", "pool", "sp")

    def __init__(self, nc, semstack, tag, ndma_sems=6):
        self.nc, self.semstack, self.tag = nc, semstack, tag
        self.ops = {e: [] for e in self.ENG}
        self.last_w, self.readers = {}, {}
        self.ndma = ndma_sems
        self.dma_count = {e: 0 for e in self.ENG}
        self.dma_prev = {}
        self.n = 0
        self.stopped = False
        self.ignore_cost = False

    COST = {"pe": 0.55, "dve": 0.35, "act": 0.35, "pool": 1.0, "sp": 0.1}

    def add(self, eng, fn, reads=(), writes=(), dma=False, cost=None):
        op = Op()
        if self.stopped:
            op.deps, op.signal, op.is_dma, op.eng = [], False, dma, eng
            return op
        if self.ignore_cost:
            cost = None
        op.cost = cost if cost is not None else (0.1 if dma else self.COST[eng])
        op.lat = (cost if cost is not None else 3.0) if dma else op.cost
        op.eng, op.fn, op.is_dma, op.signal = eng, fn, dma, False
        op.idx = self.n
        self.n += 1
        deps = []
        for r in reads:
            w = self.last_w.get(r)
            if w is not None:
                deps.append(w)
            if isinstance(r, tuple) and r[0] == "bank":
                deps.extend(o for o in self.readers.get(r, ()) if o.eng != eng)
        for w_ in writes:
            w = self.last_w.get(w_)
            if w is not None:
                deps.append(w)
            deps.extend(self.readers.get(w_, ()))
        for r in reads:
            self.readers.setdefault(r, []).append(op)
        for w_ in writes:
            self.last_w[w_] = op
            self.readers[w_] = []
        if dma:
            k = self.dma_count[eng]
            self.dma_count[eng] += 1
            slot = (eng, k % self.ndma)
            op.sem = slot
            prev = self.dma_prev.get(slot)
            if prev is not None:
                deps.append(prev)
            self.dma_prev[slot] = op
            op.signal = True
        ded, oded, seen = [], [], set()
        for d in deps:
            if d is op or id(d) in seen:
                continue
            seen.add(id(d))
            oded.append(d)
            if (not d.is_dma) and d.eng == eng and eng == "pe":
                continue
            ded.append(d)
        op.deps = ded
        op.odeps = oded
        self.ops[eng].append(op)
        return op

    def reorder(self):
        import heapq
        allops = [op for e in self.ENG for op in self.ops[e]]
        for op in allops:
            op.users, op.nrem, op.fin = [], len(op.odeps), None
        for op in allops:
            for d in op.odeps:
                d.users.append(op)
        SYNC = 1.2
        fut = {e: [] for e in self.ENG}
        now = {e: [] for e in self.ENG}
        t = {e: 0.0 for e in self.ENG}
        new = {e: [] for e in self.ENG}

        def push(op):
            r = 0.0
            for d in op.odeps:
                f = d.fin + (0.0 if (d.eng == op.eng and not d.is_dma) else SYNC)
                if f > r:
                    r = f
            heapq.heappush(fut[op.eng], (r, op.idx, op))
        for op in allops:
            if op.nrem == 0:
                push(op)
        left = len(allops)
        while left:
            best = None
            for e in self.ENG:
                while fut[e] and fut[e][0][0] <= t[e]:
                    r, i, op = heapq.heappop(fut[e])
                    heapq.heappush(now[e], (i, op))
                if now[e]:
                    st = t[e]
                elif fut[e]:
                    st = fut[e][0][0]
                else:
                    continue
                if best is None or st < best[0]:
                    best = (st, e)
            st, e = best
            if now[e]:
                i, op = heapq.heappop(now[e])
            else:
                r, i, op = heapq.heappop(fut[e])
            new[e].append(op)
            t[e] = st + op.cost
            op.fin = st + op.lat
            left -= 1
            for u in op.users:
                u.nrem -= 1
                if u.nrem == 0:
                    push(u)
        self.ops = new
        self.est = max(t.values())

    def emit(self, dummies, final_waits=()):
        nc = self.nc
        if os.environ.get("KNOSCHED") is None:
            self.reorder()
        for e, fn in dummies.items():
            o = self.add(e, fn, writes=[("bank", 0)] if e == "pe" else ())
            o.signal = True
        for e in self.ENG:
            for op in self.ops[e]:
                for d in op.deps:
                    d.signal = True
        esem = {e: self.semstack.enter_context(nc.semaphore(f"s{self.tag}_{e}")) for e in self.ENG}
        dsem = {}
        for e in self.ENG:
            for i in range(min(self.ndma, self.dma_count[e])):
                dsem[(e, i)] = self.semstack.enter_context(nc.semaphore(f"d{self.tag}_{e}{i}"))
        finals = {}
        for e in self.ENG:
            c, dc = 0, {}
            for op in self.ops[e]:
                if op.is_dma:
                    dc[op.sem] = dc.get(op.sem, 0) + 16
                    op.val = dc[op.sem]
                    op.sem = dsem[op.sem]
                    finals[id(op.sem)] = (op.sem, op.val)
                elif op.signal:
                    c += 1
                    op.val = c
                    op.sem = esem[e]
                    finals[id(op.sem)] = (op.sem, op.val)
        with nc.Block() as block:
            def run(e, eng):
                waited = {}
                for op in self.ops[e]:
                    for d in op.deps:
                        key = id(d.sem)
                        if waited.get(key, 0) >= d.val:
                            continue
                        waited[key] = d.val
                        eng.wait_ge(d.sem, d.val)
                    ins = op.fn(eng)
                    if op.signal:
                        ins.then_inc(op.sem, 16 if op.is_dma else 1)
                for sem, val in finals.values():
                    if waited.get(id(sem), 0) < val:
                        eng.wait_ge(sem, val)

            @block.tensor
            def _(eng):
                run("pe", eng)

            @block.vector
            def _(eng):
                run("dve", eng)

            @block.scalar
            def _(eng):
                run("act", eng)

            @block.gpsimd
            def _(eng):
                run("pool", eng)

            @block.sync
            def _(eng):
                run("sp", eng)


def build(debug=False):
    nc = bass.Bass("TRN2", target_bir_lowering=False)

    def din(name, shape):
        return nc.dram_tensor(name, list(shape), F32, kind="ExternalInput").ap()

    xo = din("xo", [TOK, D]); xp = din("xp", [TOK, D])
    w_in = din("w_in", [D, NIN]); w_out = din("w_out", [D, D]); pool_w = din("pool_w", [4, 128, 128])
    g1c = din("g1c", [128, 8]); g2c = din("g2c", [128, 8]); gfb = din("gfb", [128, D])
    convT = din("convT", [128, 8, 4]); igb = din("igb", [128, 1]); fgbn = din("fgbn", [128, 1])
    hngb = din("hngb", [128, 512]); pscale = din("pscale", [128, 4])
    w_router = din("w_router", [D, NE]); brb = din("brb", [128, NE])
    w_gate = din("w_gate", [NE, D, D]); w_up = din("w_up", [NE, D, D]); w_down = din("w_down", [NE, D, D])
    bgT = din("bgT", [128, NE, 8]); buT = din("buT", [128, NE, 8]); b_down = din("b_down", [NE, D])
    ident = din("ident", [128, 128]); maskrl = din("maskrl", [128, 128]); corr = din("corr", [128, 4, 16])
    flag = din("flag", [128, 1]); resetm = din("resetm", [128, 128])
    g2b = din("g2b", [128, D]); iotaj = din("iotaj", [128, 128]); ltstrict = din("ltstrict", [128, 128])
    out = nc.dram_tensor("out", [TOK, D], F32, kind="ExternalOutput").ap()
    dbg = nc.dram_tensor("dbg", [TOK, D], F32, kind="ExternalOutput").ap() if debug else None
    dbgG = nc.dram_tensor("dbgG", [TOK, NE], F32, kind="ExternalOutput").ap() if debug else None
    ne_run = int(os.environ.get("KNE", NE)) if debug else NE

    with contextlib.ExitStack() as st, contextlib.ExitStack() as semst:
        def sb(stack, name, shape, dt):
            return stack.enter_context(nc.sbuf_tensor(name, list(shape), dt))

        def ps(stack, name, shape, dt):
            return stack.enter_context(nc.psum_tensor(name, list(shape), dt))

        x1 = sb(st, "x1", [128, NT, D], F32)
        wa = sb(st, "wa", [128, 3 * 8192], BF16)
        G = sb(st, "G", [128, NT, NE], F32)
        rstd2 = sb(st, "rstd2", [128, NT], F32)
        identf = sb(st, "identf", [128, 128], F32)
        identb = sb(st, "identb", [128, 128], BF16)
        gfb_s = sb(st, "gfb_s", [128, D], F32)
        g2c_s = sb(st, "g2c_s", [128, 8], F32)
        dum = sb(st, "dum", [128, 8], F32)
        banks = [ps(st, f"bank{i}", [128, 512], F32) for i in range(8)]

        def dummies():
            return {
                "dve": lambda e: e.memset(dum[:, 0:1], 0.0),
                "act": lambda e: e.activation(out=dum[:, 1:2], in_=identf[:, 0:1], func=AF.Copy),
                "pool": lambda e: e.memset(dum[:, 3:4], 0.0),
            }

        with contextlib.ExitStack() as sa:
            S = Sched(nc, semst, "A")
            bank_i = [0]
            reserved = set()

            def nb():
                while True:
                    i = bank_i[0] % 8
                    bank_i[0] += 1
                    if i not in reserved:
                        return i

            win = wa[:, 0:8 * NIN].rearrange("p (k f) -> p k f", k=8)
            wout = sb(sa, "wout", [128, 8, D], BF16)
            dconv = sb(sa, "dconv", [128, 8, 4, 128], BF16)
            convT_s = sb(sa, "convT_s", [128, 8, 4], F32)
            g1c_s = sb(sa, "g1c_s", [128, 8], F32)
            wg = sb(sa, "wg", [128, 8, 8], BF16)
            hngb_s = sb(sa, "hngb_s", [128, 512], F32)
            poolw = sb(sa, "poolw", [128, 4, 128], BF16)
            pscale_s = sb(sa, "pscale_s", [128, 4], F32)
            wr = sb(sa, "wr", [128, 8, NE], F32)
            brb_s = sb(sa, "brb_s", [128, NE], F32)
            maskb = sb(sa, "maskb", [128, 128], BF16)
            maskf = sb(sa, "maskf", [128, 128], F32)
            corr_s = sb(sa, "corr_s", [128, 4, 16], F32)
            flag_s = sb(sa, "flag_s", [128, 1], F32)
            igb_s = sb(sa, "igb_s", [128, 1], F32)
            fgbn_s = sb(sa, "fgbn_s", [128, 1], F32)
            resetm_s = sb(sa, "resetm_s", [128, 128], F32)
            ones1 = sb(sa, "ones1", [1, 128], F32)
            rstd1 = sb(sa, "rstd1", [128, 32], F32)
            ssq = sb(sa, "ssq", [128, 48], F32)
            xin = [sb(sa, "xin0", [128, D], F32)] * 2
            hs = [sb(sa, f"hs{i}", [128, D], BF16) for i in range(2)]
            hT = [sb(sa, f"hT{i}", [128, 8, 128], BF16) for i in range(2)]
            gcol = sb(sa, "gcol", [128, 8], F32)
            grow = sb(sa, "grow", [1, 12, 128], F32)
            tokT = sb(sa, "tokT", [128, 3, 128], F32)
            bcs = sb(sa, "bcs", [128, 3, 128], F32)
            ubc = [sb(sa, f"ubc{i}", [128, 4, 3 + 128], BF16) for i in range(2)]
            halo_c = sb(sa, "halo_c", [128, 8, 3], BF16)
            qkT = [sb(sa, f"qkT{i}", [128, 8, 128], BF16) for i in range(2)]
            kTok = [sb(sa, f"kTok{i}", [128, 4, 128], BF16) for i in range(2)]
            v1 = [sb(sa, f"v1{i}", [128, 4, 129], BF16) for i in range(2)]
            osig = [sb(sa, f"osig{i}", [128, 512], BF16) for i in range(2)]
            Cst = sb(sa, "Cst", [128, 4, 129], F32)
            Ctmp = sb(sa, "Ctmp", [128, 4, 129], F32)
            Cs = sb(sa, "Cs", [128, 4, 129], BF16)
            sTp = [sb(sa, f"sTp{i}", [128, 128], BF16) for i in range(2)]
            wv = [sb(sa, f"wv{i}", [128, 129], BF16) for i in range(2)]
            dmx = sb(sa, "dmx", [128, 8], F32)
            hm = sb(sa, "hm", [128, 4, 128], F32)
            bst = sb(sa, "bst", [128, 4, 6], F32)
            mv = sb(sa, "mv", [128, 4, 2], F32)
            lnr = sb(sa, "lnr", [128, 4], F32)
            ym = sb(sa, "ym", [128, 512], BF16)
            yT = [sb(sa, f"yT{i}", [128, 8, 128], BF16) for i in range(2)]
            pu = [sb(sa, f"pu{i}", [128, 16 + 128], F32) for i in range(4)]
            phalo = sb(sa, "phalo", [128, 4, 16], F32)
            pooledT = sb(sa, "pooledT", [128, 128], BF16)
            h2f = sb(sa, "h2f", [128, D], F32)
            ymf = h2f[:, 0:512]
            gm = [hm[:, 0, :], hm[:, 1, :], hm[:, 2, :], hm[:, 3, :], h2f[:, 0:128], h2f[:, 128:256]]
            h2Tf = sb(sa, "h2Tf", [128, 8, 128], F32)
            gsb = h2Tf[:, 0:2, :]
            lg = sb(sa, "lg", [128, NE], F32)
            top8 = sb(sa, "top8", [128, 8], F32)
            rt = sb(sa, "rt", [128, 4, NE], F32)
            rsm = sb(sa, "rsm", [128, 4], F32)

            klim = float(os.environ.get("KSTAGE", "99")) if debug else 99

            def ckpt(k):
                if klim <= k:
                    S.stopped = True
            def ld(eng, dst, src, wkey, rk=()):
                return S.add(eng, lambda e: e.dma_start(out=dst, in_=src), reads=rk, writes=[wkey], dma=True)

            ld("sp", identf[:], ident, "identf")
            S.add("dve", lambda e: e.tensor_copy(out=identb[:], in_=identf[:]), reads=["identf"], writes=["identb"])
            ld("sp", maskf[:], maskrl, "maskf")
            S.add("dve", lambda e: e.tensor_copy(out=maskb[:], in_=maskf[:]), reads=["maskf"], writes=["maskb"])
            for dst, src, k in ((g1c_s, g1c, "g1c"), (g2c_s, g2c, "g2c"), (gfb_s, gfb, "gfb"), (convT_s, convT, "convT"),
                                (hngb_s, hngb, "hngb"), (pscale_s, pscale, "pscale"), (brb_s, brb, "brb"),
                                (corr_s, corr, "corr"), (flag_s, flag, "flag"), (igb_s, igb, "igb"),
                                (fgbn_s, fgbn, "fgbn"), (resetm_s, resetm, "resetm")):
                ld("sp", dst[:], src, k)
            ld("sp", wr[:], w_router.rearrange("(k p) e -> p k e", p=128), "wr")
            S.add("dve", lambda e: e.tensor_scalar(out=fgbn_s[:], in0=fgbn_s[:], scalar1=-1.0, scalar2=None, op0=ALU.mult),
                  reads=["fgbn"], writes=["fgbn"])
            w_in_v = w_in.rearrange("(k p) f -> p k f", p=128)
            ld("pool", wg[:], w_in_v[:, :, 2048:2056], "wg")
            for k in range(8):
                S.add("dve", lambda e, k=k: e.tensor_scalar(out=wg[:, k, :], in0=wg[:, k, :], scalar1=g1c_s[:, k:k + 1],
                                                            scalar2=None, op0=ALU.mult),
                      reads=["g1c", "wg"], writes=["wg"], cost=0.1)
            for k in range(8):
                ld("pool", win[:, k, :], w_in_v[:, k, :], ("wa", k))
            ld("pool", wout[:], w_out.rearrange("(k p) f -> p k f", p=128), "wout")
            ld("pool", poolw[:], pool_w.rearrange("g c d -> c g d"), "poolw")
            for k in range(8):
                if k % 2:
                    S.add("dve", lambda e, k=k: e.tensor_scalar(out=win[:, k, :], in0=win[:, k, :], scalar1=g1c_s[:, k:k + 1],
                                                                scalar2=None, op0=ALU.mult),
                          reads=["g1c", ("wa", k)], writes=[("wa", k)])
                else:
                    S.add("act", lambda e, k=k: e.activation(out=win[:, k, :], in_=win[:, k, :], func=AF.Copy,
                                                             scale=g1c_s[:, k:k + 1]),
                          reads=["g1c", ("wa", k)], writes=[("wa", k)])
            for k in range(8):
                for j in range(4):
                    S.add("dve", lambda e, k=k, j=j: e.tensor_scalar(out=dconv[:, k, j, :], in0=identf[:],
                                                                       scalar1=convT_s[:, k, j:j + 1], scalar2=None,
                                                                       op0=ALU.mult),
                          reads=["identf", "convT"], writes=[("dconv", k)])
            S.add("dve", lambda e: e.memset(ones1[:], 1.0), writes=["ones1"])
            S.add("dve", lambda e: e.memset(Cst[:], 0.0), writes=[("Cst", h) for h in range(4)])
            S.add("dve", lambda e: e.memset(halo_c[:], 0.0), writes=[("halo_c", 0), ("halo_c", 1)])
            S.add("dve", lambda e: e.memset(phalo[:], 0.0), writes=[("phalo", g) for g in range(4)])
            S.add("dve", lambda e: e.memset(v1[0][:], 1.0), writes=[("v1", 0)])
            S.add("dve", lambda e: e.memset(v1[1][:], 1.0), writes=[("v1", 1)])
            WA = [("wa", k) for k in range(8)]

            ckpt(1)
            cnt = [0]

            def norm_T(ci, first):
                i = cnt[0] % 2
                cnt[0] += 1
                own = ci >= 16
                if own:
                    xt, xkey = x1[:, ci - 16, :], ("x1", ci - 16)
                else:
                    xt, xkey = xin[0][:], ("xin", 0)
                if first or not own:
                    src = (xo if own else xp)[(ci % 16) * 128:(ci % 16 + 1) * 128, :]
                    S.add("sp", lambda e: e.dma_start(out=xt, in_=src), writes=[xkey], dma=True)
                if first:
                    S.add("act", lambda e: e.activation(out=hs[i][:], in_=xt, func=AF.Square,
                                                        accum_out=ssq[:, ci:ci + 1]),
                          reads=[xkey], writes=[("hs", i), ("ssq", ci)], cost=1.0)
                    S.add("act", lambda e: e.activation(out=ssq[:, ci:ci + 1], in_=ssq[:, ci:ci + 1], func=AF.Ln,
                                                        scale=1.0 / D, bias=EPS),
                          reads=[("ssq", ci)], writes=[("ssq", ci)])
                    S.add("act", lambda e: e.activation(out=rstd1[:, ci:ci + 1], in_=ssq[:, ci:ci + 1], func=AF.Exp,
                                                        scale=-0.5),
                          reads=[("ssq", ci)], writes=[("rstd1", ci)])
                S.add("act", lambda e: e.activation(out=hs[i][:], in_=xt, func=AF.Copy, scale=rstd1[:, ci:ci + 1]),
                      reads=[xkey, ("rstd1", ci)], writes=[("hs", i)], cost=1.1)
                b = nb()
                pb = banks[b][:].bitcast(BF16)

                def tr(e):
                    for k in range(8):
                        ins = e.transpose(out=pb[:, k * 128:(k + 1) * 128], in_=hs[i][:, k * 128:(k + 1) * 128],
                                          identity=identb[:])
                    return ins
                S.add("pe", tr, reads=[("hs", i), "identb"], writes=[("bank", b)], cost=0.8)
                S.add("dve", lambda e: e.tensor_copy(out=hT[i][:].rearrange("p k t -> p (k t)"), in_=pb[:, 0:1024]),
                      reads=[("bank", b)], writes=[("hT", i)], cost=0.6)
                return i

            gb = nb()
            reserved.add(gb)
            gall = banks[gb][:, 0:256].rearrange("p (c a) -> p c a", a=8)
            for ci in range(32):
                i = norm_T(ci, True)

                def gm_(e, i=i, ci=ci):
                    for k in range(8):
                        ins = e.matmul(gall[:, ci, :], lhsT=hT[i][:, k, :], rhs=wg[:, k, :],
                                       start=(k == 0), stop=(k == 7), skip_group_check=True)
                    return ins
                S.add("pe", gm_, reads=[("hT", i), "wg"], writes=[("bank", gb)])
            ckpt(2)
            for a in range(2):
                S.add("dve", lambda e, a=a: e.tensor_copy(out=gsb[:, a, :].rearrange("p (c h) -> p c h", h=4),
                                                          in_=gall[:, :, a * 4:(a + 1) * 4]),
                      reads=[("bank", gb)], writes=[("gsb", a)])
            reserved.discard(gb)
            tb = nb()
            tps = banks[tb]

            def trg(e):
                e.transpose(out=tps[:, 0:128], in_=gsb[:, 0, :], identity=identf[:])
                return e.transpose(out=tps[:, 128:256], in_=gsb[:, 1, :], identity=identf[:])
            S.add("pe", trg, reads=[("gsb", 0), ("gsb", 1), "identf"], writes=[("bank", tb)])
            IG, BP, DD, T0, T1, T2 = gm
            S.add("act", lambda e: e.activation(out=IG[:], in_=tps[:, 0:128], func=AF.Identity, bias=igb_s[:, 0:1]),
                  reads=[("bank", tb), "igb"], writes=["IG"])
            S.add("act", lambda e: e.activation(out=T0[:], in_=tps[:, 128:256], func=AF.Exp, scale=-1.0,
                                                bias=fgbn_s[:, 0:1]),
                  reads=[("bank", tb), "fgbn"], writes=["T0"])
            S.add("act", lambda e: e.activation(out=T0[:], in_=T0[:], func=AF.Ln, bias=1.0),
                  reads=["T0"], writes=["T0"])
            S.add("dve", lambda e: e.tensor_tensor_scan(out=BP[:], data0=resetm_s[:], data1=T0[:], initial=0.0,
                                                        op0=ALU.mult, op1=ALU.add),
                  reads=["T0", "resetm"], writes=["BP"])
            S.add("dve", lambda e: e.tensor_tensor(out=DD[:], in0=IG[:], in1=BP[:], op=ALU.add),
                  reads=["IG", "BP"], writes=["DD"])
            S.add("dve", lambda e: e.tensor_reduce(out=gcol[:, 0:1], in_=DD[:], axis=AX.X, op=ALU.max),
                  reads=["DD"], writes=["gcol"])
            S.add("dve", lambda e: e.tensor_scalar(out=gcol[:, 1:2], in0=BP[:, 127:128], scalar1=-1.0, scalar2=None,
                                                   op0=ALU.mult),
                  reads=["BP", "gcol"], writes=["gcol"])
            S.add("dve", lambda e: e.tensor_tensor(out=gcol[:, 2:3], in0=gcol[:, 0:1], in1=gcol[:, 1:2], op=ALU.add),
                  reads=["gcol"], writes=["gcol"])
            rb = nb()
            rps = banks[rb]

            def trc(e):
                for q in range(3):
                    ins = e.transpose(out=rps[0:1, q * 128:(q + 1) * 128], in_=gcol[:, q:q + 1], identity=identf[:])
                return ins
            S.add("pe", trc, reads=["gcol", "identf"], writes=[("bank", rb)])
            S.add("dve", lambda e: e.tensor_copy(out=grow[:, 0:3, :].rearrange("p a c -> p (a c)"),
                                                 in_=rps[0:1, 0:384]),
                  reads=[("bank", rb)], writes=["grow"])
            R = lambda q: grow[0:1, q, :]
            for h in range(4):
                S.add("dve", lambda e, h=h: e.tensor_tensor_scan(out=grow[0:1, 3, h:64:4], data0=grow[0:1, 1, h:64:4],
                                                                 data1=grow[0:1, 2, h:64:4], initial=0.0,
                                                                 op0=ALU.add, op1=ALU.max),
                      reads=["grow"], writes=["grow"])
            S.add("dve", lambda e: e.memset(grow[0:1, 4, 0:4], 0.0), reads=["grow"], writes=["grow"])
            S.add("dve", lambda e: e.tensor_copy(out=grow[0:1, 4, 4:64], in_=grow[0:1, 3, 0:60]),
                  reads=["grow"], writes=["grow"])
            S.add("dve", lambda e: e.tensor_scalar(out=grow[0:1, 4, 64:68], in0=grow[0:1, 3, 60:64],
                                                   scalar1=flag_s[0:1, 0:1], scalar2=None, op0=ALU.mult),
                  reads=["grow", "flag"], writes=["grow"])
            for h in range(4):
                S.add("dve", lambda e, h=h: e.tensor_tensor_scan(out=grow[0:1, 3, 64 + h:128:4],
                                                                 data0=grow[0:1, 1, 64 + h:128:4],
                                                                 data1=grow[0:1, 2, 64 + h:128:4],
                                                                 initial=grow[0:1, 4, 64 + h:65 + h],
                                                                 op0=ALU.add, op1=ALU.max),
                      reads=["grow"], writes=["grow"])
            S.add("dve", lambda e: e.tensor_copy(out=grow[0:1, 4, 68:128], in_=grow[0:1, 3, 64:124]),
                  reads=["grow"], writes=["grow"])
            S.add("dve", lambda e: e.tensor_tensor(out=R(5), in0=R(4), in1=R(0), op=ALU.max), reads=["grow"], writes=["grow"])
            S.add("dve", lambda e: e.tensor_tensor(out=R(9), in0=R(1), in1=R(4), op=ALU.add), reads=["grow"], writes=["grow"])
            S.add("dve", lambda e: e.tensor_tensor(out=R(9), in0=R(9), in1=R(3), op=ALU.subtract), reads=["grow"], writes=["grow"])
            S.add("act", lambda e: e.activation(out=R(6), in_=R(9), func=AF.Exp), reads=["grow"], writes=["grow"])
            S.add("dve", lambda e: e.tensor_tensor(out=R(9), in0=R(2), in1=R(3), op=ALU.subtract), reads=["grow"], writes=["grow"])
            S.add("act", lambda e: e.activation(out=R(7), in_=R(9), func=AF.Exp), reads=["grow"], writes=["grow"])
            S.add("dve", lambda e: e.tensor_tensor(out=R(9), in0=R(4), in1=R(5), op=ALU.subtract), reads=["grow"], writes=["grow"])
            S.add("act", lambda e: e.activation(out=R(8), in_=R(9), func=AF.Exp), reads=["grow"], writes=["grow"])
            S.add("dve", lambda e: e.tensor_scalar(out=R(10), in0=R(5), scalar1=-1.0, scalar2=LNSC, op0=ALU.mult, op1=ALU.add),
                  reads=["grow"], writes=["grow"])
            S.add("dve", lambda e: e.tensor_scalar(out=R(11), in0=R(0), scalar1=-1.0, scalar2=LNSC, op0=ALU.mult, op1=ALU.add),
                  reads=["grow"], writes=["grow"])
            S.add("dve", lambda e: e.tensor_scalar(out=R(9), in0=R(5), scalar1=-1.0, scalar2=None, op0=ALU.mult),
                  reads=["grow"], writes=["grow"])
            bb = nb()
            bps = banks[bb]

            def bc(e):
                for q in range(3):
                    ins = e.matmul(bps[:, q * 128:(q + 1) * 128], lhsT=ones1[0:1, :], rhs=R(6 + q), start=True, stop=True,
                                   skip_group_check=True)
                return ins
            S.add("pe", bc, reads=["grow", "ones1"], writes=[("bank", bb)])
            S.add("dve", lambda e: e.tensor_copy(out=bcs[:].rearrange("p a c -> p (a c)"), in_=bps[:, 0:384]),
                  reads=[("bank", bb)], writes=["bcs"])
            cb = nb()
            cps = banks[cb]

            def trr(e):
                for q in range(3):
                    ins = e.matmul(cps[:, q:q + 1], lhsT=R(9 + q), rhs=ones1[0:1, 0:1], start=True, stop=True,
                                   skip_group_check=True)
                return ins
            S.add("pe", trr, reads=["grow", "ones1"], writes=[("bank", cb)])
            S.add("dve", lambda e: e.tensor_copy(out=gcol[:, 3:6], in_=cps[:, 0:3]), reads=[("bank", cb)], writes=["gcol"])
            S.add("act", lambda e: e.activation(out=T0[:], in_=DD[:], func=AF.Exp, bias=gcol[:, 4:5]),
                  reads=["DD", "gcol"], writes=["T0"])
            S.add("act", lambda e: e.activation(out=T1[:], in_=DD[:], func=AF.Exp, bias=gcol[:, 5:6]),
                  reads=["DD", "gcol"], writes=["T1"])
            S.add("act", lambda e: e.activation(out=T2[:], in_=BP[:], func=AF.Exp, bias=gcol[:, 3:4]),
                  reads=["BP", "gcol"], writes=["T2"])
            kb = nb()
            kps = banks[kb]

            def trt(e):
                for q, t in enumerate((T0, T1, T2)):
                    ins = e.transpose(out=kps[:, q * 128:(q + 1) * 128], in_=t[:], identity=identf[:])
                return ins
            S.add("pe", trt, reads=["T0", "T1", "T2", "identf"], writes=[("bank", kb)])
            S.add("dve", lambda e: e.tensor_copy(out=tokT[:].rearrange("p a c -> p (a c)"), in_=kps[:, 0:384]),
                  reads=[("bank", kb)], writes=["tokT"])

            ucnt = [0]

            def proj_conv(i, cg, p, etmp, ekey, save_only=False):
                b = nb()
                pb = banks[b]

                def mm(e):
                    for c in range(4):
                        fk = cg * 4 + c
                        for k in range(8):
                            ins = e.matmul(pb[:, c * 128:(c + 1) * 128], lhsT=win[:, k, fk * 128:(fk + 1) * 128],
                                           rhs=hT[i][:, k, :], start=(k == 0), stop=(k == 7), skip_group_check=True)
                    return ins
                S.add("pe", mm, reads=[("hT", i)] + WA, writes=[("bank", b)], cost=2.6)
                u = ucnt[0] % 2
                ucnt[0] += 1
                S.add("dve", lambda e: e.tensor_copy(out=ubc[u][:, :, 0:3], in_=halo_c[:, cg * 4:(cg + 1) * 4, :]),
                      reads=[("halo_c", cg)], writes=[("ubc", u)])
                S.add("act", lambda e: e.activation(out=ubc[u][:, :, 3:131], in_=pb[:, :].rearrange("p (c t) -> p c t", c=4),
                                                    func=AF.Copy),
                      reads=[("bank", b)], writes=[("ubc", u)], cost=0.6)
                S.add("dve", lambda e: e.tensor_copy(out=halo_c[:, cg * 4:(cg + 1) * 4, :], in_=ubc[u][:, :, 128:131]),
                      reads=[("ubc", u)], writes=[("halo_c", cg)])
                if save_only:
                    return
                b2 = nb()
                pb2 = banks[b2]

                def cv(e):
                    for c in range(4):
                        fk = cg * 4 + c
                        for j in range(4):
                            ins = e.matmul(pb2[:, c * 128:(c + 1) * 128], lhsT=dconv[:, fk, j, :], rhs=ubc[u][:, c, j:j + 128],
                                           start=(j == 0), stop=(j == 3), skip_group_check=True)
                    return ins
                S.add("pe", cv, reads=[("ubc", u)] + [("dconv", cg * 4 + c) for c in range(4)], writes=[("bank", b2)], cost=1.4)
                S.add("act", lambda e: e.activation(out=etmp, in_=pb2[:, :], func=AF.Exp, scale=-1.0),
                      reads=[("bank", b2)], writes=[ekey], cost=0.6)
                S.add("act", lambda e: e.activation(out=etmp, in_=etmp, func=AF.Ln, bias=1.0),
                      reads=[ekey], writes=[ekey], cost=0.6)
                S.add("act", lambda e: e.activation(out=etmp, in_=etmp, func=AF.Exp, scale=-1.0),
                      reads=[ekey], writes=[ekey], cost=0.6)
                S.add("dve", lambda e: e.tensor_tensor(out=qkT[p][:, cg * 4:(cg + 1) * 4, :].rearrange("p c t -> p (c t)"),
                                                       in0=pb2[:, :], in1=etmp, op=ALU.mult),
                      reads=[("bank", b2), ekey], writes=[("qkT", p, cg * 4 + c) for c in range(4)], cost=0.6)

            def proj_v(i, p):
                b = nb()
                pb = banks[b]

                def mm(e):
                    for k in range(8):
                        ins = e.matmul(pb[:, :], lhsT=hT[i][:, k, :], rhs=win[:, k, 1024:1536], start=(k == 0), stop=(k == 7))
                    return ins
                S.add("pe", mm, reads=[("hT", i)] + WA, writes=[("bank", b)], cost=2.5)
                S.add("act", lambda e: e.activation(out=v1[p][:, :, 0:128], in_=pb[:, :].rearrange("p (h d) -> p h d", h=4),
                                                    func=AF.Copy),
                      reads=[("bank", b)], writes=[("v1", p)])

            def k_tok(p):
                b = nb()
                pb = banks[b][:].bitcast(BF16)

                def tr(e):
                    for h in range(4):
                        ins = e.transpose(out=pb[:, h * 128:(h + 1) * 128], in_=qkT[p][:, 4 + h, :], identity=identb[:])
                    return ins
                S.add("pe", tr, reads=[("qkT", p, 4 + h) for h in range(4)] + ["identb"], writes=[("bank", b)])
                S.add("dve", lambda e: e.tensor_copy(out=kTok[p][:].rearrange("p h d -> p (h d)"), in_=pb[:, 0:512]),
                      reads=[("bank", b)], writes=[("kTok", p)])

            def pool_proj(i, g, dst_fn):
                b = nb()
                pb = banks[b]

                def mm(e):
                    for k in range(8):
                        ins = e.matmul(pb[:, 0:128], lhsT=win[:, k, 2056 + g * 128:2056 + (g + 1) * 128],
                                       rhs=hT[i][:, k, :], start=(k == 0), stop=(k == 7))
                    return ins
                S.add("pe", mm, reads=[("hT", i)] + WA, writes=[("bank", b)])
                return b, pb

            def state_update(ci, p):
                for h in range(4):
                    ch = ci * 4 + h
                    w_ = wv[h % 2]
                    S.add("act", lambda e, h=h, ch=ch, w_=w_: e.activation(out=w_[:], in_=v1[p][:, h, :], func=AF.Copy,
                                                                           scale=tokT[:, 1, ch:ch + 1]),
                          reads=[("v1", p), "tokT"], writes=[("wv", h % 2)])
                    b = nb()
                    pb = banks[b]
                    S.add("pe", lambda e, h=h, w_=w_, pb=pb: e.matmul(pb[:, 0:129], lhsT=kTok[p][:, h, :], rhs=w_[:],
                                                                      start=True, stop=True),
                          reads=[("kTok", p), ("wv", h % 2)], writes=[("bank", b)])
                    S.add("dve", lambda e, h=h, ch=ch: e.tensor_scalar(out=Ctmp[:, h, :], in0=Cst[:, h, :],
                                                                       scalar1=bcs[:, 0, ch:ch + 1], scalar2=None,
                                                                       op0=ALU.mult),
                          reads=[("Cst", h), "bcs"], writes=[("Ctmp", h)])
                    S.add("dve", lambda e, h=h, ch=ch, pb=pb: e.scalar_tensor_tensor(out=Cst[:, h, :], in0=pb[:, 0:129],
                                                                                     scalar=bcs[:, 1, ch:ch + 1],
                                                                                     in1=Ctmp[:, h, :], op0=ALU.mult,
                                                                                     op1=ALU.add),
                          reads=[("bank", b), ("Ctmp", h), "bcs"], writes=[("Cst", h)])

            def front_prefix(ci, p):
                i = norm_T(ci, False)
                et, ek = h2f[:, 512:1024], "h2f"
                proj_conv(i, 1, p, et, ek)
                if ci == 15:
                    proj_conv(i, 0, p, et, ek, save_only=True)
                    for g in range(4):
                        b, pb = pool_proj(i, g, None)
                        S.add("act", lambda e, g=g, pb=pb: e.activation(out=phalo[:, g, :], in_=pb[:, 112:128], func=AF.Copy),
                              reads=[("bank", b)], writes=[("phalo", g)])
                proj_v(i, p)
                k_tok(p)

            for ci in range(16):
                front_prefix(ci, ci % 2)
                if ci > 0:
                    state_update(ci - 1, (ci - 1) % 2)
            ckpt(4)

            def front_own(ti, p):
                ci = 16 + ti
                i = norm_T(ci, False)
                et, ek = xin[0][:, 0:512], ("xin", 0)
                proj_conv(i, 0, p, et, ek)
                proj_conv(i, 1, p, et, ek)
                proj_v(i, p)
                b = nb()
                pb = banks[b]

                def mm(e):
                    for k in range(8):
                        ins = e.matmul(pb[:, :], lhsT=hT[i][:, k, :], rhs=win[:, k, 1536:2048], start=(k == 0), stop=(k == 7))
                    return ins
                S.add("pe", mm, reads=[("hT", i)] + WA, writes=[("bank", b)], cost=2.5)
                S.add("act", lambda e: e.activation(out=et, in_=pb[:, :], func=AF.Exp, scale=-1.0),
                      reads=[("bank", b)], writes=[ek], cost=0.6)
                S.add("dve", lambda e: e.tensor_scalar(out=et, in0=et, scalar1=1.0, scalar2=None, op0=ALU.add),
                      reads=[ek], writes=[ek], cost=0.5)
                def rcp(e):
                    with nc.allow_low_precision("sigmoid gate is stored in bf16 (matmul-operand precision)"):
                        return e.reciprocal(out=osig[p][:], in_=et)
                S.add("dve", rcp, reads=[ek], writes=[("osig", p)], cost=0.5)
                k_tok(p)
                for g in range(4):
                    b, pbg = pool_proj(i, g, None)
                    A_ = pu[0]
                    S.add("dve", lambda e, g=g: e.tensor_copy(out=A_[:, 0:16], in_=phalo[:, g, :]),
                          reads=[("phalo", g)], writes=["puA"])
                    S.add("act", lambda e, pbg=pbg: e.activation(out=A_[:, 16:144], in_=pbg[:, 0:128], func=AF.Copy),
                          reads=[("bank", b)], writes=["puA"])
                    S.add("dve", lambda e, g=g: e.tensor_copy(out=phalo[:, g, :], in_=A_[:, 128:144]),
                          reads=["puA"], writes=[("phalo", g)])
                    src_t, src_k = A_, "puA"
                    sh, lo = 1, 1
                    for s_ in range(g + 1):
                        dst_t = pu[1 + (s_ % 3)]
                        dk = "pu%d" % (1 + (s_ % 3))
                        S.add("dve", lambda e, src_t=src_t, dst_t=dst_t, sh=sh, lo=lo: e.tensor_tensor(
                            out=dst_t[:, lo:144], in0=src_t[:, lo:144], in1=src_t[:, lo - sh:144 - sh], op=ALU.add),
                            reads=[src_k], writes=[dk])
                        src_t, src_k = dst_t, dk
                        sh *= 2
                        lo = 2 * sh - 1
                    wdw = float(2 ** (g + 1))
                    if ti == 0:
                        S.add("dve", lambda e, src_t=src_t, g=g: e.tensor_tensor(out=src_t[:, 16:32], in0=src_t[:, 16:32],
                                                                                 in1=corr_s[:, g, :], op=ALU.mult),
                              reads=[src_k, "corr"], writes=[src_k])
                    S.add("dve", lambda e, src_t=src_t, wdw=wdw: e.scalar_tensor_tensor(
                        out=pooledT[:], in0=src_t[:, 16:144], scalar=1.0 / wdw, in1=A_[:, 16:144], op0=ALU.mult,
                        op1=ALU.subtract),
                        reads=[src_k, "puA"], writes=["pooledT"])
                    b2 = nb()
                    pb2 = banks[b2]
                    S.add("pe", lambda e, g=g, pb2=pb2: e.matmul(pb2[:, 0:128], lhsT=poolw[:, g, :], rhs=pooledT[:],
                                                                 start=True, stop=True),
                          reads=["pooledT", "poolw"], writes=[("bank", b2)])
                    S.add("act", lambda e, g=g, pb2=pb2: e.activation(out=yT[p][:, 4 + g, :], in_=pb2[:, 0:128], func=AF.Copy,
                                                                      scale=pscale_s[:, g:g + 1]),
                          reads=[("bank", b2), "pscale"], writes=[("yT", p, 4 + g)])

            def back_own(ti, p):
                ci = 16 + ti
                for h in range(4):
                    ch = ci * 4 + h
                    b = nb()
                    pb = banks[b]
                    S.add("pe", lambda e, h=h, pb=pb: e.matmul(pb[:, 0:128], lhsT=qkT[p][:, 4 + h, :], rhs=qkT[p][:, h, :],
                                                               start=True, stop=True),
                          reads=[("qkT", p, 4 + h), ("qkT", p, h)], writes=[("bank", b)])
                    s_ = sTp[h % 2]
                    S.add("dve", lambda e, ch=ch, pb=pb, s_=s_: e.scalar_tensor_tensor(
                        out=s_[:], in0=pb[:, 0:128], scalar=tokT[:, 0, ch:ch + 1], in1=maskb[:], op0=ALU.mult, op1=ALU.mult),
                        reads=[("bank", b), "tokT", "maskb"], writes=[("sTp", h % 2)])
                    S.add("act", lambda e, h=h, ch=ch: e.activation(out=Cs[:, h, :], in_=Cst[:, h, :], func=AF.Copy,
                                                                    scale=bcs[:, 2, ch:ch + 1]),
                          reads=[("Cst", h), "bcs"], writes=[("Cs", h)])
                    b2 = nb()
                    pb2 = banks[b2]

                    def nd(e, h=h, pb2=pb2, s_=s_):
                        e.matmul(pb2[:, 0:129], lhsT=s_[:], rhs=v1[p][:, h, :], start=True, stop=False)
                        return e.matmul(pb2[:, 0:129], lhsT=qkT[p][:, h, :], rhs=Cs[:, h, :], start=False, stop=True)
                    S.add("pe", nd, reads=[("sTp", h % 2), ("v1", p), ("qkT", p, h), ("Cs", h)], writes=[("bank", b2)])
                    S.add("dve", lambda e, h=h, pb2=pb2: e.tensor_scalar(out=dmx[:, h:h + 1], in0=pb2[:, 128:129], scalar1=-1.0,
                                                                         scalar2=None, op0=ALU.mult),
                          reads=[("bank", b2)], writes=[("dmx", h)])
                    S.add("dve", lambda e, h=h, pb2=pb2: e.tensor_tensor(out=dmx[:, h:h + 1], in0=pb2[:, 128:129],
                                                                         in1=dmx[:, h:h + 1], op=ALU.max),
                          reads=[("bank", b2), ("dmx", h)], writes=[("dmx", h)])
                    S.add("dve", lambda e, h=h, ch=ch: e.tensor_scalar(
                        out=dmx[:, h:h + 1], in0=dmx[:, h:h + 1], scalar1=tokT[:, 2, ch:ch + 1], scalar2=None,
                        op0=ALU.max),
                        reads=[("dmx", h), "tokT"], writes=[("dmx", h)])
                    S.add("dve", lambda e, h=h: e.reciprocal(out=dmx[:, 4 + h:5 + h], in_=dmx[:, h:h + 1]),
                          reads=[("dmx", h)], writes=[("dmx", 4 + h)])
                    S.add("act", lambda e, h=h, pb2=pb2: e.activation(out=hm[:, h, :], in_=pb2[:, 0:128], func=AF.Copy,
                                                                      scale=dmx[:, 4 + h:5 + h]),
                          reads=[("bank", b2), ("dmx", 4 + h)], writes=[("hm", h)])
                    S.add("dve", lambda e, h=h: e.bn_stats(out=bst[:, h, :], in_=hm[:, h, :]),
                          reads=[("hm", h)], writes=[("bst", h)])
                    S.add("dve", lambda e, h=h: e.bn_aggr(out=mv[:, h, :], in_=bst[:, h, :]),
                          reads=[("bst", h)], writes=[("mv", h)])
                state_update(ci, p)
                S.add("act", lambda e: e.activation(out=lnr[:], in_=mv[:, :, 1], func=AF.Ln, bias=EPS),
                      reads=[("mv", h) for h in range(4)], writes=["lnr"])
                S.add("act", lambda e: e.activation(out=lnr[:], in_=lnr[:], func=AF.Exp, scale=-0.5),
                      reads=["lnr"], writes=["lnr"])
                for h in range(4):
                    S.add("dve", lambda e, h=h: e.tensor_scalar(out=h2f[:, h * 128:(h + 1) * 128], in0=hm[:, h, :],
                                                                scalar1=mv[:, h, 0:1], scalar2=lnr[:, h:h + 1],
                                                                op0=ALU.subtract, op1=ALU.mult),
                          reads=[("hm", h), ("mv", h), "lnr"], writes=["h2f"])
                S.add("dve", lambda e: e.tensor_tensor(out=ymf, in0=ymf, in1=hngb_s[:], op=ALU.mult),
                      reads=["h2f", "hngb"], writes=["h2f"])
                S.add("dve", lambda e: e.tensor_tensor(out=ym[:], in0=ymf, in1=osig[p][:], op=ALU.mult),
                      reads=["h2f", ("osig", p)], writes=["ym"])
                b = nb()
                pbb = banks[b][:].bitcast(BF16)

                def tr(e, pbb=pbb):
                    for h in range(4):
                        ins = e.transpose(out=pbb[:, h * 128:(h + 1) * 128], in_=ym[:, h * 128:(h + 1) * 128],
                                          identity=identb[:])
                    return ins
                S.add("pe", tr, reads=["ym", "identb"], writes=[("bank", b)])
                S.add("act", lambda e, pbb=pbb: e.activation(out=yT[p][:, 0:4, :].rearrange("p h t -> p (h t)"), in_=pbb[:, 0:512],
                                                             func=AF.Copy),
                      reads=[("bank", b)], writes=[("yT", p, h) for h in range(4)])
                for hf in range(2):
                    b = nb()
                    pb = banks[b]

                    def mm(e, hf=hf, pb=pb):
                        for k in range(8):
                            ins = e.matmul(pb[:, :], lhsT=yT[p][:, k, :], rhs=wout[:, k, hf * 512:(hf + 1) * 512],
                                           start=(k == 0), stop=(k == 7))
                        return ins
                    S.add("pe", mm, reads=[("yT", p, k) for k in range(8)] + ["wout"], writes=[("bank", b)], cost=2.5)
                    S.add("dve", lambda e, hf=hf, pb=pb: e.tensor_tensor(
                        out=x1[:, ti, hf * 512:(hf + 1) * 512], in0=pb[:, :], in1=x1[:, ti, hf * 512:(hf + 1) * 512], op=ALU.add),
                        reads=[("bank", b), ("x1", ti)], writes=[("x1", ti)])
                S.add("act", lambda e: e.activation(out=h2f[:], in_=x1[:, ti, :], func=AF.Square,
                                                    accum_out=ssq[:, 32 + ti:33 + ti]),
                      reads=[("x1", ti)], writes=["h2f", ("ssq", 32 + ti)])
                S.add("act", lambda e: e.activation(out=ssq[:, 32 + ti:33 + ti], in_=ssq[:, 32 + ti:33 + ti],
                                                    func=AF.Ln, scale=1.0 / D, bias=EPS),
                      reads=[("ssq", 32 + ti)], writes=[("ssq", 32 + ti)])
                S.add("act", lambda e: e.activation(out=rstd2[:, ti:ti + 1], in_=ssq[:, 32 + ti:33 + ti],
                                                    func=AF.Exp, scale=-0.5),
                      reads=[("ssq", 32 + ti)], writes=[("rstd2", ti)])
                S.add("act", lambda e: e.activation(out=h2f[:], in_=x1[:, ti, :], func=AF.Copy, scale=rstd2[:, ti:ti + 1]),
                      reads=[("x1", ti), ("rstd2", ti)], writes=["h2f"])
                for hf in range(2):
                    b = nb()
                    pb = banks[b]

                    def tr(e, hf=hf, pb=pb):
                        for j in range(4):
                            k = hf * 4 + j
                            ins = e.transpose(out=pb[:, j * 128:(j + 1) * 128], in_=h2f[:, k * 128:(k + 1) * 128],
                                              identity=identf[:])
                        return ins
                    S.add("pe", tr, reads=["h2f", "identf"], writes=[("bank", b)])
                    for j in range(4):
                        k = hf * 4 + j
                        S.add("act" if hf else "dve",
                              (lambda e, k=k, j=j, pb=pb: e.activation(out=h2Tf[:, k, :], in_=pb[:, j * 128:(j + 1) * 128],
                                                                       func=AF.Copy, scale=g2c_s[:, k:k + 1])) if hf else
                              (lambda e, k=k, j=j, pb=pb: e.tensor_scalar(out=h2Tf[:, k, :], in0=pb[:, j * 128:(j + 1) * 128],
                                                                          scalar1=g2c_s[:, k:k + 1], scalar2=None,
                                                                          op0=ALU.mult)),
                              reads=[("bank", b), "g2c"], writes=[("h2Tf", k)])
                b = nb()
                pb = banks[b]

                def rmm(e, pb=pb):
                    for k in range(8):
                        ins = e.matmul(pb[:, 0:NE], lhsT=h2Tf[:, k, :], rhs=wr[:, k, :], start=(k == 0), stop=(k == 7))
                    return ins
                S.add("pe", rmm, reads=[("h2Tf", k) for k in range(8)] + ["wr"], writes=[("bank", b)])
                S.add("dve", lambda e, pb=pb: e.tensor_tensor(out=lg[:], in0=pb[:, 0:NE], in1=brb_s[:], op=ALU.add),
                      reads=[("bank", b), "brb"], writes=["lg"])
                S.add("dve", lambda e: e.max(out=top8[:], in_=lg[:]), reads=["lg"], writes=["top8"])
                S.add("dve", lambda e: e.tensor_scalar(out=rt[:, 0, :], in0=lg[:], scalar1=top8[:, 3:4], scalar2=None,
                                                       op0=ALU.is_ge),
                      reads=["lg", "top8"], writes=["rt0"])
                S.add("dve", lambda e: e.tensor_scalar(out=rsm[:, 0:1], in0=top8[:, 0:1], scalar1=-1.0, scalar2=None,
                                                       op0=ALU.mult),
                      reads=["top8"], writes=["rsm0"])
                S.add("act", lambda e: e.activation(out=rt[:, 1, :], in_=lg[:], func=AF.Exp, bias=rsm[:, 0:1]),
                      reads=["lg", "rsm0"], writes=["rt1"])
                S.add("dve", lambda e: e.tensor_tensor(out=rt[:, 2, :], in0=rt[:, 1, :], in1=rt[:, 0, :], op=ALU.mult),
                      reads=["rt0", "rt1"], writes=["rt2"])
                S.add("dve", lambda e: e.tensor_reduce(out=rsm[:, 1:2], in_=rt[:, 2, :], axis=AX.X, op=ALU.add),
                      reads=["rt2"], writes=["rsm1"])
                S.add("dve", lambda e: e.reciprocal(out=rsm[:, 2:3], in_=rsm[:, 1:2]), reads=["rsm1"], writes=["rsm2"])
                S.add("dve", lambda e: e.tensor_scalar(out=G[:, ti, :], in0=rt[:, 2, :], scalar1=rsm[:, 2:3],
                                                       scalar2=None, op0=ALU.mult),
                      reads=["rt2", "rsm2"], writes=[("G", ti)])

            front_own(0, 0)
            state_update(15, 1)
            for h in range(4):
                S.add("dve", lambda e, h=h: e.tensor_scalar(out=Cst[:, h, :], in0=Cst[:, h, :], scalar1=flag_s[:, 0:1],
                                                            scalar2=None, op0=ALU.mult),
                      reads=[("Cst", h), "flag"], writes=[("Cst", h)])
            for ti in range(NT):
                if ti + 1 < NT:
                    front_own(ti + 1, (ti + 1) % 2)
                back_own(ti, ti % 2)
            S.stopped = False
            if debug:
                for ti in range(NT):
                    S.add("sp", lambda e, ti=ti: e.dma_start(out=dbg[ti * 128:(ti + 1) * 128, :], in_=x1[:, ti, :]),
                          reads=[("x1", ti)], dma=True)
                    S.add("sp", lambda e, ti=ti: e.dma_start(out=dbgG[ti * 128:(ti + 1) * 128, :], in_=G[:, ti, :]),
                          reads=[("G", ti)], dma=True)
            dm = dummies()
            dpb = banks[0]
            dm["pe"] = lambda e: e.matmul(dpb[0:1, 0:1], lhsT=ones1[0:1, 0:1], rhs=ones1[0:1, 0:1], start=True, stop=True,
                                          skip_group_check=True)
            S.emit(dm)

        with contextlib.ExitStack() as sbk:
            if debug and os.environ.get("KSKIPB"):
                return nc
            S = Sched(nc, semst, "B")
            S.ignore_cost = True
            bank_i = [0]

            def nb():
                i = bank_i[0] % 8
                bank_i[0] += 1
                return i
            h2 = sb(sbk, "h2", [128, NT, D], BF16)
            g2b_s = sb(sbk, "g2b_s", [128, D], F32)
            bgT_s = sb(sbk, "bgT_s", [128, NE, 8], F32)
            buT_s = sb(sbk, "buT_s", [128, NE, 8], F32)
            bdn = sb(sbk, "bdn", [NE, D], F32)
            GTs = [sb(sbk, f"GTs{i}", [NE, 128], F32) for i in range(2)]
            slot = sb(sbk, "slot", [128, NT, NE], F32)
            Ghl = sb(sbk, "Ghl", [128, NT, NE, 2], BF16)
            Mb = sb(sbk, "Mb", [128, NT, NE], BF16)
            Mf = [sb(sbk, f"Mf{i}", [128, NE], F32) for i in range(2)]
            Gr = [sb(sbk, f"Gr{i}", [128, NE], F32) for i in range(2)]
            iota_s = sb(sbk, "iota_s", [128, 128], F32)
            ltf = sb(sbk, "ltf", [128, 128], F32)
            lts = sb(sbk, "lts", [128, 128], BF16)
            onesb = sb(sbk, "onesb", [128, 128], BF16)
            P = [sb(sbk, "P0", [128, NT, 128], BF16)] * 2
            PT = [sb(sbk, f"PT{i}", [128, 4, 512], BF16) for i in range(2)]
            gsel = [sb(sbk, f"gsel{i}", [128, 4], F32) for i in range(2)]
            xTs = sb(sbk, "xTs", [128, 8, 512], BF16)
            aT = sb(sbk, "aT", [128, 8, 512], BF16)
            ysc = [sb(sbk, f"ysc{i}", [128, D], BF16) for i in range(2)]
            gc = [sb(sbk, "gc0", [128, 512], F32)] * 2
            sg = [sb(sbk, "sg0", [128, 512], F32)] * 2
            uc = [sb(sbk, "uc0", [128, 512], F32)] * 2
            ones1b = sb(sbk, "ones1b", [1, 8], F32)
            for dst, src, k in ((bgT_s, bgT, "bgT"), (buT_s, buT, "buT"), (bdn, b_down, "bdn"), (g2b_s, g2b, "g2b"),
                                (iota_s, iotaj, "iota"), (ltf, ltstrict, "ltf")):
                S.add("sp", lambda e, dst=dst, src=src: e.dma_start(out=dst[:], in_=src), writes=[k], dma=True)
            S.add("dve", lambda e: e.memset(ones1b[:], 1.0), writes=["ones1b"])
            S.add("dve", lambda e: e.memset(onesb[:], 1.0), writes=["onesb"])
            S.add("dve", lambda e: e.tensor_copy(out=lts[:], in_=ltf[:]), reads=["ltf"], writes=["lts"])
            S.add("dve", lambda e: e.tensor_scalar(out=buT_s[:], in0=buT_s[:], scalar1=1.0, scalar2=None, op0=ALU.add),
                  reads=["buT"], writes=["buT"])
            wsl = [wa[:, s * 8192:(s + 1) * 8192].rearrange("p (k f) -> p k f", k=8) for s in range(3)]
            wsrc = []
            for e_ in range(NE):
                wsrc += [w_gate[e_], w_up[e_], w_down[e_]]

            def wload(mi):
                s = mi % 3
                S.add("pool", lambda e, mi=mi, s=s: e.dma_start(out=wsl[s], in_=wsrc[mi].rearrange("(k p) f -> p k f", p=128)),
                      writes=[("ws", s)], dma=True, cost=14.0)
            if ne_run > 0:
                wload(0); wload(1); wload(2)
            for ti in range(NT):
                i = ti % 2
                S.add("dve", lambda e, ti=ti: e.scalar_tensor_tensor(out=h2[:, ti, :], in0=x1[:, ti, :],
                                                                     scalar=rstd2[:, ti:ti + 1], in1=g2b_s[:],
                                                                     op0=ALU.mult, op1=ALU.mult),
                      reads=[("x1", ti), "g2b", ("rstd2", ti)], writes=[("h2", ti)])
                S.add("dve", lambda e, ti=ti: e.tensor_scalar(out=Mb[:, ti, :], in0=G[:, ti, :], scalar1=0.0, scalar2=None,
                                                              op0=ALU.is_gt),
                      reads=[("G", ti)], writes=[("Mb", ti)])
                S.add("dve", lambda e, ti=ti: e.tensor_copy(out=Ghl[:, ti, :, 0], in_=G[:, ti, :]),
                      reads=[("G", ti)], writes=[("Ghl", ti)])
                S.add("dve", lambda e, ti=ti, i=i: e.tensor_tensor(out=Gr[i][:], in0=G[:, ti, :], in1=Ghl[:, ti, :, 0],
                                                                   op=ALU.subtract),
                      reads=[("G", ti), ("Ghl", ti)], writes=[("Gr", i)])
                S.add("dve", lambda e, ti=ti, i=i: e.tensor_copy(out=Ghl[:, ti, :, 1], in_=Gr[i][:]),
                      reads=[("Gr", i)], writes=[("Ghl", ti)])
                b = nb()
                pb = banks[b]
                S.add("pe", lambda e, ti=ti, pb=pb: e.transpose(out=pb[0:NE, 0:128], in_=G[:, ti, :], identity=identf[:]),
                      reads=[("G", ti)], writes=[("bank", b)])
                S.add("act", lambda e, i=i, pb=pb: e.activation(out=GTs[i][:], in_=pb[0:NE, 0:128], func=AF.Copy),
                      reads=[("bank", b)], writes=[("GTs", i)])
                for hf in range(2):
                    b = nb()
                    pb = banks[b]
                    S.add("pe", lambda e, i=i, hf=hf, pb=pb: e.matmul(pb[:, :], lhsT=GTs[i][:], rhs=bdn[:, hf * 512:(hf + 1) * 512],
                                                                      start=True, stop=True),
                          reads=[("GTs", i), "bdn"], writes=[("bank", b)])
                    S.add("dve", lambda e, ti=ti, hf=hf, pb=pb: e.tensor_tensor(
                        out=x1[:, ti, hf * 512:(hf + 1) * 512], in0=pb[:, :], in1=x1[:, ti, hf * 512:(hf + 1) * 512], op=ALU.add),
                        reads=[("bank", b), ("x1", ti)], writes=[("x1", ti)])
            for ti in range(NT):
                i = ti % 2
                prev = list(range(ti % 4, ti, 4))
                b = nb()
                pb = banks[b]

                def rk(e, ti=ti, prev=prev, pb=pb):
                    ins = e.matmul(pb[:, 0:NE], lhsT=lts[:], rhs=Mb[:, ti, :], start=True, stop=(not prev))
                    for tj in prev:
                        ins = e.matmul(pb[:, 0:NE], lhsT=onesb[:], rhs=Mb[:, tj, :], start=False, stop=(tj == prev[-1]))
                    return ins
                S.add("pe", rk, reads=[("Mb", tj) for tj in prev + [ti]] + ["lts", "onesb"], writes=[("bank", b)])
                S.add("dve", lambda e, ti=ti, i=i: e.tensor_scalar(out=Mf[i][:], in0=G[:, ti, :], scalar1=0.0, scalar2=None,
                                                                   op0=ALU.is_gt),
                      reads=[("G", ti)], writes=[("Mf", i)])
                S.add("dve", lambda e, ti=ti, i=i, pb=pb: e.scalar_tensor_tensor(out=slot[:, ti, :], in0=pb[:, 0:NE], scalar=1.0,
                                                                                 in1=Mf[i][:], op0=ALU.add, op1=ALU.mult),
                      reads=[("bank", b), ("Mf", i)], writes=[("slot", ti)])
                S.add("dve", lambda e, ti=ti: e.tensor_scalar(out=slot[:, ti, :], in0=slot[:, ti, :], scalar1=-1.0, scalar2=None,
                                                              op0=ALU.add),
                      reads=[("slot", ti)], writes=[("slot", ti)])
            H2 = [("h2", ti) for ti in range(NT)]
            for e_ in range(ne_run):
                sG, sU, sD = 0, 1, 2
                pi = e_ % 2
                Pe, PTe, gse = P[0], PT[pi], gsel[pi]
                for ti in range(NT):
                    S.add("dve", lambda e, ti=ti, e_=e_, Pe=Pe: e.tensor_scalar(out=Pe[:, ti, :], in0=iota_s[:],
                                                                               scalar1=slot[:, ti, e_:e_ + 1], scalar2=None,
                                                                               op0=ALU.is_equal),
                          reads=["iota", ("slot", ti)], writes=[("P", 0, ti % 4)], cost=0.2)
                for kc in range(8):
                    b = nb()
                    pb = banks[b]

                    def ga(e, kc=kc, pb=pb, Pe=Pe):
                        for g in range(4):
                            for r in range(4):
                                ins = e.matmul(pb[:, g * 128:(g + 1) * 128], lhsT=h2[:, 4 * r + g, kc * 128:(kc + 1) * 128],
                                               rhs=Pe[:, 4 * r + g, :], start=(r == 0), stop=(r == 3), skip_group_check=True)
                        return ins
                    S.add("pe", ga, reads=H2 + [("P", 0, g) for g in range(4)], writes=[("bank", b)], cost=1.6)
                    if kc % 2:
                        S.add("act", lambda e, kc=kc, pb=pb: e.activation(out=xTs[:, kc, :], in_=pb[:, :], func=AF.Copy),
                              reads=[("bank", b)], writes=[("xTs", kc)])
                    else:
                        S.add("dve", lambda e, kc=kc, pb=pb: e.tensor_copy(out=xTs[:, kc, :], in_=pb[:, :]),
                              reads=[("bank", b)], writes=[("xTs", kc)])
                for g in range(4):
                    b = nb()
                    pbb = banks[b][:].bitcast(BF16)

                    def trp(e, g=g, pbb=pbb, Pe=Pe):
                        for r in range(4):
                            ins = e.transpose(out=pbb[:, r * 128:(r + 1) * 128], in_=Pe[:, 4 * r + g, :], identity=identb[:])
                        return ins
                    S.add("pe", trp, reads=[("P", 0, g)], writes=[("bank", b)])
                    S.add("act", lambda e, g=g, pbb=pbb, PTe=PTe: e.activation(out=PTe[:, g, :], in_=pbb[:, 0:512], func=AF.Copy),
                          reads=[("bank", b)], writes=[("PT", pi, g)])
                b = nb()
                pbg = banks[b]

                def gs(e, pbg=pbg, Pe=Pe, e_=e_):
                    for g in range(4):
                        for r in range(4):
                            ins = e.matmul(pbg[:, 2 * g:2 * g + 2], lhsT=Pe[:, 4 * r + g, :], rhs=Ghl[:, 4 * r + g, e_, :],
                                           start=(r == 0), stop=(r == 3), skip_group_check=True)
                    return ins
                S.add("pe", gs, reads=[("P", 0, g) for g in range(4)] + [("Ghl", ti) for ti in range(NT)], writes=[("bank", b)])
                S.add("dve", lambda e, pbg=pbg, gse=gse: e.tensor_reduce(out=gse[:], in_=pbg[:, 0:8].rearrange("p (g two) -> p g two", two=2),
                                                                        axis=AX.X, op=ALU.add),
                      reads=[("bank", b)], writes=[("gsel", pi)])
                for fc in range(8):
                    j = 0
                    bg_, bu_ = nb(), nb()
                    pg, pu_ = banks[bg_], banks[bu_]

                    def mmg(e, fc=fc, pg=pg):
                        for k in range(8):
                            ins = e.matmul(pg[:, :], lhsT=wsl[sG][:, k, fc * 128:(fc + 1) * 128], rhs=xTs[:, k, :],
                                           start=(k == 0), stop=(k == 7))
                        return ins

                    def mmu(e, fc=fc, pu_=pu_):
                        for k in range(8):
                            ins = e.matmul(pu_[:, :], lhsT=wsl[sU][:, k, fc * 128:(fc + 1) * 128], rhs=xTs[:, k, :],
                                           start=(k == 0), stop=(k == 7))
                        return ins
                    XT = [("xTs", k) for k in range(8)]
                    S.add("pe", mmg, reads=[("ws", sG)] + XT, writes=[("bank", bg_)], cost=2.5)
                    S.add("pe", mmu, reads=[("ws", sU)] + XT, writes=[("bank", bu_)], cost=2.5)
                    S.add("dve", lambda e, fc=fc, e_=e_, pg=pg, j=j: e.tensor_scalar(
                        out=gc[j][:], in0=pg[:, :], scalar1=bgT_s[:, e_, fc:fc + 1], scalar2=7.0, op0=ALU.add, op1=ALU.min),
                        reads=[("bank", bg_), "bgT"], writes=[("gc", j)], cost=0.6)
                    S.add("act", lambda e, j=j: e.activation(out=sg[j][:], in_=gc[j][:], func=AF.Sigmoid, scale=1.702),
                          reads=[("gc", j)], writes=[("sg", j)], cost=0.6)
                    S.add("act", lambda e, fc=fc, e_=e_, pu_=pu_, j=j: e.activation(out=uc[j][:], in_=pu_[:, :], func=AF.Identity,
                                                                                    bias=buT_s[:, e_, fc:fc + 1]),
                          reads=[("bank", bu_), "buT"], writes=[("uc", j)], cost=0.7)
                    S.add("dve", lambda e, j=j: e.tensor_scalar(out=uc[j][:], in0=uc[j][:], scalar1=-6.0, scalar2=8.0,
                                                                op0=ALU.max, op1=ALU.min),
                          reads=[("uc", j)], writes=[("uc", j)], cost=0.6)
                    S.add("dve", lambda e, j=j: e.tensor_tensor(out=gc[j][:], in0=gc[j][:], in1=sg[j][:], op=ALU.mult),
                          reads=[("gc", j), ("sg", j)], writes=[("gc", j)], cost=0.6)
                    S.add("dve", lambda e, j=j, fc=fc: e.tensor_tensor(out=aT[:, fc, :], in0=gc[j][:], in1=uc[j][:], op=ALU.mult),
                          reads=[("gc", j), ("uc", j)], writes=[("aT", fc)], cost=0.6)
                if e_ + 1 < ne_run:
                    wload(3 * (e_ + 1)); wload(3 * (e_ + 1) + 1)
                AT = [("aT", k) for k in range(8)]
                for g in range(4):
                    yi = g % 2
                    for hf in range(2):
                        b = nb()
                        pb = banks[b]

                        def mmd(e, g=g, hf=hf, pb=pb):
                            for k in range(8):
                                ins = e.matmul(pb[:, :], lhsT=aT[:, k, g * 128:(g + 1) * 128],
                                               rhs=wsl[sD][:, k, hf * 512:(hf + 1) * 512], start=(k == 0), stop=(k == 7))
                            return ins
                        S.add("pe", mmd, reads=AT + [("ws", sD)], writes=[("bank", b)], cost=2.5)
                        S.add("act", lambda e, g=g, hf=hf, pb=pb, yi=yi, gse=gse: e.activation(
                            out=ysc[yi][:, hf * 512:(hf + 1) * 512], in_=pb[:, :], func=AF.Copy, scale=gse[:, g:g + 1]),
                            reads=[("bank", b), ("gsel", pi)], writes=[("ysc", yi)], cost=0.6)
                    for r in range(4):
                        ti = 4 * r + g
                        for hf in range(2):
                            b = nb()
                            pb = banks[b]
                            S.add("pe", lambda e, g=g, r=r, hf=hf, pb=pb, yi=yi, PTe=PTe: e.matmul(
                                pb[:, :], lhsT=PTe[:, g, r * 128:(r + 1) * 128], rhs=ysc[yi][:, hf * 512:(hf + 1) * 512],
                                start=True, stop=True),
                                reads=[("PT", pi, g), ("ysc", yi)], writes=[("bank", b)], cost=0.32)
                            S.add("dve", lambda e, ti=ti, hf=hf, pb=pb: e.tensor_tensor(
                                out=x1[:, ti, hf * 512:(hf + 1) * 512], in0=pb[:, :], in1=x1[:, ti, hf * 512:(hf + 1) * 512],
                                op=ALU.add),
                                reads=[("bank", b), ("x1", ti)], writes=[("x1", ti)], cost=0.6)
                if e_ + 1 < ne_run:
                    wload(3 * (e_ + 1) + 2)
            for ti in range(NT):
                S.add("act", lambda e, ti=ti: e.activation(out=h2[:, ti, :], in_=x1[:, ti, :], func=AF.Square,
                                                           accum_out=rstd2[:, ti:ti + 1]),
                      reads=[("x1", ti)], writes=[("h2", ti), ("rstd2", ti)])
                S.add("act", lambda e, ti=ti: e.activation(out=rstd2[:, ti:ti + 1], in_=rstd2[:, ti:ti + 1], func=AF.Ln,
                                                           scale=1.0 / D, bias=EPS),
                      reads=[("rstd2", ti)], writes=[("rstd2", ti)])
                S.add("act", lambda e, ti=ti: e.activation(out=rstd2[:, ti:ti + 1], in_=rstd2[:, ti:ti + 1], func=AF.Exp,
                                                           scale=-0.5),
                      reads=[("rstd2", ti)], writes=[("rstd2", ti)])
                S.add("dve", lambda e, ti=ti: e.scalar_tensor_tensor(out=x1[:, ti, :], in0=x1[:, ti, :],
                                                                     scalar=rstd2[:, ti:ti + 1], in1=gfb_s[:],
                                                                     op0=ALU.mult, op1=ALU.mult),
                      reads=[("x1", ti), ("rstd2", ti), "gfb"], writes=[("x1", ti)])
                S.add("sp", lambda e, ti=ti: e.dma_start(out=out[ti * 128:(ti + 1) * 128, :], in_=x1[:, ti, :]),
                      reads=[("x1", ti)], dma=True)
            dm = dummies()
            dpb = banks[0]
            dm["pe"] = lambda e: e.matmul(dpb[0:1, 0:1], lhsT=ones1b[0:1, 0:1], rhs=ones1b[0:1, 0:1], start=True, stop=True,
                                          skip_group_check=True)
            S.emit(dm)
    return nc


_NC = None


def _prep(inputs):
    f = lambda a: np.ascontiguousarray(np.asarray(a, dtype=np.float32))
    x = f(inputs["x"])
    rep = lambda v, n=128: f(np.broadcast_to(np.asarray(v, np.float32).reshape(1, -1), (n, np.asarray(v).size)))
    col = lambda v: f(np.asarray(v, np.float32).reshape(8, 128).T)
    common = {
        "w_in": f(inputs["w_in"][0]), "w_out": f(inputs["w_out"][0]), "pool_w": f(inputs["pool_w"][0]),
        "g1c": col(inputs["norm1_g"][0]), "g2c": col(inputs["norm2_g"][0]), "gfb": rep(inputs["normf_g"]),
        "convT": f(np.asarray(inputs["conv_w"][0], np.float32).T.reshape(8, 128, 4).transpose(1, 0, 2)),
        "igb": f(np.tile(np.asarray(inputs["ig_b"][0], np.float32), 32).reshape(128, 1)),
        "fgbn": f(np.tile(np.asarray(inputs["fg_b"][0], np.float32), 32).reshape(128, 1)),
        "hngb": rep(inputs["head_norm_g"][0]),
        "pscale": f(np.asarray(inputs["pool_scale"][0], np.float32).reshape(4, 128).T),
        "w_router": f(inputs["w_router"][0]), "brb": rep(inputs["b_router"][0]),
        "w_gate": f(inputs["w_gate"][0]), "w_up": f(inputs["w_up"][0]), "w_down": f(inputs["w_down"][0]),
        "bgT": f(np.asarray(inputs["b_gate"][0], np.float32).reshape(NE, 8, 128).transpose(2, 0, 1)),
        "buT": f(np.asarray(inputs["b_up"][0], np.float32).reshape(NE, 8, 128).transpose(2, 0, 1)),
        "b_down": f(inputs["b_down"][0]),
        "ident": np.eye(128, dtype=np.float32),
        "maskrl": np.triu(np.ones((128, 128), np.float32)),
        "g2b": rep(inputs["norm2_g"][0]),
        "iotaj": np.ascontiguousarray(np.broadcast_to(np.arange(128, dtype=np.float32)[None, :], (128, 128))),
        "ltstrict": np.triu(np.ones((128, 128), np.float32), 1),
    }
    rm = np.ones((128, 128), np.float32)
    rm[:, 0] = 0.0
    common["resetm"] = rm
    corr_even = np.ones((128, 4, 16), np.float32)
    for g, w in enumerate((2, 4, 8, 16)):
        t = np.arange(16)
        corr_even[:, g, :] = (w / np.minimum(t + 1, w)).astype(np.float32)[None, :]
    maps = []
    for c in range(8):
        b, half = c // 2, c % 2
        m = dict(common)
        m["xo"] = f(x[b, half * TOK:(half + 1) * TOK])
        m["xp"] = f(x[b, 0:TOK]) if half else np.zeros((TOK, D), np.float32)
        m["flag"] = np.full((128, 1), float(half), np.float32)
        m["corr"] = np.ones((128, 4, 16), np.float32) if half else corr_even
        maps.append(m)
    return maps


def kernel(**inputs):
    global _NC
    debug = bool(os.environ.get("KDEBUG"))
    nc = build(debug)
    maps = _prep(inputs)
    res = run_bass_kernel_spmd(nc, maps, core_ids=list(range(8)))
    outp = np.zeros((4, 4096, D), np.float32)
    for c in range(8):
        outp[c // 2, (c % 2) * TOK:(c % 2 + 1) * TOK] = res.results[c]["out"]
    return outp
```

```python
import contextlib
import math
import os
import numpy as np
import concourse.bass as bass
import concourse.mybir as mybir
from concourse.alu_op_type import AluOpType as ALU
from concourse.bass_utils import run_bass_kernel_spmd

F32 = mybir.dt.float32
BF16 = mybir.dt.bfloat16
AF = mybir.ActivationFunctionType
AX = mybir.AxisListType

D = 1024
TOK = 2048
NT = TOK // 128
NE = 32
NIN = 2568
EPS = 1e-5
LNSC = math.log(128.0 ** -0.5)


class Op:
    __slots__ = ("eng", "fn", "deps", "odeps", "signal", "sem", "val", "is_dma", "idx", "cost", "lat", "fin", "nrem", "users")


class Sched:
    ENG = ("pe", "dve", "act", "pool", "sp")

    def __init__(self, nc, semstack, tag, ndma_sems=6):
        self.nc, self.semstack, self.tag = nc, semstack, tag
        self.ops = {e: [] for e in self.ENG}
        self.last_w, self.readers = {}, {}
        self.ndma = ndma_sems
        self.dma_count = {e: 0 for e in self.ENG}
        self.dma_prev = {}
        self.n = 0
        self.stopped = False
        self.ignore_cost = False

    COST = {"pe": 0.55, "dve": 0.35, "act": 0.35, "pool": 1.0, "sp": 0.1}

    def add(self, eng, fn, reads=(), writes=(), dma=False, cost=None):
        op = Op()
        if self.stopped:
            op.deps, op.signal, op.is_dma, op.eng = [], False, dma, eng
            return op
        if self.ignore_cost:
            cost = None
        op.cost = cost if cost is not None else (0.1 if dma else self.COST[eng])
        op.lat = (cost if cost is not None else 3.0) if dma else op.cost
        op.eng, op.fn, op.is_dma, op.signal = eng, fn, dma, False
        op.idx = self.n
        self.n += 1
        deps = []
        for r in reads:
            w = self.last_w.get(r)
            if w is not None:
                deps.append(w)
            if isinstance(r, tuple) and r[0] == "bank":
                deps.extend(o for o in self.readers.get(r, ()) if o.eng != eng)
        for w_ in writes:
            w = self.last_w.get(w_)
            if w is not None:
                deps.append(w)
            deps.extend(self.readers.get(w_, ()))
        for r in reads:
            self.readers.setdefault(r, []).append(op)
        for w_ in writes:
            self.last_w[w_] = op
            self.readers[w_] = []
        if dma:
            k = self.dma_count[eng]
            self.dma_count[eng] += 1
            slot = (eng, k % self.ndma)
            op.sem = slot
            prev = self.dma_prev.get(slot)
            if prev is not None:
                deps.append(prev)
            self.dma_prev[slot] = op
            op.signal = True
        ded, oded, seen = [], [], set()
        for d in deps:
            if d is op or id(d) in seen:
                continue
            seen.add(id(d))
            oded.append(d)
            if (not d.is_dma) and d.eng == eng and eng == "pe":
                continue
            ded.append(d)
        op.deps = ded
        op.odeps = oded
        self.ops[eng].append(op)
        return op

    def reorder(self):
        import heapq
        allops = [op for e in self.ENG for op in self.ops[e]]
        for op in allops:
            op.users, op.nrem, op.fin = [], len(op.odeps), None
        for op in allops:
            for d in op.odeps:
                d.users.append(op)
        SYNC = 0.3
        fut = {e: [] for e in self.ENG}
        now = {e: [] for e in self.ENG}
        t = {e: 0.0 for e in self.ENG}
        new = {e: [] for e in self.ENG}

        def push(op):
            r = 0.0
            for d in op.odeps:
                f = d.fin + (0.0 if (d.eng == op.eng and not d.is_dma) else SYNC)
                if f > r:
                    r = f
            heapq.heappush(fut[op.eng], (r, op.idx, op))
        for op in allops:
            if op.nrem == 0:
                push(op)
        left = len(allops)
        while left:
            best = None
            for e in self.ENG:
                while fut[e] and fut[e][0][0] <= t[e]:
                    r, i, op = heapq.heappop(fut[e])
                    heapq.heappush(now[e], (i, op))
                if now[e]:
                    st = t[e]
                elif fut[e]:
                    st = fut[e][0][0]
                else:
                    continue
                if best is None or st < best[0]:
                    best = (st, e)
            st, e = best
            if now[e]:
                i, op = heapq.heappop(now[e])
            else:
                r, i, op = heapq.heappop(fut[e])
            new[e].append(op)
            t[e] = st + op.cost
            op.fin = st + op.lat
            left -= 1
            for u in op.users:
                u.nrem -= 1
                if u.nrem == 0:
                    push(u)
        self.ops = new
        self.est = max(t.values())

    def emit(self, dummies, final_waits=()):
        nc = self.nc
        if os.environ.get("KNOSCHED") is None:
            self.reorder()
        for e, fn in dummies.items():
            o = self.add(e, fn, writes=[("bank", 0)] if e == "pe" else ())
            o.signal = True
        for e in self.ENG:
            for op in self.ops[e]:
                for d in op.deps:
                    d.signal = True
        esem = {e: self.semstack.enter_context(nc.semaphore(f"s{self.tag}_{e}")) for e in self.ENG}
        dsem = {}
        for e in self.ENG:
            for i in range(min(self.ndma, self.dma_count[e])):
                dsem[(e, i)] = self.semstack.enter_context(nc.semaphore(f"d{self.tag}_{e}{i}"))
        finals = {}
        for e in self.ENG:
            c, dc = 0, {}
            for op in self.ops[e]:
                if op.is_dma:
                    dc[op.sem] = dc.get(op.sem, 0) + 16
                    op.val = dc[op.sem]
                    op.sem = dsem[op.sem]
                    finals[id(op.sem)] = (op.sem, op.val)
                elif op.signal:
                    c += 1
                    op.val = c
                    op.sem = esem[e]
                    finals[id(op.sem)] = (op.sem, op.val)
        with nc.Block() as block:
            def run(e, eng):
                waited = {}
                for op in self.ops[e]:
                    for d in op.deps:
                        key = id(d.sem)
                        if waited.get(key, 0) >= d.val:
                            continue
                        waited[key] = d.val
                        eng.wait_ge(d.sem, d.val)
                    ins = op.fn(eng)
                    if op.signal:
                        ins.then_inc(op.sem, 16 if op.is_dma else 1)
                for sem, val in finals.values():
                    if waited.get(id(sem), 0) < val:
                        eng.wait_ge(sem, val)

            @block.tensor
            def _(eng):
                run("pe", eng)

            @block.vector
            def _(eng):
                run("dve", eng)

            @block.scalar
            def _(eng):
                run("act", eng)

            @block.gpsimd
            def _(eng):
                run("pool", eng)

            @block.sync
            def _(eng):
                run("sp", eng)


def build(debug=False):
    nc = bass.Bass("TRN2", target_bir_lowering=False)

    def din(name, shape):
        return nc.dram_tensor(name, list(shape), F32, kind="ExternalInput").ap()

    xo = din("xo", [TOK, D]); xp = din("xp", [TOK, D])
    w_in = din("w_in", [D, NIN]); w_out = din("w_out", [D, D]); pool_w = din("pool_w", [4, 128, 128])
    g1c = din("g1c", [128, 8]); g2c = din("g2c", [128, 8]); gfb = din("gfb", [128, D])
    convT = din("convT", [128, 8, 4]); igb = din("igb", [128, 1]); fgbn = din("fgbn", [128, 1])
    hngb = din("hngb", [128, 512]); pscale = din("pscale", [128, 4])
    w_router = din("w_router", [D, NE]); brb = din("brb", [128, NE])
    w_gate = din("w_gate", [NE, D, D]); w_up = din("w_up", [NE, D, D]); w_down = din("w_down", [NE, D, D])
    bgT = din("bgT", [128, NE, 8]); buT = din("buT", [128, NE, 8]); b_down = din("b_down", [NE, D])
    ident = din("ident", [128, 128]); maskrl = din("maskrl", [128, 128]); corr = din("corr", [128, 4, 16])
    flag = din("flag", [128, 1]); resetm = din("resetm", [128, 128])
    g2b = din("g2b", [128, D]); iotaj = din("iotaj", [128, 128]); ltstrict = din("ltstrict", [128, 128])
    out = nc.dram_tensor("out", [TOK, D], F32, kind="ExternalOutput").ap()
    dbg = nc.dram_tensor("dbg", [TOK, D], F32, kind="ExternalOutput").ap() if debug else None
    dbgG = nc.dram_tensor("dbgG", [TOK, NE], F32, kind="ExternalOutput").ap() if debug else None
    ne_run = int(os.environ.get("KNE", NE)) if debug else NE

    with contextlib.ExitStack() as st, contextlib.ExitStack() as semst:
        def sb(stack, name, shape, dt):
            return stack.enter_context(nc.sbuf_tensor(name, list(shape), dt))

        def ps(stack, name, shape, dt):
            return stack.enter_context(nc.psum_tensor(name, list(shape), dt))

        x1 = sb(st, "x1", [128, NT, D], F32)
        wa = sb(st, "wa", [128, 3 * 8192], BF16)
        G = sb(st, "G", [128, NT, NE], F32)
        rstd2 = sb(st, "rstd2", [128, NT], F32)
        identf = sb(st, "identf", [128, 128], F32)
        identb = sb(st, "identb", [128, 128], BF16)
        gfb_s = sb(st, "gfb_s", [128, D], F32)
        g2c_s = sb(st, "g2c_s", [128, 8], F32)
        dum = sb(st, "dum", [128, 8], F32)
        banks = [ps(st, f"bank{i}", [128, 512], F32) for i in range(8)]

        def dummies():
            return {
                "dve": lambda e: e.memset(dum[:, 0:1], 0.0),
                "act": lambda e: e.activation(out=dum[:, 1:2], in_=identf[:, 0:1], func=AF.Copy),
                "pool": lambda e: e.memset(dum[:, 3:4], 0.0),
            }

        with contextlib.ExitStack() as sa:
            S = Sched(nc, semst, "A")
            bank_i = [0]
            reserved = set()

            def nb():
                while True:
                    i = bank_i[0] % 8
                    bank_i[0] += 1
                    if i not in reserved:
                        return i

            win = wa[:, 0:8 * NIN].rearrange("p (k f) -> p k f", k=8)
            wout = sb(sa, "wout", [128, 8, D], BF16)
            dconv = sb(sa, "dconv", [128, 8, 4, 128], BF16)
            convT_s = sb(sa, "convT_s", [128, 8, 4], F32)
            g1c_s = sb(sa, "g1c_s", [128, 8], F32)
            wg = sb(sa, "wg", [128, 8, 8], BF16)
            hngb_s = sb(sa, "hngb_s", [128, 512], F32)
            poolw = sb(sa, "poolw", [128, 4, 128], BF16)
            pscale_s = sb(sa, "pscale_s", [128, 4], F32)
            wr = sb(sa, "wr", [128, 8, NE], F32)
            brb_s = sb(sa, "brb_s", [128, NE], F32)
            maskb = sb(sa, "maskb", [128, 128], BF16)
            maskf = sb(sa, "maskf", [128, 128], F32)
            corr_s = sb(sa, "corr_s", [128, 4, 16], F32)
            flag_s = sb(sa, "flag_s", [128, 1], F32)
            igb_s = sb(sa, "igb_s", [128, 1], F32)
            fgbn_s = sb(sa, "fgbn_s", [128, 1], F32)
            resetm_s = sb(sa, "resetm_s", [128, 128], F32)
            ones1 = sb(sa, "ones1", [1, 128], F32)
            rstd1 = sb(sa, "rstd1", [128, 32], F32)
            ssq = sb(sa, "ssq", [128, 48], F32)
            xin = [sb(sa, "xin0", [128, D], F32)] * 2
            hs = [sb(sa, f"hs{i}", [128, D], BF16) for i in range(2)]
            hT = [sb(sa, f"hT{i}", [128, 8, 128], BF16) for i in range(2)]
            gcol = sb(sa, "gcol", [128, 8], F32)
            grow = sb(sa, "grow", [1, 12, 128], F32)
            tokT = sb(sa, "tokT", [128, 3, 128], F32)
            bcs = sb(sa, "bcs", [128, 3, 128], F32)
            ubc = [sb(sa, f"ubc{i}", [128, 4, 3 + 128], BF16) for i in range(2)]
            halo_c = sb(sa, "halo_c", [128, 8, 3], BF16)
            qkT = [sb(sa, f"qkT{i}", [128, 8, 128], BF16) for i in range(2)]
            kTok = [sb(sa, f"kTok{i}", [128, 4, 128], BF16) for i in range(2)]
            v1 = [sb(sa, f"v1{i}", [128, 4, 129], BF16) for i in range(2)]
            osig = [sb(sa, f"osig{i}", [128, 512], BF16) for i in range(2)]
            Cst = sb(sa, "Cst", [128, 4, 129], F32)
            Ctmp = sb(sa, "Ctmp", [128, 4, 129], F32)
            Cs = sb(sa, "Cs", [128, 4, 129], BF16)
            sTp = [sb(sa, f"sTp{i}", [128, 128], BF16) for i in range(2)]
            wv = [sb(sa, f"wv{i}", [128, 129], BF16) for i in range(2)]
            dmx = sb(sa, "dmx", [128, 8], F32)
            hm = sb(sa, "hm", [128, 4, 128], F32)
            bst = sb(sa, "bst", [128, 4, 6], F32)
            mv = sb(sa, "mv", [128, 4, 2], F32)
            lnr = sb(sa, "lnr", [128, 4], F32)
            ym = sb(sa, "ym", [128, 512], BF16)
            yT = [sb(sa, f"yT{i}", [128, 8, 128], BF16) for i in range(2)]
            pu = [sb(sa, f"pu{i}", [128, 16 + 128], F32) for i in range(4)]
            phalo = sb(sa, "phalo", [128, 4, 16], F32)
            pooledT = sb(sa, "pooledT", [128, 128], BF16)
            h2f = sb(sa, "h2f", [128, D], F32)
            ymf = h2f[:, 0:512]
            gm = [hm[:, 0, :], hm[:, 1, :], hm[:, 2, :], hm[:, 3, :], h2f[:, 0:128], h2f[:, 128:256]]
            h2Tf = sb(sa, "h2Tf", [128, 8, 128], F32)
            gsb = h2Tf[:, 0:2, :]
            lg = sb(sa, "lg", [128, NE], F32)
            top8 = sb(sa, "top8", [128, 8], F32)
            rt = sb(sa, "rt", [128, 4, NE], F32)
            rsm = sb(sa, "rsm", [128, 4], F32)

            klim = float(os.environ.get("KSTAGE", "99")) if debug else 99

            def ckpt(k):
                if klim <= k:
                    S.stopped = True
            def ld(eng, dst, src, wkey, rk=()):
                return S.add(eng, lambda e: e.dma_start(out=dst, in_=src), reads=rk, writes=[wkey], dma=True)

            ld("sp", identf[:], ident, "identf")
            S.add("dve", lambda e: e.tensor_copy(out=identb[:], in_=identf[:]), reads=["identf"], writes=["identb"])
            ld("sp", maskf[:], maskrl, "maskf")
            S.add("dve", lambda e: e.tensor_copy(out=maskb[:], in_=maskf[:]), reads=["maskf"], writes=["maskb"])
            for dst, src, k in ((g1c_s, g1c, "g1c"), (g2c_s, g2c, "g2c"), (gfb_s, gfb, "gfb"), (convT_s, convT, "convT"),
                                (hngb_s, hngb, "hngb"), (pscale_s, pscale, "pscale"), (brb_s, brb, "brb"),
                                (corr_s, corr, "corr"), (flag_s, flag, "flag"), (igb_s, igb, "igb"),
                                (fgbn_s, fgbn, "fgbn"), (resetm_s, resetm, "resetm")):
                ld("sp", dst[:], src, k)
            ld("sp", wr[:], w_router.rearrange("(k p) e -> p k e", p=128), "wr")
            S.add("dve", lambda e: e.tensor_scalar(out=fgbn_s[:], in0=fgbn_s[:], scalar1=-1.0, scalar2=None, op0=ALU.mult),
                  reads=["fgbn"], writes=["fgbn"])
            w_in_v = w_in.rearrange("(k p) f -> p k f", p=128)
            ld("pool", wg[:], w_in_v[:, :, 2048:2056], "wg")
            for k in range(8):
                S.add("dve", lambda e, k=k: e.tensor_scalar(out=wg[:, k, :], in0=wg[:, k, :], scalar1=g1c_s[:, k:k + 1],
                                                            scalar2=None, op0=ALU.mult),
                      reads=["g1c", "wg"], writes=["wg"], cost=0.1)
            for k in range(8):
                ld("pool", win[:, k, :], w_in_v[:, k, :], ("wa", k))
            ld("pool", wout[:], w_out.rearrange("(k p) f -> p k f", p=128), "wout")
            ld("pool", poolw[:], pool_w.rearrange("g c d -> c g d"), "poolw")
            for k in range(8):
                if k % 2:
                    S.add("dve", lambda e, k=k: e.tensor_scalar(out=win[:, k, :], in0=win[:, k, :], scalar1=g1c_s[:, k:k + 1],
                                                                scalar2=None, op0=ALU.mult),
                          reads=["g1c", ("wa", k)], writes=[("wa", k)])
                else:
                    S.add("act", lambda e, k=k: e.activation(out=win[:, k, :], in_=win[:, k, :], func=AF.Copy,
                                                             scale=g1c_s[:, k:k + 1]),
                          reads=["g1c", ("wa", k)], writes=[("wa", k)])
            for k in range(8):
                for j in range(4):
                    S.add("dve", lambda e, k=k, j=j: e.tensor_scalar(out=dconv[:, k, j, :], in0=identf[:],
                                                                       scalar1=convT_s[:, k, j:j + 1], scalar2=None,
                                                                       op0=ALU.mult),
                          reads=["identf", "convT"], writes=[("dconv", k)])
            S.add("dve", lambda e: e.memset(ones1[:], 1.0), writes=["ones1"])
            S.add("dve", lambda e: e.memset(Cst[:], 0.0), writes=[("Cst", h) for h in range(4)])
            S.add("dve", lambda e: e.memset(halo_c[:], 0.0), writes=[("halo_c", 0), ("halo_c", 1)])
            S.add("dve", lambda e: e.memset(phalo[:], 0.0), writes=[("phalo", g) for g in range(4)])
            S.add("dve", lambda e: e.memset(v1[0][:], 1.0), writes=[("v1", 0)])
            S.add("dve", lambda e: e.memset(v1[1][:], 1.0), writes=[("v1", 1)])
            WA = [("wa", k) for k in range(8)]

            ckpt(1)
            cnt = [0]

            def norm_T(ci, first):
                i = cnt[0] % 2
                cnt[0] += 1
                own = ci >= 16
                if own:
                    xt, xkey = x1[:, ci - 16, :], ("x1", ci - 16)
                else:
                    xt, xkey = xin[0][:], ("xin", 0)
                if first or not own:
                    src = (xo if own else xp)[(ci % 16) * 128:(ci % 16 + 1) * 128, :]
                    S.add("sp", lambda e: e.dma_start(out=xt, in_=src), writes=[xkey], dma=True)
                if first:
                    S.add("act", lambda e: e.activation(out=hs[i][:], in_=xt, func=AF.Square,
                                                        accum_out=ssq[:, ci:ci + 1]),
                          reads=[xkey], writes=[("hs", i), ("ssq", ci)], cost=1.0)
                    S.add("act", lambda e: e.activation(out=ssq[:, ci:ci + 1], in_=ssq[:, ci:ci + 1], func=AF.Ln,
                                                        scale=1.0 / D, bias=EPS),
                          reads=[("ssq", ci)], writes=[("ssq", ci)])
                    S.add("act", lambda e: e.activation(out=rstd1[:, ci:ci + 1], in_=ssq[:, ci:ci + 1], func=AF.Exp,
                                                        scale=-0.5),
                          reads=[("ssq", ci)], writes=[("rstd1", ci)])
                S.add("act", lambda e: e.activation(out=hs[i][:], in_=xt, func=AF.Copy, scale=rstd1[:, ci:ci + 1]),
                      reads=[xkey, ("rstd1", ci)], writes=[("hs", i)], cost=1.1)
                b = nb()
                pb = banks[b][:].bitcast(BF16)

                def tr(e):
                    for k in range(8):
                        ins = e.transpose(out=pb[:, k * 128:(k + 1) * 128], in_=hs[i][:, k * 128:(k + 1) * 128],
                                          identity=identb[:])
                    return ins
                S.add("pe", tr, reads=[("hs", i), "identb"], writes=[("bank", b)], cost=0.8)
                S.add("dve", lambda e: e.tensor_copy(out=hT[i][:].rearrange("p k t -> p (k t)"), in_=pb[:, 0:1024]),
                      reads=[("bank", b)], writes=[("hT", i)], cost=0.6)
                return i

            gb = nb()
            reserved.add(gb)
            gall = banks[gb][:, 0:256].rearrange("p (c a) -> p c a", a=8)
            for ci in range(32):
                i = norm_T(ci, True)

                def gm_(e, i=i, ci=ci):
                    for k in range(8):
                        ins = e.matmul(gall[:, ci, :], lhsT=hT[i][:, k, :], rhs=wg[:, k, :],
                                       start=(k == 0), stop=(k == 7), skip_group_check=True)
                    return ins
                S.add("pe", gm_, reads=[("hT", i), "wg"], writes=[("bank", gb)])
            ckpt(2)
            for a in range(2):
                S.add("dve", lambda e, a=a: e.tensor_copy(out=gsb[:, a, :].rearrange("p (c h) -> p c h", h=4),
                                                          in_=gall[:, :, a * 4:(a + 1) * 4]),
                      reads=[("bank", gb)], writes=[("gsb", a)])
            reserved.discard(gb)
            tb = nb()
            tps = banks[tb]

            def trg(e):
                e.transpose(out=tps[:, 0:128], in_=gsb[:, 0, :], identity=identf[:])
                return e.transpose(out=tps[:, 128:256], in_=gsb[:, 1, :], identity=identf[:])
            S.add("pe", trg, reads=[("gsb", 0), ("gsb", 1), "identf"], writes=[("bank", tb)])
            IG, BP, DD, T0, T1, T2 = gm
            S.add("act", lambda e: e.activation(out=IG[:], in_=tps[:, 0:128], func=AF.Identity, bias=igb_s[:, 0:1]),
                  reads=[("bank", tb), "igb"], writes=["IG"])
            S.add("act", lambda e: e.activation(out=T0[:], in_=tps[:, 128:256], func=AF.Exp, scale=-1.0,
                                                bias=fgbn_s[:, 0:1]),
                  reads=[("bank", tb), "fgbn"], writes=["T0"])
            S.add("act", lambda e: e.activation(out=T0[:], in_=T0[:], func=AF.Ln, bias=1.0),
                  reads=["T0"], writes=["T0"])
            S.add("dve", lambda e: e.tensor_tensor_scan(out=BP[:], data0=resetm_s[:], data1=T0[:], initial=0.0,
                                                        op0=ALU.mult, op1=ALU.add),
                  reads=["T0", "resetm"], writes=["BP"])
            S.add("dve", lambda e: e.tensor_tensor(out=DD[:], in0=IG[:], in1=BP[:], op=ALU.add),
                  reads=["IG", "BP"], writes=["DD"])
            S.add("dve", lambda e: e.tensor_reduce(out=gcol[:, 0:1], in_=DD[:], axis=AX.X, op=ALU.max),
                  reads=["DD"], writes=["gcol"])
            S.add("dve", lambda e: e.tensor_scalar(out=gcol[:, 1:2], in0=BP[:, 127:128], scalar1=-1.0, scalar2=None,
                                                   op0=ALU.mult),
                  reads=["BP", "gcol"], writes=["gcol"])
            S.add("dve", lambda e: e.tensor_tensor(out=gcol[:, 2:3], in0=gcol[:, 0:1], in1=gcol[:, 1:2], op=ALU.add),
                  reads=["gcol"], writes=["gcol"])
            rb = nb()
            rps = banks[rb]

            def trc(e):
                for q in range(3):
                    ins = e.transpose(out=rps[0:1, q * 128:(q + 1) * 128], in_=gcol[:, q:q + 1], identity=identf[:])
                return ins
            S.add("pe", trc, reads=["gcol", "identf"], writes=[("bank", rb)])
            S.add("dve", lambda e: e.tensor_copy(out=grow[:, 0:3, :].rearrange("p a c -> p (a c)"),
                                                 in_=rps[0:1, 0:384]),
                  reads=[("bank", rb)], writes=["grow"])
            R = lambda q: grow[0:1, q, :]
            for h in range(4):
                S.add("dve", lambda e, h=h: e.tensor_tensor_scan(out=grow[0:1, 3, h:64:4], data0=grow[0:1, 1, h:64:4],
                                                                 data1=grow[0:1, 2, h:64:4], initial=0.0,
                                                                 op0=ALU.add, op1=ALU.max),
                      reads=["grow"], writes=["grow"])
            S.add("dve", lambda e: e.memset(grow[0:1, 4, 0:4], 0.0), reads=["grow"], writes=["grow"])
            S.add("dve", lambda e: e.tensor_copy(out=grow[0:1, 4, 4:64], in_=grow[0:1, 3, 0:60]),
                  reads=["grow"], writes=["grow"])
            S.add("dve", lambda e: e.tensor_scalar(out=grow[0:1, 4, 64:68], in0=grow[0:1, 3, 60:64],
                                                   scalar1=flag_s[0:1, 0:1], scalar2=None, op0=ALU.mult),
                  reads=["grow", "flag"], writes=["grow"])
            for h in range(4):
                S.add("dve", lambda e, h=h: e.tensor_tensor_scan(out=grow[0:1, 3, 64 + h:128:4],
                                                                 data0=grow[0:1, 1, 64 + h:128:4],
                                                                 data1=grow[0:1, 2, 64 + h:128:4],
                                                                 initial=grow[0:1, 4, 64 + h:65 + h],
                                                                 op0=ALU.add, op1=ALU.max),
                      reads=["grow"], writes=["grow"])
            S.add("dve", lambda e: e.tensor_copy(out=grow[0:1, 4, 68:128], in_=grow[0:1, 3, 64:124]),
                  reads=["grow"], writes=["grow"])
            S.add("dve", lambda e: e.tensor_tensor(out=R(5), in0=R(4), in1=R(0), op=ALU.max), reads=["grow"], writes=["grow"])
            S.add("dve", lambda e: e.tensor_tensor(out=R(9), in0=R(1), in1=R(4), op=ALU.add), reads=["grow"], writes=["grow"])
            S.add("dve", lambda e: e.tensor_tensor(out=R(9), in0=R(9), in1=R(3), op=ALU.subtract), reads=["grow"], writes=["grow"])
            S.add("act", lambda e: e.activation(out=R(6), in_=R(9), func=AF.Exp), reads=["grow"], writes=["grow"])
            S.add("dve", lambda e: e.tensor_tensor(out=R(9), in0=R(2), in1=R(3), op=ALU.subtract), reads=["grow"], writes=["grow"])
            S.add("act", lambda e: e.activation(out=R(7), in_=R(9), func=AF.Exp), reads=["grow"], writes=["grow"])
            S.add("dve", lambda e: e.tensor_tensor(out=R(9), in0=R(4), in1=R(5), op=ALU.subtract), reads=["grow"], writes=["grow"])
            S.add("act", lambda e: e.activation(out=R(8), in_=R(9), func=AF.Exp), reads=["grow"], writes=["grow"])
            S.add("dve", lambda e: e.tensor_scalar(out=R(10), in0=R(5), scalar1=-1.0, scalar2=LNSC, op0=ALU.mult, op1=ALU.add),
                  reads=["grow"], writes=["grow"])
            S.add("dve", lambda e: e.tensor_scalar(out=R(11), in0=R(0), scalar1=-1.0, scalar2=LNSC, op0=ALU.mult, op1=ALU.add),
                  reads=["grow"], writes=["grow"])
            S.add("dve", lambda e: e.tensor_scalar(out=R(9), in0=R(5), scalar1=-1.0, scalar2=None, op0=ALU.mult),
                  reads=["grow"], writes=["grow"])
            bb = nb()
            bps = banks[bb]

            def bc(e):
                for q in range(3):
                    ins = e.matmul(bps[:, q * 128:(q + 1) * 128], lhsT=ones1[0:1, :], rhs=R(6 + q), start=True, stop=True,
                                   skip_group_check=True)
                return ins
            S.add("pe", bc, reads=["grow", "ones1"], writes=[("bank", bb)])
            S.add("dve", lambda e: e.tensor_copy(out=bcs[:].rearrange("p a c -> p (a c)"), in_=bps[:, 0:384]),
                  reads=[("bank", bb)], writes=["bcs"])
            cb = nb()
            cps = banks[cb]

            def trr(e):
                for q in range(3):
                    ins = e.matmul(cps[:, q:q + 1], lhsT=R(9 + q), rhs=ones1[0:1, 0:1], start=True, stop=True,
                                   skip_group_check=True)
                return ins
            S.add("pe", trr, reads=["grow", "ones1"], writes=[("bank", cb)])
            S.add("dve", lambda e: e.tensor_copy(out=gcol[:, 3:6], in_=cps[:, 0:3]), reads=[("bank", cb)], writes=["gcol"])
            S.add("act", lambda e: e.activation(out=T0[:], in_=DD[:], func=AF.Exp, bias=gcol[:, 4:5]),
                  reads=["DD", "gcol"], writes=["T0"])
            S.add("act", lambda e: e.activation(out=T1[:], in_=DD[:], func=AF.Exp, bias=gcol[:, 5:6]),
                  reads=["DD", "gcol"], writes=["T1"])
            S.add("act", lambda e: e.activation(out=T2[:], in_=BP[:], func=AF.Exp, bias=gcol[:, 3:4]),
                  reads=["BP", "gcol"], writes=["T2"])
            kb = nb()
            kps = banks[kb]

            def trt(e):
                for q, t in enumerate((T0, T1, T2)):
                    ins = e.transpose(out=kps[:, q * 128:(q + 1) * 128], in_=t[:], identity=identf[:])
                return ins
            S.add("pe", trt, reads=["T0", "T1", "T2", "identf"], writes=[("bank", kb)])
            S.add("dve", lambda e: e.tensor_copy(out=tokT[:].rearrange("p a c -> p (a c)"), in_=kps[:, 0:384]),
                  reads=[("bank", kb)], writes=["tokT"])

            ucnt = [0]

            def proj_conv(i, cg, p, etmp, ekey, save_only=False):
                b = nb()
                pb = banks[b]

                def mm(e):
                    for c in range(4):
                        fk = cg * 4 + c
                        for k in range(8):
                            ins = e.matmul(pb[:, c * 128:(c + 1) * 128], lhsT=win[:, k, fk * 128:(fk + 1) * 128],
                                           rhs=hT[i][:, k, :], start=(k == 0), stop=(k == 7), skip_group_check=True)
                    return ins
                S.add("pe", mm, reads=[("hT", i)] + WA, writes=[("bank", b)], cost=2.6)
                u = ucnt[0] % 2
                ucnt[0] += 1
                S.add("dve", lambda e: e.tensor_copy(out=ubc[u][:, :, 0:3], in_=halo_c[:, cg * 4:(cg + 1) * 4, :]),
                      reads=[("halo_c", cg)], writes=[("ubc", u)])
                S.add("act", lambda e: e.activation(out=ubc[u][:, :, 3:131], in_=pb[:, :].rearrange("p (c t) -> p c t", c=4),
                                                    func=AF.Copy),
                      reads=[("bank", b)], writes=[("ubc", u)], cost=0.6)
                S.add("dve", lambda e: e.tensor_copy(out=halo_c[:, cg * 4:(cg + 1) * 4, :], in_=ubc[u][:, :, 128:131]),
                      reads=[("ubc", u)], writes=[("halo_c", cg)])
                if save_only:
                    return
                b2 = nb()
                pb2 = banks[b2]

                def cv(e):
                    for c in range(4):
                        fk = cg * 4 + c
                        for j in range(4):
                            ins = e.matmul(pb2[:, c * 128:(c + 1) * 128], lhsT=dconv[:, fk, j, :], rhs=ubc[u][:, c, j:j + 128],
                                           start=(j == 0), stop=(j == 3), skip_group_check=True)
                    return ins
                S.add("pe", cv, reads=[("ubc", u)] + [("dconv", cg * 4 + c) for c in range(4)], writes=[("bank", b2)], cost=1.4)
                S.add("act", lambda e: e.activation(out=etmp, in_=pb2[:, :], func=AF.Exp, scale=-1.0),
                      reads=[("bank", b2)], writes=[ekey], cost=0.6)
                S.add("act", lambda e: e.activation(out=etmp, in_=etmp, func=AF.Ln, bias=1.0),
                      reads=[ekey], writes=[ekey], cost=0.6)
                S.add("act", lambda e: e.activation(out=etmp, in_=etmp, func=AF.Exp, scale=-1.0),
                      reads=[ekey], writes=[ekey], cost=0.6)
                S.add("dve", lambda e: e.tensor_tensor(out=qkT[p][:, cg * 4:(cg + 1) * 4, :].rearrange("p c t -> p (c t)"),
                                                       in0=pb2[:, :], in1=etmp, op=ALU.mult),
                      reads=[("bank", b2), ekey], writes=[("qkT", p, cg * 4 + c) for c in range(4)], cost=0.6)

            def proj_v(i, p):
                b = nb()
                pb = banks[b]

                def mm(e):
                    for k in range(8):
                        ins = e.matmul(pb[:, :], lhsT=hT[i][:, k, :], rhs=win[:, k, 1024:1536], start=(k == 0), stop=(k == 7))
                    return ins
                S.add("pe", mm, reads=[("hT", i)] + WA, writes=[("bank", b)], cost=2.5)
                S.add("act", lambda e: e.activation(out=v1[p][:, :, 0:128], in_=pb[:, :].rearrange("p (h d) -> p h d", h=4),
                                                    func=AF.Copy),
                      reads=[("bank", b)], writes=[("v1", p)])

            def k_tok(p):
                b = nb()
                pb = banks[b][:].bitcast(BF16)

                def tr(e):
                    for h in range(4):
                        ins = e.transpose(out=pb[:, h * 128:(h + 1) * 128], in_=qkT[p][:, 4 + h, :], identity=identb[:])
                    return ins
                S.add("pe", tr, reads=[("qkT", p, 4 + h) for h in range(4)] + ["identb"], writes=[("bank", b)])
                S.add("dve", lambda e: e.tensor_copy(out=kTok[p][:].rearrange("p h d -> p (h d)"), in_=pb[:, 0:512]),
                      reads=[("bank", b)], writes=[("kTok", p)])

            def pool_proj(i, g, dst_fn):
                b = nb()
                pb = banks[b]

                def mm(e):
                    for k in range(8):
                        ins = e.matmul(pb[:, 0:128], lhsT=win[:, k, 2056 + g * 128:2056 + (g + 1) * 128],
                                       rhs=hT[i][:, k, :], start=(k == 0), stop=(k == 7))
                    return ins
                S.add("pe", mm, reads=[("hT", i)] + WA, writes=[("bank", b)])
                return b, pb

            def state_update(ci, p):
                for h in range(4):
                    ch = ci * 4 + h
                    w_ = wv[h % 2]
                    S.add("act", lambda e, h=h, ch=ch, w_=w_: e.activation(out=w_[:], in_=v1[p][:, h, :], func=AF.Copy,
                                                                           scale=tokT[:, 1, ch:ch + 1]),
                          reads=[("v1", p), "tokT"], writes=[("wv", h % 2)])
                    b = nb()
                    pb = banks[b]
                    S.add("pe", lambda e, h=h, w_=w_, pb=pb: e.matmul(pb[:, 0:129], lhsT=kTok[p][:, h, :], rhs=w_[:],
                                                                      start=True, stop=True),
                          reads=[("kTok", p), ("wv", h % 2)], writes=[("bank", b)])
                    S.add("dve", lambda e, h=h, ch=ch: e.tensor_scalar(out=Ctmp[:, h, :], in0=Cst[:, h, :],
                                                                       scalar1=bcs[:, 0, ch:ch + 1], scalar2=None,
                                                                       op0=ALU.mult),
                          reads=[("Cst", h), "bcs"], writes=[("Ctmp", h)])
                    S.add("dve", lambda e, h=h, ch=ch, pb=pb: e.scalar_tensor_tensor(out=Cst[:, h, :], in0=pb[:, 0:129],
                                                                                     scalar=bcs[:, 1, ch:ch + 1],
                                                                                     in1=Ctmp[:, h, :], op0=ALU.mult,
                                                                                     op1=ALU.add),
                          reads=[("bank", b), ("Ctmp", h), "bcs"], writes=[("Cst", h)])

            def front_prefix(ci, p):
                i = norm_T(ci, False)
                et, ek = h2f[:, 512:1024], "h2f"
                proj_conv(i, 1, p, et, ek)
                if ci == 15:
                    proj_conv(i, 0, p, et, ek, save_only=True)
                    for g in range(4):
                        b, pb = pool_proj(i, g, None)
                        S.add("act", lambda e, g=g, pb=pb: e.activation(out=phalo[:, g, :], in_=pb[:, 112:128], func=AF.Copy),
                              reads=[("bank", b)], writes=[("phalo", g)])
                proj_v(i, p)
                k_tok(p)

            for ci in range(16):
                front_prefix(ci, ci % 2)
                if ci > 0:
                    state_update(ci - 1, (ci - 1) % 2)
            ckpt(4)

            def front_own(ti, p):
                ci = 16 + ti
                i = norm_T(ci, False)
                et, ek = xin[0][:, 0:512], ("xin", 0)
                proj_conv(i, 0, p, et, ek)
                proj_conv(i, 1, p, et, ek)
                proj_v(i, p)
                b = nb()
                pb = banks[b]

                def mm(e):
                    for k in range(8):
                        ins = e.matmul(pb[:, :], lhsT=hT[i][:, k, :], rhs=win[:, k, 1536:2048], start=(k == 0), stop=(k == 7))
                    return ins
                S.add("pe", mm, reads=[("hT", i)] + WA, writes=[("bank", b)], cost=2.5)
                S.add("act", lambda e: e.activation(out=et, in_=pb[:, :], func=AF.Exp, scale=-1.0),
                      reads=[("bank", b)], writes=[ek], cost=0.6)
                S.add("dve", lambda e: e.tensor_scalar(out=et, in0=et, scalar1=1.0, scalar2=None, op0=ALU.add),
                      reads=[ek], writes=[ek], cost=0.5)
                def rcp(e):
                    with nc.allow_low_precision("sigmoid gate is stored in bf16 (matmul-operand precision)"):
                        return e.reciprocal(out=osig[p][:], in_=et)
                S.add("dve", rcp, reads=[ek], writes=[("osig", p)], cost=0.5)
                k_tok(p)
                for g in range(4):
                    b, pbg = pool_proj(i, g, None)
                    A_ = pu[0]
                    S.add("dve", lambda e, g=g: e.tensor_copy(out=A_[:, 0:16], in_=phalo[:, g, :]),
                          reads=[("phalo", g)], writes=["puA"])
                    S.add("act", lambda e, pbg=pbg: e.activation(out=A_[:, 16:144], in_=pbg[:, 0:128], func=AF.Copy),
                          reads=[("bank", b)], writes=["puA"])
                    S.add("dve", lambda e, g=g: e.tensor_copy(out=phalo[:, g, :], in_=A_[:, 128:144]),
                          reads=["puA"], writes=[("phalo", g)])
                    src_t, src_k = A_, "puA"
                    sh, lo = 1, 1
                    for s_ in range(g + 1):
                        dst_t = pu[1 + (s_ % 3)]
                        dk = "pu%d" % (1 + (s_ % 3))
                        S.add("dve", lambda e, src_t=src_t, dst_t=dst_t, sh=sh, lo=lo: e.tensor_tensor(
                            out=dst_t[:, lo:144], in0=src_t[:, lo:144], in1=src_t[:, lo - sh:144 - sh], op=ALU.add),
                            reads=[src_k], writes=[dk])
                        src_t, src_k = dst_t, dk
                        sh *= 2
                        lo = 2 * sh - 1
                    wdw = float(2 ** (g + 1))
                    if ti == 0:
                        S.add("dve", lambda e, src_t=src_t, g=g: e.tensor_tensor(out=src_t[:, 16:32], in0=src_t[:, 16:32],
                                                                                 in1=corr_s[:, g, :], op=ALU.mult),
                              reads=[src_k, "corr"], writes=[src_k])
                    S.add("dve", lambda e, src_t=src_t, wdw=wdw: e.scalar_tensor_tensor(
                        out=pooledT[:], in0=src_t[:, 16:144], scalar=1.0 / wdw, in1=A_[:, 16:144], op0=ALU.mult,
                        op1=ALU.subtract),
                        reads=[src_k, "puA"], writes=["pooledT"])
                    b2 = nb()
                    pb2 = banks[b2]
                    S.add("pe", lambda e, g=g, pb2=pb2: e.matmul(pb2[:, 0:128], lhsT=poolw[:, g, :], rhs=pooledT[:],
                                                                 start=True, stop=True),
                          reads=["pooledT", "poolw"], writes=[("bank", b2)])
                    S.add("act", lambda e, g=g, pb2=pb2: e.activation(out=yT[p][:, 4 + g, :], in_=pb2[:, 0:128], func=AF.Copy,
                                                                      scale=pscale_s[:, g:g + 1]),
                          reads=[("bank", b2), "pscale"], writes=[("yT", p, 4 + g)])

            def back_own(ti, p):
                ci = 16 + ti
                for h in range(4):
                    ch = ci * 4 + h
                    b = nb()
                    pb = banks[b]
                    S.add("pe", lambda e, h=h, pb=pb: e.matmul(pb[:, 0:128], lhsT=qkT[p][:, 4 + h, :], rhs=qkT[p][:, h, :],
                                                               start=True, stop=True),
                          reads=[("qkT", p, 4 + h), ("qkT", p, h)], writes=[("bank", b)])
                    s_ = sTp[h % 2]
                    S.add("dve", lambda e, ch=ch, pb=pb, s_=s_: e.scalar_tensor_tensor(
                        out=s_[:], in0=pb[:, 0:128], scalar=tokT[:, 0, ch:ch + 1], in1=maskb[:], op0=ALU.mult, op1=ALU.mult),
                        reads=[("bank", b), "tokT", "maskb"], writes=[("sTp", h % 2)])
                    S.add("act", lambda e, h=h, ch=ch: e.activation(out=Cs[:, h, :], in_=Cst[:, h, :], func=AF.Copy,
                                                                    scale=bcs[:, 2, ch:ch + 1]),
                          reads=[("Cst", h), "bcs"], writes=[("Cs", h)])
                    b2 = nb()
                    pb2 = banks[b2]

                    def nd(e, h=h, pb2=pb2, s_=s_):
                        e.matmul(pb2[:, 0:129], lhsT=s_[:], rhs=v1[p][:, h, :], start=True, stop=False)
                        return e.matmul(pb2[:, 0:129], lhsT=qkT[p][:, h, :], rhs=Cs[:, h, :], start=False, stop=True)
                    S.add("pe", nd, reads=[("sTp", h % 2), ("v1", p), ("qkT", p, h), ("Cs", h)], writes=[("bank", b2)])
                    S.add("dve", lambda e, h=h, pb2=pb2: e.tensor_scalar(out=dmx[:, h:h + 1], in0=pb2[:, 128:129], scalar1=-1.0,
                                                                         scalar2=None, op0=ALU.mult),
                          reads=[("bank", b2)], writes=[("dmx", h)])
                    S.add("dve", lambda e, h=h, pb2=pb2: e.tensor_tensor(out=dmx[:, h:h + 1], in0=pb2[:, 128:129],
                                                                         in1=dmx[:, h:h + 1], op=ALU.max),
                          reads=[("bank", b2), ("dmx", h)], writes=[("dmx", h)])
                    S.add("dve", lambda e, h=h, ch=ch: e.tensor_scalar(
                        out=dmx[:, h:h + 1], in0=dmx[:, h:h + 1], scalar1=tokT[:, 2, ch:ch + 1], scalar2=None,
                        op0=ALU.max),
                        reads=[("dmx", h), "tokT"], writes=[("dmx", h)])
                    S.add("dve", lambda e, h=h: e.reciprocal(out=dmx[:, 4 + h:5 + h], in_=dmx[:, h:h + 1]),
                          reads=[("dmx", h)], writes=[("dmx", 4 + h)])
                    S.add("act", lambda e, h=h, pb2=pb2: e.activation(out=hm[:, h, :], in_=pb2[:, 0:128], func=AF.Copy,
                                                                      scale=dmx[:, 4 + h:5 + h]),
                          reads=[("bank", b2), ("dmx", 4 + h)], writes=[("hm", h)])
                    S.add("dve", lambda e, h=h: e.bn_stats(out=bst[:, h, :], in_=hm[:, h, :]),
                          reads=[("hm", h)], writes=[("bst", h)])
                    S.add("dve", lambda e, h=h: e.bn_aggr(out=mv[:, h, :], in_=bst[:, h, :]),
                          reads=[("bst", h)], writes=[("mv", h)])
                state_update(ci, p)
                S.add("act", lambda e: e.activation(out=lnr[:], in_=mv[:, :, 1], func=AF.Ln, bias=EPS),
                      reads=[("mv", h) for h in range(4)], writes=["lnr"])
                S.add("act", lambda e: e.activation(out=lnr[:], in_=lnr[:], func=AF.Exp, scale=-0.5),
                      reads=["lnr"], writes=["lnr"])
                for h in range(4):
                    S.add("dve", lambda e, h=h: e.tensor_scalar(out=h2f[:, h * 128:(h + 1) * 128], in0=hm[:, h, :],
                                                                scalar1=mv[:, h, 0:1], scalar2=lnr[:, h:h + 1],
                                                                op0=ALU.subtract, op1=ALU.mult),
                          reads=[("hm", h), ("mv", h), "lnr"], writes=["h2f"])
                S.add("dve", lambda e: e.tensor_tensor(out=ymf, in0=ymf, in1=hngb_s[:], op=ALU.mult),
                      reads=["h2f", "hngb"], writes=["h2f"])
                S.add("dve", lambda e: e.tensor_tensor(out=ym[:], in0=ymf, in1=osig[p][:], op=ALU.mult),
                      reads=["h2f", ("osig", p)], writes=["ym"])
                b = nb()
                pbb = banks[b][:].bitcast(BF16)

                def tr(e, pbb=pbb):
                    for h in range(4):
                        ins = e.transpose(out=pbb[:, h * 128:(h + 1) * 128], in_=ym[:, h * 128:(h + 1) * 128],
                                          identity=identb[:])
                    return ins
                S.add("pe", tr, reads=["ym", "identb"], writes=[("bank", b)])
                S.add("act", lambda e, pbb=pbb: e.activation(out=yT[p][:, 0:4, :].rearrange("p h t -> p (h t)"), in_=pbb[:, 0:512],
                                                             func=AF.Copy),
                      reads=[("bank", b)], writes=[("yT", p, h) for h in range(4)])
                for hf in range(2):
                    b = nb()
                    pb = banks[b]

                    def mm(e, hf=hf, pb=pb):
                        for k in range(8):
                            ins = e.matmul(pb[:, :], lhsT=yT[p][:, k, :], rhs=wout[:, k, hf * 512:(hf + 1) * 512],
                                           start=(k == 0), stop=(k == 7))
                        return ins
                    S.add("pe", mm, reads=[("yT", p, k) for k in range(8)] + ["wout"], writes=[("bank", b)], cost=2.5)
                    S.add("dve", lambda e, hf=hf, pb=pb: e.tensor_tensor(
                        out=x1[:, ti, hf * 512:(hf + 1) * 512], in0=pb[:, :], in1=x1[:, ti, hf * 512:(hf + 1) * 512], op=ALU.add),
                        reads=[("bank", b), ("x1", ti)], writes=[("x1", ti)])
                S.add("act", lambda e: e.activation(out=h2f[:], in_=x1[:, ti, :], func=AF.Square,
                                                    accum_out=ssq[:, 32 + ti:33 + ti]),
                      reads=[("x1", ti)], writes=["h2f", ("ssq", 32 + ti)])
                S.add("act", lambda e: e.activation(out=ssq[:, 32 + ti:33 + ti], in_=ssq[:, 32 + ti:33 + ti],
                                                    func=AF.Ln, scale=1.0 / D, bias=EPS),
                      reads=[("ssq", 32 + ti)], writes=[("ssq", 32 + ti)])
                S.add("act", lambda e: e.activation(out=rstd2[:, ti:ti + 1], in_=ssq[:, 32 + ti:33 + ti],
                                                    func=AF.Exp, scale=-0.5),
                      reads=[("ssq", 32 + ti)], writes=[("rstd2", ti)])
                S.add("act", lambda e: e.activation(out=h2f[:], in_=x1[:, ti, :], func=AF.Copy, scale=rstd2[:, ti:ti + 1]),
                      reads=[("x1", ti), ("rstd2", ti)], writes=["h2f"])
                for hf in range(2):
                    b = nb()
                    pb = banks[b]

                    def tr(e, hf=hf, pb=pb):
                        for j in range(4):
                            k = hf * 4 + j
                            ins = e.transpose(out=pb[:, j * 128:(j + 1) * 128], in_=h2f[:, k * 128:(k + 1) * 128],
                                              identity=identf[:])
                        return ins
                    S.add("pe", tr, reads=["h2f", "identf"], writes=[("bank", b)])
                    for j in range(4):
                        k = hf * 4 + j
                        S.add("act" if hf else "dve",
                              (lambda e, k=k, j=j, pb=pb: e.activation(out=h2Tf[:, k, :], in_=pb[:, j * 128:(j + 1) * 128],
                                                                       func=AF.Copy, scale=g2c_s[:, k:k + 1])) if hf else
                              (lambda e, k=k, j=j, pb=pb: e.tensor_scalar(out=h2Tf[:, k, :], in0=pb[:, j * 128:(j + 1) * 128],
                                                                          scalar1=g2c_s[:, k:k + 1], scalar2=None,
                                                                          op0=ALU.mult)),
                              reads=[("bank", b), "g2c"], writes=[("h2Tf", k)])
                b = nb()
                pb = banks[b]

                def rmm(e, pb=pb):
                    for k in range(8):
                        ins = e.matmul(pb[:, 0:NE], lhsT=h2Tf[:, k, :], rhs=wr[:, k, :], start=(k == 0), stop=(k == 7))
                    return ins
                S.add("pe", rmm, reads=[("h2Tf", k) for k in range(8)] + ["wr"], writes=[("bank", b)])
                S.add("dve", lambda e, pb=pb: e.tensor_tensor(out=lg[:], in0=pb[:, 0:NE], in1=brb_s[:], op=ALU.add),
                      reads=[("bank", b), "brb"], writes=["lg"])
                S.add("dve", lambda e: e.max(out=top8[:], in_=lg[:]), reads=["lg"], writes=["top8"])
                S.add("dve", lambda e: e.tensor_scalar(out=rt[:, 0, :], in0=lg[:], scalar1=top8[:, 3:4], scalar2=None,
                                                       op0=ALU.is_ge),
                      reads=["lg", "top8"], writes=["rt0"])
                S.add("dve", lambda e: e.tensor_scalar(out=rsm[:, 0:1], in0=top8[:, 0:1], scalar1=-1.0, scalar2=None,
                                                       op0=ALU.mult),
                      reads=["top8"], writes=["rsm0"])
                S.add("act", lambda e: e.activation(out=rt[:, 1, :], in_=lg[:], func=AF.Exp, bias=rsm[:, 0:1]),
                      reads=["lg", "rsm0"], writes=["rt1"])
                S.add("dve", lambda e: e.tensor_tensor(out=rt[:, 2, :], in0=rt[:, 1, :], in1=rt[:, 0, :], op=ALU.mult),
                      reads=["rt0", "rt1"], writes=["rt2"])
                S.add("dve", lambda e: e.tensor_reduce(out=rsm[:, 1:2], in_=rt[:, 2, :], axis=AX.X, op=ALU.add),
                      reads=["rt2"], writes=["rsm1"])
                S.add("dve", lambda e: e.reciprocal(out=rsm[:, 2:3], in_=rsm[:, 1:2]), reads=["rsm1"], writes=["rsm2"])
                S.add("dve", lambda e: e.tensor_scalar(out=G[:, ti, :], in0=rt[:, 2, :], scalar1=rsm[:, 2:3],
                                                       scalar2=None, op0=ALU.mult),
                      reads=["rt2", "rsm2"], writes=[("G", ti)])

            front_own(0, 0)
            state_update(15, 1)
            for h in range(4):
                S.add("dve", lambda e, h=h: e.tensor_scalar(out=Cst[:, h, :], in0=Cst[:, h, :], scalar1=flag_s[:, 0:1],
                                                            scalar2=None, op0=ALU.mult),
                      reads=[("Cst", h), "flag"], writes=[("Cst", h)])
            for ti in range(NT):
                if ti + 1 < NT:
                    front_own(ti + 1, (ti + 1) % 2)
                back_own(ti, ti % 2)
            S.stopped = False
            if debug:
                for ti in range(NT):
                    S.add("sp", lambda e, ti=ti: e.dma_start(out=dbg[ti * 128:(ti + 1) * 128, :], in_=x1[:, ti, :]),
                          reads=[("x1", ti)], dma=True)
                    S.add("sp", lambda e, ti=ti: e.dma_start(out=dbgG[ti * 128:(ti + 1) * 128, :], in_=G[:, ti, :]),
                          reads=[("G", ti)], dma=True)
            dm = dummies()
            dpb = banks[0]
            dm["pe"] = lambda e: e.matmul(dpb[0:1, 0:1], lhsT=ones1[0:1, 0:1], rhs=ones1[0:1, 0:1], start=True, stop=True,
                                          skip_group_check=True)
            S.emit(dm)

        with contextlib.ExitStack() as sbk:
            if debug and os.environ.get("KSKIPB"):
                return nc
            S = Sched(nc, semst, "B")
            S.ignore_cost = True
            bank_i = [0]

            def nb():
                i = bank_i[0] % 8
                bank_i[0] += 1
                return i
            h2 = sb(sbk, "h2", [128, NT, D], BF16)
            g2b_s = sb(sbk, "g2b_s", [128, D], F32)
            bgT_s = sb(sbk, "bgT_s", [128, NE, 8], F32)
            buT_s = sb(sbk, "buT_s", [128, NE, 8], F32)
            bdn = sb(sbk, "bdn", [NE, D], F32)
            GTs = [sb(sbk, f"GTs{i}", [NE, 128], F32) for i in range(2)]
            slot = sb(sbk, "slot", [128, NT, NE], F32)
            Ghl = sb(sbk, "Ghl", [128, NT, NE, 2], BF16)
            Mb = sb(sbk, "Mb", [128, NT, NE], BF16)
            Mf = [sb(sbk, f"Mf{i}", [128, NE], F32) for i in range(2)]
            Gr = [sb(sbk, f"Gr{i}", [128, NE], F32) for i in range(2)]
            iota_s = sb(sbk, "iota_s", [128, 128], F32)
            ltf = sb(sbk, "ltf", [128, 128], F32)
            lts = sb(sbk, "lts", [128, 128], BF16)
            onesb = sb(sbk, "onesb", [128, 128], BF16)
            P = [sb(sbk, "P0", [128, NT, 128], BF16)] * 2
            PT = [sb(sbk, f"PT{i}", [128, 4, 512], BF16) for i in range(2)]
            gsel = [sb(sbk, f"gsel{i}", [128, 4], F32) for i in range(2)]
            xTs = sb(sbk, "xTs", [128, 8, 512], BF16)
            aT = sb(sbk, "aT", [128, 8, 512], BF16)
            ysc = [sb(sbk, f"ysc{i}", [128, D], BF16) for i in range(2)]
            gc = [sb(sbk, "gc0", [128, 512], F32)] * 2
            sg = [sb(sbk, "sg0", [128, 512], F32)] * 2
            uc = [sb(sbk, "uc0", [128, 512], F32)] * 2
            ones1b = sb(sbk, "ones1b", [1, 8], F32)
            for dst, src, k in ((bgT_s, bgT, "bgT"), (buT_s, buT, "buT"), (bdn, b_down, "bdn"), (g2b_s, g2b, "g2b"),
                                (iota_s, iotaj, "iota"), (ltf, ltstrict, "ltf")):
                S.add("sp", lambda e, dst=dst, src=src: e.dma_start(out=dst[:], in_=src), writes=[k], dma=True)
            S.add("dve", lambda e: e.memset(ones1b[:], 1.0), writes=["ones1b"])
            S.add("dve", lambda e: e.memset(onesb[:], 1.0), writes=["onesb"])
            S.add("dve", lambda e: e.tensor_copy(out=lts[:], in_=ltf[:]), reads=["ltf"], writes=["lts"])
            S.add("dve", lambda e: e.tensor_scalar(out=buT_s[:], in0=buT_s[:], scalar1=1.0, scalar2=None, op0=ALU.add),
                  reads=["buT"], writes=["buT"])
            wsl = [wa[:, s * 8192:(s + 1) * 8192].rearrange("p (k f) -> p k f", k=8) for s in range(3)]
            wsrc = []
            for e_ in range(NE):
                wsrc += [w_gate[e_], w_up[e_], w_down[e_]]

            def wload(mi):
                s = mi % 3
                S.add("pool", lambda e, mi=mi, s=s: e.dma_start(out=wsl[s], in_=wsrc[mi].rearrange("(k p) f -> p k f", p=128)),
                      writes=[("ws", s)], dma=True, cost=14.0)
            if ne_run > 0:
                wload(0); wload(1); wload(2)
            for ti in range(NT):
                i = ti % 2
                S.add("dve", lambda e, ti=ti: e.scalar_tensor_tensor(out=h2[:, ti, :], in0=x1[:, ti, :],
                                                                     scalar=rstd2[:, ti:ti + 1], in1=g2b_s[:],
                                                                     op0=ALU.mult, op1=ALU.mult),
                      reads=[("x1", ti), "g2b", ("rstd2", ti)], writes=[("h2", ti)])
                S.add("dve", lambda e, ti=ti: e.tensor_scalar(out=Mb[:, ti, :], in0=G[:, ti, :], scalar1=0.0, scalar2=None,
                                                              op0=ALU.is_gt),
                      reads=[("G", ti)], writes=[("Mb", ti)])
                S.add("dve", lambda e, ti=ti: e.tensor_copy(out=Ghl[:, ti, :, 0], in_=G[:, ti, :]),
                      reads=[("G", ti)], writes=[("Ghl", ti)])
                S.add("dve", lambda e, ti=ti, i=i: e.tensor_tensor(out=Gr[i][:], in0=G[:, ti, :], in1=Ghl[:, ti, :, 0],
                                                                   op=ALU.subtract),
                      reads=[("G", ti), ("Ghl", ti)], writes=[("Gr", i)])
                S.add("dve", lambda e, ti=ti, i=i: e.tensor_copy(out=Ghl[:, ti, :, 1], in_=Gr[i][:]),
                      reads=[("Gr", i)], writes=[("Ghl", ti)])
                b = nb()
                pb = banks[b]
                S.add("pe", lambda e, ti=ti, pb=pb: e.transpose(out=pb[0:NE, 0:128], in_=G[:, ti, :], identity=identf[:]),
                      reads=[("G", ti)], writes=[("bank", b)])
                S.add("act", lambda e, i=i, pb=pb: e.activation(out=GTs[i][:], in_=pb[0:NE, 0:128], func=AF.Copy),
                      reads=[("bank", b)], writes=[("GTs", i)])
                for hf in range(2):
                    b = nb()
                    pb = banks[b]
                    S.add("pe", lambda e, i=i, hf=hf, pb=pb: e.matmul(pb[:, :], lhsT=GTs[i][:], rhs=bdn[:, hf * 512:(hf + 1) * 512],
                                                                      start=True, stop=True),
                          reads=[("GTs", i), "bdn"], writes=[("bank", b)])
                    S.add("dve", lambda e, ti=ti, hf=hf, pb=pb: e.tensor_tensor(
                        out=x1[:, ti, hf * 512:(hf + 1) * 512], in0=pb[:, :], in1=x1[:, ti, hf * 512:(hf + 1) * 512], op=ALU.add),
                        reads=[("bank", b), ("x1", ti)], writes=[("x1", ti)])
            for ti in range(NT):
                i = ti % 2
                prev = list(range(ti % 4, ti, 4))
                b = nb()
                pb = banks[b]

                def rk(e, ti=ti, prev=prev, pb=pb):
                    ins = e.matmul(pb[:, 0:NE], lhsT=lts[:], rhs=Mb[:, ti, :], start=True, stop=(not prev))
                    for tj in prev:
                        ins = e.matmul(pb[:, 0:NE], lhsT=onesb[:], rhs=Mb[:, tj, :], start=False, stop=(tj == prev[-1]))
                    return ins
                S.add("pe", rk, reads=[("Mb", tj) for tj in prev + [ti]] + ["lts", "onesb"], writes=[("bank", b)])
                S.add("dve", lambda e, ti=ti, i=i: e.tensor_scalar(out=Mf[i][:], in0=G[:, ti, :], scalar1=0.0, scalar2=None,
                                                                   op0=ALU.is_gt),
                      reads=[("G", ti)], writes=[("Mf", i)])
                S.add("dve", lambda e, ti=ti, i=i, pb=pb: e.scalar_tensor_tensor(out=slot[:, ti, :], in0=pb[:, 0:NE], scalar=1.0,
                                                                                 in1=Mf[i][:], op0=ALU.add, op1=ALU.mult),
                      reads=[("bank", b), ("Mf", i)], writes=[("slot", ti)])
                S.add("dve", lambda e, ti=ti: e.tensor_scalar(out=slot[:, ti, :], in0=slot[:, ti, :], scalar1=-1.0, scalar2=None,
                                                              op0=ALU.add),
                      reads=[("slot", ti)], writes=[("slot", ti)])
            H2 = [("h2", ti) for ti in range(NT)]
            for e_ in range(ne_run):
                sG, sU, sD = 0, 1, 2
                pi = e_ % 2
                Pe, PTe, gse = P[0], PT[pi], gsel[pi]
                for ti in range(NT):
                    S.add("dve", lambda e, ti=ti, e_=e_, Pe=Pe: e.tensor_scalar(out=Pe[:, ti, :], in0=iota_s[:],
                                                                               scalar1=slot[:, ti, e_:e_ + 1], scalar2=None,
                                                                               op0=ALU.is_equal),
                          reads=["iota", ("slot", ti)], writes=[("P", 0, ti % 4)], cost=0.2)
                for kc in range(8):
                    b = nb()
                    pb = banks[b]

                    def ga(e, kc=kc, pb=pb, Pe=Pe):
                        for g in range(4):
                            for r in range(4):
                                ins = e.matmul(pb[:, g * 128:(g + 1) * 128], lhsT=h2[:, 4 * r + g, kc * 128:(kc + 1) * 128],
                                               rhs=Pe[:, 4 * r + g, :], start=(r == 0), stop=(r == 3), skip_group_check=True)
                        return ins
                    S.add("pe", ga, reads=H2 + [("P", 0, g) for g in range(4)], writes=[("bank", b)], cost=1.6)
                    if kc % 2:
                        S.add("act", lambda e, kc=kc, pb=pb: e.activation(out=xTs[:, kc, :], in_=pb[:, :], func=AF.Copy),
                              reads=[("bank", b)], writes=[("xTs", kc)])
                    else:
                        S.add("dve", lambda e, kc=kc, pb=pb: e.tensor_copy(out=xTs[:, kc, :], in_=pb[:, :]),
                              reads=[("bank", b)], writes=[("xTs", kc)])
                for g in range(4):
                    b = nb()
                    pbb = banks[b][:].bitcast(BF16)

                    def trp(e, g=g, pbb=pbb, Pe=Pe):
                        for r in range(4):
                            ins = e.transpose(out=pbb[:, r * 128:(r + 1) * 128], in_=Pe[:, 4 * r + g, :], identity=identb[:])
                        return ins
                    S.add("pe", trp, reads=[("P", 0, g)], writes=[("bank", b)])
                    S.add("act", lambda e, g=g, pbb=pbb, PTe=PTe: e.activation(out=PTe[:, g, :], in_=pbb[:, 0:512], func=AF.Copy),
                          reads=[("bank", b)], writes=[("PT", pi, g)])
                b = nb()
                pbg = banks[b]

                def gs(e, pbg=pbg, Pe=Pe, e_=e_):
                    for g in range(4):
                        for r in range(4):
                            ins = e.matmul(pbg[:, 2 * g:2 * g + 2], lhsT=Pe[:, 4 * r + g, :], rhs=Ghl[:, 4 * r + g, e_, :],
                                           start=(r == 0), stop=(r == 3), skip_group_check=True)
                    return ins
                S.add("pe", gs, reads=[("P", 0, g) for g in range(4)] + [("Ghl", ti) for ti in range(NT)], writes=[("bank", b)])
                S.add("dve", lambda e, pbg=pbg, gse=gse: e.tensor_reduce(out=gse[:], in_=pbg[:, 0:8].rearrange("p (g two) -> p g two", two=2),
                                                                        axis=AX.X, op=ALU.add),
                      reads=[("bank", b)], writes=[("gsel", pi)])
                for fc in range(8):
                    j = 0
                    bg_, bu_ = nb(), nb()
                    pg, pu_ = banks[bg_], banks[bu_]

                    def mmg(e, fc=fc, pg=pg):
                        for k in range(8):
                            ins = e.matmul(pg[:, :], lhsT=wsl[sG][:, k, fc * 128:(fc + 1) * 128], rhs=xTs[:, k, :],
                                           start=(k == 0), stop=(k == 7))
                        return ins

                    def mmu(e, fc=fc, pu_=pu_):
                        for k in range(8):
                            ins = e.matmul(pu_[:, :], lhsT=wsl[sU][:, k, fc * 128:(fc + 1) * 128], rhs=xTs[:, k, :],
                                           start=(k == 0), stop=(k == 7))
                        return ins
                    XT = [("xTs", k) for k in range(8)]
                    S.add("pe", mmg, reads=[("ws", sG)] + XT, writes=[("bank", bg_)], cost=2.5)
                    S.add("pe", mmu, reads=[("ws", sU)] + XT, writes=[("bank", bu_)], cost=2.5)
                    S.add("dve", lambda e, fc=fc, e_=e_, pg=pg, j=j: e.tensor_scalar(
                        out=gc[j][:], in0=pg[:, :], scalar1=bgT_s[:, e_, fc:fc + 1], scalar2=7.0, op0=ALU.add, op1=ALU.min),
                        reads=[("bank", bg_), "bgT"], writes=[("gc", j)], cost=0.6)
                    S.add("act", lambda e, j=j: e.activation(out=sg[j][:], in_=gc[j][:], func=AF.Sigmoid, scale=1.702),
                          reads=[("gc", j)], writes=[("sg", j)], cost=0.6)
                    S.add("act", lambda e, fc=fc, e_=e_, pu_=pu_, j=j: e.activation(out=uc[j][:], in_=pu_[:, :], func=AF.Identity,
                                                                                    bias=buT_s[:, e_, fc:fc + 1]),
                          reads=[("bank", bu_), "buT"], writes=[("uc", j)], cost=0.7)
                    S.add("dve", lambda e, j=j: e.tensor_scalar(out=uc[j][:], in0=uc[j][:], scalar1=-6.0, scalar2=8.0,
                                                                op0=ALU.max, op1=ALU.min),
                          reads=[("uc", j)], writes=[("uc", j)], cost=0.6)
                    S.add("dve", lambda e, j=j: e.tensor_tensor(out=gc[j][:], in0=gc[j][:], in1=sg[j][:], op=ALU.mult),
                          reads=[("gc", j), ("sg", j)], writes=[("gc", j)], cost=0.6)
                    S.add("dve", lambda e, j=j, fc=fc: e.tensor_tensor(out=aT[:, fc, :], in0=gc[j][:], in1=uc[j][:], op=ALU.mult),
                          reads=[("gc", j), ("uc", j)], writes=[("aT", fc)], cost=0.6)
                if e_ + 1 < ne_run:
                    wload(3 * (e_ + 1)); wload(3 * (e_ + 1) + 1)
                AT = [("aT", k) for k in range(8)]
                for g in range(4):
                    yi = g % 2
                    for hf in range(2):
                        b = nb()
                        pb = banks[b]

                        def mmd(e, g=g, hf=hf, pb=pb):
                            for k in range(8):
                                ins = e.matmul(pb[:, :], lhsT=aT[:, k, g * 128:(g + 1) * 128],
                                               rhs=wsl[sD][:, k, hf * 512:(hf + 1) * 512], start=(k == 0), stop=(k == 7))
                            return ins
                        S.add("pe", mmd, reads=AT + [("ws", sD)], writes=[("bank", b)], cost=2.5)
                        S.add("act", lambda e, g=g, hf=hf, pb=pb, yi=yi, gse=gse: e.activation(
                            out=ysc[yi][:, hf * 512:(hf + 1) * 512], in_=pb[:, :], func=AF.Copy, scale=gse[:, g:g + 1]),
                            reads=[("bank", b), ("gsel", pi)], writes=[("ysc", yi)], cost=0.6)
                    for r in range(4):
                        ti = 4 * r + g
                        for hf in range(2):
                            b = nb()
                            pb = banks[b]
                            S.add("pe", lambda e, g=g, r=r, hf=hf, pb=pb, yi=yi, PTe=PTe: e.matmul(
                                pb[:, :], lhsT=PTe[:, g, r * 128:(r + 1) * 128], rhs=ysc[yi][:, hf * 512:(hf + 1) * 512],
                                start=True, stop=True),
                                reads=[("PT", pi, g), ("ysc", yi)], writes=[("bank", b)], cost=0.32)
                            S.add("dve", lambda e, ti=ti, hf=hf, pb=pb: e.tensor_tensor(
                                out=x1[:, ti, hf * 512:(hf + 1) * 512], in0=pb[:, :], in1=x1[:, ti, hf * 512:(hf + 1) * 512],
                                op=ALU.add),
                                reads=[("bank", b), ("x1", ti)], writes=[("x1", ti)], cost=0.6)
                if e_ + 1 < ne_run:
                    wload(3 * (e_ + 1) + 2)
            for ti in range(NT):
                S.add("act", lambda e, ti=ti: e.activation(out=h2[:, ti, :], in_=x1[:, ti, :], func=AF.Square,
                                                           accum_out=rstd2[:, ti:ti + 1]),
                      reads=[("x1", ti)], writes=[("h2", ti), ("rstd2", ti)])
                S.add("act", lambda e, ti=ti: e.activation(out=rstd2[:, ti:ti + 1], in_=rstd2[:, ti:ti + 1], func=AF.Ln,
                                                           scale=1.0 / D, bias=EPS),
                      reads=[("rstd2", ti)], writes=[("rstd2", ti)])
                S.add("act", lambda e, ti=ti: e.activation(out=rstd2[:, ti:ti + 1], in_=rstd2[:, ti:ti + 1], func=AF.Exp,
                                                           scale=-0.5),
                      reads=[("rstd2", ti)], writes=[("rstd2", ti)])
                S.add("dve", lambda e, ti=ti: e.scalar_tensor_tensor(out=x1[:, ti, :], in0=x1[:, ti, :],
                                                                     scalar=rstd2[:, ti:ti + 1], in1=gfb_s[:],
                                                                     op0=ALU.mult, op1=ALU.mult),
                      reads=[("x1", ti), ("rstd2", ti), "gfb"], writes=[("x1", ti)])
                S.add("sp", lambda e, ti=ti: e.dma_start(out=out[ti * 128:(ti + 1) * 128, :], in_=x1[:, ti, :]),
                      reads=[("x1", ti)], dma=True)
            dm = dummies()
            dpb = banks[0]
            dm["pe"] = lambda e: e.matmul(dpb[0:1, 0:1], lhsT=ones1b[0:1, 0:1], rhs=ones1b[0:1, 0:1], start=True, stop=True,
                                          skip_group_check=True)
            S.emit(dm)
    return nc


_NC = None


def _prep(inputs):
    f = lambda a: np.ascontiguousarray(np.asarray(a, dtype=np.float32))
    x = f(inputs["x"])
    rep = lambda v, n=128: f(np.broadcast_to(np.asarray(v, np.float32).reshape(1, -1), (n, np.asarray(v).size)))
    col = lambda v: f(np.asarray(v, np.float32).reshape(8, 128).T)
    common = {
        "w_in": f(inputs["w_in"][0]), "w_out": f(inputs["w_out"][0]), "pool_w": f(inputs["pool_w"][0]),
        "g1c": col(inputs["norm1_g"][0]), "g2c": col(inputs["norm2_g"][0]), "gfb": rep(inputs["normf_g"]),
        "convT": f(np.asarray(inputs["conv_w"][0], np.float32).T.reshape(8, 128, 4).transpose(1, 0, 2)),
        "igb": f(np.tile(np.asarray(inputs["ig_b"][0], np.float32), 32).reshape(128, 1)),
        "fgbn": f(np.tile(np.asarray(inputs["fg_b"][0], np.float32), 32).reshape(128, 1)),
        "hngb": rep(inputs["head_norm_g"][0]),
        "pscale": f(np.asarray(inputs["pool_scale"][0], np.float32).reshape(4, 128).T),
        "w_router": f(inputs["w_router"][0]), "brb": rep(inputs["b_router"][0]),
        "w_gate": f(inputs["w_gate"][0]), "w_up": f(inputs["w_up"][0]), "w_down": f(inputs["w_down"][0]),
        "bgT": f(np.asarray(inputs["b_gate"][0], np.float32).reshape(NE, 8, 128).transpose(2, 0, 1)),
        "buT": f(np.asarray(inputs["b_up"][0], np.float32).reshape(NE, 8, 128).transpose(2, 0, 1)),
        "b_down": f(inputs["b_down"][0]),
        "ident": np.eye(128, dtype=np.float32),
        "maskrl": np.triu(np.ones((128, 128), np.float32)),
        "g2b": rep(inputs["norm2_g"][0]),
        "iotaj": np.ascontiguousarray(np.broadcast_to(np.arange(128, dtype=np.float32)[None, :], (128, 128))),
        "ltstrict": np.triu(np.ones((128, 128), np.float32), 1),
    }
    rm = np.ones((128, 128), np.float32)
    rm[:, 0] = 0.0
    common["resetm"] = rm
    corr_even = np.ones((128, 4, 16), np.float32)
    for g, w in enumerate((2, 4, 8, 16)):
        t = np.arange(16)
        corr_even[:, g, :] = (w / np.minimum(t + 1, w)).astype(np.float32)[None, :]
    maps = []
    for c in range(8):
        b, half = c // 2, c % 2
        m = dict(common)
        m["xo"] = f(x[b, half * TOK:(half + 1) * TOK])
        m["xp"] = f(x[b, 0:TOK]) if half else np.zeros((TOK, D), np.float32)
        m["flag"] = np.full((128, 1), float(half), np.float32)
        m["corr"] = np.ones((128, 4, 16), np.float32) if half else corr_even
        maps.append(m)
    return maps


def kernel(**inputs):
    global _NC
    debug = bool(os.environ.get("KDEBUG"))
    nc = build(debug)
    maps = _prep(inputs)
    res = run_bass_kernel_spmd(nc, maps, core_ids=list(range(8)))
    outp = np.zeros((4, 4096, D), np.float32)
    for c in range(8):
        outp[c // 2, (c % 2) * TOK:(c % 2 + 1) * TOK] = res.results[c]["out"]
    return outp
```

```python
import contextlib
import math
import os
import numpy as np
import concourse.bass as bass
import concourse.mybir as mybir
from concourse.alu_op_type import AluOpType as ALU
from concourse.bass_utils import run_bass_kernel_spmd

F32 = mybir.dt.float32
BF16 = mybir.dt.bfloat16
AF = mybir.ActivationFunctionType
AX = mybir.AxisListType

D = 1024
TOK = 2048
NT = TOK // 128
NE = 32
NIN = 2568
EPS = 1e-5
LNSC = math.log(128.0 ** -0.5)


class Op:
    __slots__ = ("eng", "fn", "deps", "odeps", "signal", "sem", "val", "is_dma", "idx", "cost", "lat", "fin", "nrem", "users")


class Sched:
    ENG = ("pe", "dve", "act", "pool", "sp")

    def __init__(self, nc, semstack, tag, ndma_sems=6):
        self.nc, self.semstack, self.tag = nc, semstack, tag
        self.ops = {e: [] for e in self.ENG}
        self.last_w, self.readers = {}, {}
        self.ndma = ndma_sems
        self.dma_count = {e: 0 for e in self.ENG}
        self.dma_prev = {}
        self.n = 0
        self.stopped = False
        self.ignore_cost = False

    COST = {"pe": 0.55, "dve": 0.35, "act": 0.35, "pool": 1.0, "sp": 0.1}

    def add(self, eng, fn, reads=(), writes=(), dma=False, cost=None):
        op = Op()
        if self.stopped:
            op.deps, op.signal, op.is_dma, op.eng = [], False, dma, eng
            return op
        if self.ignore_cost:
            cost = None
        op.cost = cost if cost is not None else (0.1 if dma else self.COST[eng])
        op.lat = (cost if cost is not None else 3.0) if dma else op.cost
        op.eng, op.fn, op.is_dma, op.signal = eng, fn, dma, False
        op.idx = self.n
        self.n += 1
        deps = []
        for r in reads:
            w = self.last_w.get(r)
            if w is not None:
                deps.append(w)
            if isinstance(r, tuple) and r[0] == "bank":
                deps.extend(o for o in self.readers.get(r, ()) if o.eng != eng)
        for w_ in writes:
            w = self.last_w.get(w_)
            if w is not None:
                deps.append(w)
            deps.extend(self.readers.get(w_, ()))
        for r in reads:
            self.readers.setdefault(r, []).append(op)
        for w_ in writes:
            self.last_w[w_] = op
            self.readers[w_] = []
        if dma:
            k = self.dma_count[eng]
            self.dma_count[eng] += 1
            slot = (eng, k % self.ndma)
            op.sem = slot
            prev = self.dma_prev.get(slot)
            if prev is not None:
                deps.append(prev)
            self.dma_prev[slot] = op
            op.signal = True
        ded, oded, seen = [], [], set()
        for d in deps:
            if d is op or id(d) in seen:
                continue
            seen.add(id(d))
            oded.append(d)
            if (not d.is_dma) and d.eng == eng and eng == "pe":
                continue
            ded.append(d)
        op.deps = ded
        op.odeps = oded
        self.ops[eng].append(op)
        return op

    def reorder(self):
        import heapq
        allops = [op for e in self.ENG for op in self.ops[e]]
        for op in allops:
            op.users, op.nrem, op.fin = [], len(op.odeps), None
        for op in allops:
            for d in op.odeps:
                d.users.append(op)
        SYNC = 0.6
        fut = {e: [] for e in self.ENG}
        now = {e: [] for e in self.ENG}
        t = {e: 0.0 for e in self.ENG}
        new = {e: [] for e in self.ENG}

        def push(op):
            r = 0.0
            for d in op.odeps:
                f = d.fin + (0.0 if (d.eng == op.eng and not d.is_dma) else SYNC)
                if f > r:
                    r = f
            heapq.heappush(fut[op.eng], (r, op.idx, op))
        for op in allops:
            if op.nrem == 0:
                push(op)
        left = len(allops)
        while left:
            best = None
            for e in self.ENG:
                while fut[e] and fut[e][0][0] <= t[e]:
                    r, i, op = heapq.heappop(fut[e])
                    heapq.heappush(now[e], (i, op))
                if now[e]:
                    st = t[e]
                elif fut[e]:
                    st = fut[e][0][0]
                else:
                    continue
                if best is None or st < best[0]:
                    best = (st, e)
            st, e = best
            if now[e]:
                i, op = heapq.heappop(now[e])
            else:
                r, i, op = heapq.heappop(fut[e])
            new[e].append(op)
            t[e] = st + op.cost
            op.fin = st + op.lat
            left -= 1
            for u in op.users:
                u.nrem -= 1
                if u.nrem == 0:
                    push(u)
        self.ops = new
        self.est = max(t.values())

    def emit(self, dummies, final_waits=()):
        nc = self.nc
        if os.environ.get("KNOSCHED") is None:
            self.reorder()
        for e, fn in dummies.items():
            o = self.add(e, fn, writes=[("bank", 0)] if e == "pe" else ())
            o.signal = True
        for e in self.ENG:
            for op in self.ops[e]:
                for d in op.deps:
                    d.signal = True
        esem = {e: self.semstack.enter_context(nc.semaphore(f"s{self.tag}_{e}")) for e in self.ENG}
        dsem = {}
        for e in self.ENG:
            for i in range(min(self.ndma, self.dma_count[e])):
                dsem[(e, i)] = self.semstack.enter_context(nc.semaphore(f"d{self.tag}_{e}{i}"))
        finals = {}
        for e in self.ENG:
            c, dc = 0, {}
            for op in self.ops[e]:
                if op.is_dma:
                    dc[op.sem] = dc.get(op.sem, 0) + 16
                    op.val = dc[op.sem]
                    op.sem = dsem[op.sem]
                    finals[id(op.sem)] = (op.sem, op.val)
                elif op.signal:
                    c += 1
                    op.val = c
                    op.sem = esem[e]
                    finals[id(op.sem)] = (op.sem, op.val)
        with nc.Block() as block:
            def run(e, eng):
                waited = {}
                for op in self.ops[e]:
                    for d in op.deps:
                        key = id(d.sem)
                        if waited.get(key, 0) >= d.val:
                            continue
                        waited[key] = d.val
                        eng.wait_ge(d.sem, d.val)
                    ins = op.fn(eng)
                    if op.signal:
                        ins.then_inc(op.sem, 16 if op.is_dma else 1)
                for sem, val in finals.values():
                    if waited.get(id(sem), 0) < val:
                        eng.wait_ge(sem, val)

            @block.tensor
            def _(eng):
                run("pe", eng)

            @block.vector
            def _(eng):
                run("dve", eng)

            @block.scalar
            def _(eng):
                run("act", eng)

            @block.gpsimd
            def _(eng):
                run("pool", eng)

            @block.sync
            def _(eng):
                run("sp", eng)


def build(debug=False):
    nc = bass.Bass("TRN2", target_bir_lowering=False)

    def din(name, shape):
        return nc.dram_tensor(name, list(shape), F32, kind="ExternalInput").ap()

    xo = din("xo", [TOK, D]); xp = din("xp", [TOK, D])
    w_in = din("w_in", [D, NIN]); w_out = din("w_out", [D, D]); pool_w = din("pool_w", [4, 128, 128])
    g1c = din("g1c", [128, 8]); g2c = din("g2c", [128, 8]); gfb = din("gfb", [128, D])
    convT = din("convT", [128, 8, 4]); igb = din("igb", [128, 1]); fgbn = din("fgbn", [128, 1])
    hngb = din("hngb", [128, 512]); pscale = din("pscale", [128, 4])
    w_router = din("w_router", [D, NE]); brb = din("brb", [128, NE])
    w_gate = din("w_gate", [NE, D, D]); w_up = din("w_up", [NE, D, D]); w_down = din("w_down", [NE, D, D])
    bgT = din("bgT", [128, NE, 8]); buT = din("buT", [128, NE, 8]); b_down = din("b_down", [NE, D])
    ident = din("ident", [128, 128]); maskrl = din("maskrl", [128, 128]); corr = din("corr", [128, 4, 16])
    flag = din("flag", [128, 1]); resetm = din("resetm", [128, 128])
    g2b = din("g2b", [128, D]); iotaj = din("iotaj", [128, 128]); ltstrict = din("ltstrict", [128, 128])
    out = nc.dram_tensor("out", [TOK, D], F32, kind="ExternalOutput").ap()
    dbg = nc.dram_tensor("dbg", [TOK, D], F32, kind="ExternalOutput").ap() if debug else None
    dbgG = nc.dram_tensor("dbgG", [TOK, NE], F32, kind="ExternalOutput").ap() if debug else None
    ne_run = int(os.environ.get("KNE", NE)) if debug else NE

    with contextlib.ExitStack() as st, contextlib.ExitStack() as semst:
        def sb(stack, name, shape, dt):
            return stack.enter_context(nc.sbuf_tensor(name, list(shape), dt))

        def ps(stack, name, shape, dt):
            return stack.enter_context(nc.psum_tensor(name, list(shape), dt))

        x1 = sb(st, "x1", [128, NT, D], F32)
        wa = sb(st, "wa", [128, 3 * 8192], BF16)
        G = sb(st, "G", [128, NT, NE], F32)
        rstd2 = sb(st, "rstd2", [128, NT], F32)
        identf = sb(st, "identf", [128, 128], F32)
        identb = sb(st, "identb", [128, 128], BF16)
        gfb_s = sb(st, "gfb_s", [128, D], F32)
        g2c_s = sb(st, "g2c_s", [128, 8], F32)
        dum = sb(st, "dum", [128, 8], F32)
        banks = [ps(st, f"bank{i}", [128, 512], F32) for i in range(8)]

        def dummies():
            return {
                "dve": lambda e: e.memset(dum[:, 0:1], 0.0),
                "act": lambda e: e.activation(out=dum[:, 1:2], in_=identf[:, 0:1], func=AF.Copy),
                "pool": lambda e: e.memset(dum[:, 3:4], 0.0),
            }

        with contextlib.ExitStack() as sa:
            S = Sched(nc, semst, "A")
            bank_i = [0]
            reserved = set()

            def nb():
                while True:
                    i = bank_i[0] % 8
                    bank_i[0] += 1
                    if i not in reserved:
                        return i

            win = wa[:, 0:8 * NIN].rearrange("p (k f) -> p k f", k=8)
            wout = sb(sa, "wout", [128, 8, D], BF16)
            dconv = sb(sa, "dconv", [128, 8, 4, 128], BF16)
            convT_s = sb(sa, "convT_s", [128, 8, 4], F32)
            g1c_s = sb(sa, "g1c_s", [128, 8], F32)
            wg = sb(sa, "wg", [128, 8, 8], BF16)
            hngb_s = sb(sa, "hngb_s", [128, 512], F32)
            poolw = sb(sa, "poolw", [128, 4, 128], BF16)
            pscale_s = sb(sa, "pscale_s", [128, 4], F32)
            wr = sb(sa, "wr", [128, 8, NE], F32)
            brb_s = sb(sa, "brb_s", [128, NE], F32)
            maskb = sb(sa, "maskb", [128, 128], BF16)
            maskf = sb(sa, "maskf", [128, 128], F32)
            corr_s = sb(sa, "corr_s", [128, 4, 16], F32)
            flag_s = sb(sa, "flag_s", [128, 1], F32)
            igb_s = sb(sa, "igb_s", [128, 1], F32)
            fgbn_s = sb(sa, "fgbn_s", [128, 1], F32)
            resetm_s = sb(sa, "resetm_s", [128, 128], F32)
            ones1 = sb(sa, "ones1", [1, 128], F32)
            rstd1 = sb(sa, "rstd1", [128, 32], F32)
            ssq = sb(sa, "ssq", [128, 48], F32)
            xin = [sb(sa, "xin0", [128, D], F32)] * 2
            hs = [sb(sa, f"hs{i}", [128, D], BF16) for i in range(2)]
            hT = [sb(sa, f"hT{i}", [128, 8, 128], BF16) for i in range(2)]
            gcol = sb(sa, "gcol", [128, 8], F32)
            grow = sb(sa, "grow", [1, 12, 128], F32)
            tokT = sb(sa, "tokT", [128, 3, 128], F32)
            bcs = sb(sa, "bcs", [128, 3, 128], F32)
            ubc = [sb(sa, f"ubc{i}", [128, 4, 3 + 128], BF16) for i in range(2)]
            halo_c = sb(sa, "halo_c", [128, 8, 3], BF16)
            qkT = [sb(sa, f"qkT{i}", [128, 8, 128], BF16) for i in range(2)]
            kTok = [sb(sa, f"kTok{i}", [128, 4, 128], BF16) for i in range(2)]
            v1 = [sb(sa, f"v1{i}", [128, 4, 129], BF16) for i in range(2)]
            osig = [sb(sa, f"osig{i}", [128, 512], BF16) for i in range(2)]
            Cst = sb(sa, "Cst", [128, 4, 129], F32)
            Ctmp = sb(sa, "Ctmp", [128, 4, 129], F32)
            Cs = sb(sa, "Cs", [128, 4, 129], BF16)
            sTp = [sb(sa, f"sTp{i}", [128, 128], BF16) for i in range(2)]
            wv = [sb(sa, f"wv{i}", [128, 129], BF16) for i in range(2)]
            dmx = sb(sa, "dmx", [128, 8], F32)
            hm = sb(sa, "hm", [128, 4, 128], F32)
            bst = sb(sa, "bst", [128, 4, 6], F32)
            mv = sb(sa, "mv", [128, 4, 2], F32)
            lnr = sb(sa, "lnr", [128, 4], F32)
            ym = sb(sa, "ym", [128, 512], BF16)
            yT = [sb(sa, f"yT{i}", [128, 8, 128], BF16) for i in range(2)]
            pu = [sb(sa, f"pu{i}", [128, 16 + 128], F32) for i in range(4)]
            phalo = sb(sa, "phalo", [128, 4, 16], F32)
            pooledT = sb(sa, "pooledT", [128, 128], BF16)
            h2f = sb(sa, "h2f", [128, D], F32)
            ymf = h2f[:, 0:512]
            gm = [hm[:, 0, :], hm[:, 1, :], hm[:, 2, :], hm[:, 3, :], h2f[:, 0:128], h2f[:, 128:256]]
            h2Tf = sb(sa, "h2Tf", [128, 8, 128], F32)
            gsb = h2Tf[:, 0:2, :]
            lg = sb(sa, "lg", [128, NE], F32)
            top8 = sb(sa, "top8", [128, 8], F32)
            rt = sb(sa, "rt", [128, 4, NE], F32)
            rsm = sb(sa, "rsm", [128, 4], F32)

            klim = float(os.environ.get("KSTAGE", "99")) if debug else 99

            def ckpt(k):
                if klim <= k:
                    S.stopped = True
            def ld(eng, dst, src, wkey, rk=()):
                return S.add(eng, lambda e: e.dma_start(out=dst, in_=src), reads=rk, writes=[wkey], dma=True)

            ld("sp", identf[:], ident, "identf")
            S.add("dve", lambda e: e.tensor_copy(out=identb[:], in_=identf[:]), reads=["identf"], writes=["identb"])
            ld("sp", maskf[:], maskrl, "maskf")
            S.add("dve", lambda e: e.tensor_copy(out=maskb[:], in_=maskf[:]), reads=["maskf"], writes=["maskb"])
            for dst, src, k in ((g1c_s, g1c, "g1c"), (g2c_s, g2c, "g2c"), (gfb_s, gfb, "gfb"), (convT_s, convT, "convT"),
                                (hngb_s, hngb, "hngb"), (pscale_s, pscale, "pscale"), (brb_s, brb, "brb"),
                                (corr_s, corr, "corr"), (flag_s, flag, "flag"), (igb_s, igb, "igb"),
                                (fgbn_s, fgbn, "fgbn"), (resetm_s, resetm, "resetm")):
                ld("sp", dst[:], src, k)
            ld("sp", wr[:], w_router.rearrange("(k p) e -> p k e", p=128), "wr")
            S.add("dve", lambda e: e.tensor_scalar(out=fgbn_s[:], in0=fgbn_s[:], scalar1=-1.0, scalar2=None, op0=ALU.mult),
                  reads=["fgbn"], writes=["fgbn"])
            w_in_v = w_in.rearrange("(k p) f -> p k f", p=128)
            ld("pool", wg[:], w_in_v[:, :, 2048:2056], "wg")
            for k in range(8):
                S.add("dve", lambda e, k=k: e.tensor_scalar(out=wg[:, k, :], in0=wg[:, k, :], scalar1=g1c_s[:, k:k + 1],
                                                            scalar2=None, op0=ALU.mult),
                      reads=["g1c", "wg"], writes=["wg"], cost=0.1)
            for k in range(8):
                ld("pool", win[:, k, :], w_in_v[:, k, :], ("wa", k))
            ld("pool", wout[:], w_out.rearrange("(k p) f -> p k f", p=128), "wout")
            ld("pool", poolw[:], pool_w.rearrange("g c d -> c g d"), "poolw")
            for k in range(8):
                if k % 2:
                    S.add("dve", lambda e, k=k: e.tensor_scalar(out=win[:, k, :], in0=win[:, k, :], scalar1=g1c_s[:, k:k + 1],
                                                                scalar2=None, op0=ALU.mult),
                          reads=["g1c", ("wa", k)], writes=[("wa", k)])
                else:
                    S.add("act", lambda e, k=k: e.activation(out=win[:, k, :], in_=win[:, k, :], func=AF.Copy,
                                                             scale=g1c_s[:, k:k + 1]),
                          reads=["g1c", ("wa", k)], writes=[("wa", k)])
            for k in range(8):
                for j in range(4):
                    S.add("dve", lambda e, k=k, j=j: e.tensor_scalar(out=dconv[:, k, j, :], in0=identf[:],
                                                                       scalar1=convT_s[:, k, j:j + 1], scalar2=None,
                                                                       op0=ALU.mult),
                          reads=["identf", "convT"], writes=[("dconv", k)])
            S.add("dve", lambda e: e.memset(ones1[:], 1.0), writes=["ones1"])
            S.add("dve", lambda e: e.memset(Cst[:], 0.0), writes=[("Cst", h) for h in range(4)])
            S.add("dve", lambda e: e.memset(halo_c[:], 0.0), writes=[("halo_c", 0), ("halo_c", 1)])
            S.add("dve", lambda e: e.memset(phalo[:], 0.0), writes=[("phalo", g) for g in range(4)])
            S.add("dve", lambda e: e.memset(v1[0][:], 1.0), writes=[("v1", 0)])
            S.add("dve", lambda e: e.memset(v1[1][:], 1.0), writes=[("v1", 1)])
            WA = [("wa", k) for k in range(8)]

            ckpt(1)
            cnt = [0]

            def norm_T(ci, first):
                i = cnt[0] % 2
                cnt[0] += 1
                own = ci >= 16
                if own:
                    xt, xkey = x1[:, ci - 16, :], ("x1", ci - 16)
                elif ci % 2 == 0:
                    xt, xkey = xin[0][:], ("xin", 0)
                else:
                    xt, xkey = h2f[:], "h2f"
                if first or not own:
                    src = (xo if own else xp)[(ci % 16) * 128:(ci % 16 + 1) * 128, :]
                    S.add("sp", lambda e: e.dma_start(out=xt, in_=src), writes=[xkey], dma=True)
                if first:
                    S.add("act", lambda e: e.activation(out=hs[i][:], in_=xt, func=AF.Square,
                                                        accum_out=ssq[:, ci:ci + 1]),
                          reads=[xkey], writes=[("hs", i), ("ssq", ci)], cost=1.0)
                    S.add("act", lambda e: e.activation(out=ssq[:, ci:ci + 1], in_=ssq[:, ci:ci + 1], func=AF.Ln,
                                                        scale=1.0 / D, bias=EPS),
                          reads=[("ssq", ci)], writes=[("ssq", ci)])
                    S.add("act", lambda e: e.activation(out=rstd1[:, ci:ci + 1], in_=ssq[:, ci:ci + 1], func=AF.Exp,
                                                        scale=-0.5),
                          reads=[("ssq", ci)], writes=[("rstd1", ci)])
                S.add("act", lambda e: e.activation(out=hs[i][:], in_=xt, func=AF.Copy, scale=rstd1[:, ci:ci + 1]),
                      reads=[xkey, ("rstd1", ci)], writes=[("hs", i)], cost=1.1)
                b = nb()
                pb = banks[b][:].bitcast(BF16)

                def tr(e):
                    for k in range(8):
                        ins = e.transpose(out=pb[:, k * 128:(k + 1) * 128], in_=hs[i][:, k * 128:(k + 1) * 128],
                                          identity=identb[:])
                    return ins
                S.add("pe", tr, reads=[("hs", i), "identb"], writes=[("bank", b)], cost=0.8)
                S.add("dve", lambda e: e.tensor_copy(out=hT[i][:].rearrange("p k t -> p (k t)"), in_=pb[:, 0:1024]),
                      reads=[("bank", b)], writes=[("hT", i)], cost=0.6)
                return i

            gb = nb()
            reserved.add(gb)
            gall = banks[gb][:, 0:256].rearrange("p (c a) -> p c a", a=8)
            for ci in range(32):
                i = norm_T(ci, True)

                def gm_(e, i=i, ci=ci):
                    for k in range(8):
                        ins = e.matmul(gall[:, ci, :], lhsT=hT[i][:, k, :], rhs=wg[:, k, :],
                                       start=(k == 0), stop=(k == 7), skip_group_check=True)
                    return ins
                S.add("pe", gm_, reads=[("hT", i), "wg"], writes=[("bank", gb)])
            ckpt(2)
            for a in range(2):
                S.add("dve", lambda e, a=a: e.tensor_copy(out=gsb[:, a, :].rearrange("p (c h) -> p c h", h=4),
                                                          in_=gall[:, :, a * 4:(a + 1) * 4]),
                      reads=[("bank", gb)], writes=[("gsb", a)])
            reserved.discard(gb)
            tb = nb()
            tps = banks[tb]

            def trg(e):
                e.transpose(out=tps[:, 0:128], in_=gsb[:, 0, :], identity=identf[:])
                return e.transpose(out=tps[:, 128:256], in_=gsb[:, 1, :], identity=identf[:])
            S.add("pe", trg, reads=[("gsb", 0), ("gsb", 1), "identf"], writes=[("bank", tb)])
            IG, BP, DD, T0, T1, T2 = gm
            S.add("act", lambda e: e.activation(out=IG[:], in_=tps[:, 0:128], func=AF.Identity, bias=igb_s[:, 0:1]),
                  reads=[("bank", tb), "igb"], writes=["IG"])
            S.add("act", lambda e: e.activation(out=T0[:], in_=tps[:, 128:256], func=AF.Exp, scale=-1.0,
                                                bias=fgbn_s[:, 0:1]),
                  reads=[("bank", tb), "fgbn"], writes=["T0"])
            S.add("act", lambda e: e.activation(out=T0[:], in_=T0[:], func=AF.Ln, bias=1.0),
                  reads=["T0"], writes=["T0"])
            S.add("dve", lambda e: e.tensor_tensor_scan(out=BP[:], data0=resetm_s[:], data1=T0[:], initial=0.0,
                                                        op0=ALU.mult, op1=ALU.add),
                  reads=["T0", "resetm"], writes=["BP"])
            S.add("dve", lambda e: e.tensor_tensor(out=DD[:], in0=IG[:], in1=BP[:], op=ALU.add),
                  reads=["IG", "BP"], writes=["DD"])
            S.add("dve", lambda e: e.tensor_reduce(out=gcol[:, 0:1], in_=DD[:], axis=AX.X, op=ALU.max),
                  reads=["DD"], writes=["gcol"])
            S.add("dve", lambda e: e.tensor_scalar(out=gcol[:, 1:2], in0=BP[:, 127:128], scalar1=-1.0, scalar2=None,
                                                   op0=ALU.mult),
                  reads=["BP", "gcol"], writes=["gcol"])
            S.add("dve", lambda e: e.tensor_tensor(out=gcol[:, 2:3], in0=gcol[:, 0:1], in1=gcol[:, 1:2], op=ALU.add),
                  reads=["gcol"], writes=["gcol"])
            rb = nb()
            rps = banks[rb]

            def trc(e):
                for q in range(3):
                    ins = e.transpose(out=rps[0:1, q * 128:(q + 1) * 128], in_=gcol[:, q:q + 1], identity=identf[:])
                return ins
            S.add("pe", trc, reads=["gcol", "identf"], writes=[("bank", rb)])
            S.add("dve", lambda e: e.tensor_copy(out=grow[:, 0:3, :].rearrange("p a c -> p (a c)"),
                                                 in_=rps[0:1, 0:384]),
                  reads=[("bank", rb)], writes=["grow"])
            R = lambda q: grow[0:1, q, :]
            for h in range(4):
                S.add("dve", lambda e, h=h: e.tensor_tensor_scan(out=grow[0:1, 3, h:64:4], data0=grow[0:1, 1, h:64:4],
                                                                 data1=grow[0:1, 2, h:64:4], initial=0.0,
                                                                 op0=ALU.add, op1=ALU.max),
                      reads=["grow"], writes=["grow"])
            S.add("dve", lambda e: e.memset(grow[0:1, 4, 0:4], 0.0), reads=["grow"], writes=["grow"])
            S.add("dve", lambda e: e.tensor_copy(out=grow[0:1, 4, 4:64], in_=grow[0:1, 3, 0:60]),
                  reads=["grow"], writes=["grow"])
            S.add("dve", lambda e: e.tensor_scalar(out=grow[0:1, 4, 64:68], in0=grow[0:1, 3, 60:64],
                                                   scalar1=flag_s[0:1, 0:1], scalar2=None, op0=ALU.mult),
                  reads=["grow", "flag"], writes=["grow"])
            for h in range(4):
                S.add("dve", lambda e, h=h: e.tensor_tensor_scan(out=grow[0:1, 3, 64 + h:128:4],
                                                                 data0=grow[0:1, 1, 64 + h:128:4],
                                                                 data1=grow[0:1, 2, 64 + h:128:4],
                                                                 initial=grow[0:1, 4, 64 + h:65 + h],
                                                                 op0=ALU.add, op1=ALU.max),
                      reads=["grow"], writes=["grow"])
            S.add("dve", lambda e: e.tensor_copy(out=grow[0:1, 4, 68:128], in_=grow[0:1, 3, 64:124]),
                  reads=["grow"], writes=["grow"])
            S.add("dve", lambda e: e.tensor_tensor(out=R(5), in0=R(4), in1=R(0), op=ALU.max), reads=["grow"], writes=["grow"])
            S.add("dve", lambda e: e.tensor_tensor(out=R(9), in0=R(1), in1=R(4), op=ALU.add), reads=["grow"], writes=["grow"])
            S.add("dve", lambda e: e.tensor_tensor(out=R(9), in0=R(9), in1=R(3), op=ALU.subtract), reads=["grow"], writes=["grow"])
            S.add("act", lambda e: e.activation(out=R(6), in_=R(9), func=AF.Exp), reads=["grow"], writes=["grow"])
            S.add("dve", lambda e: e.tensor_tensor(out=R(9), in0=R(2), in1=R(3), op=ALU.subtract), reads=["grow"], writes=["grow"])
            S.add("act", lambda e: e.activation(out=R(7), in_=R(9), func=AF.Exp), reads=["grow"], writes=["grow"])
            S.add("dve", lambda e: e.tensor_tensor(out=R(9), in0=R(4), in1=R(5), op=ALU.subtract), reads=["grow"], writes=["grow"])
            S.add("act", lambda e: e.activation(out=R(8), in_=R(9), func=AF.Exp), reads=["grow"], writes=["grow"])
            S.add("dve", lambda e: e.tensor_scalar(out=R(10), in0=R(5), scalar1=-1.0, scalar2=LNSC, op0=ALU.mult, op1=ALU.add),
                  reads=["grow"], writes=["grow"])
            S.add("dve", lambda e: e.tensor_scalar(out=R(11), in0=R(0), scalar1=-1.0, scalar2=LNSC, op0=ALU.mult, op1=ALU.add),
                  reads=["grow"], writes=["grow"])
            S.add("dve", lambda e: e.tensor_scalar(out=R(9), in0=R(5), scalar1=-1.0, scalar2=None, op0=ALU.mult),
                  reads=["grow"], writes=["grow"])
            bb = nb()
            bps = banks[bb]

            def bc(e):
                for q in range(3):
                    ins = e.matmul(bps[:, q * 128:(q + 1) * 128], lhsT=ones1[0:1, :], rhs=R(6 + q), start=True, stop=True,
                                   skip_group_check=True)
                return ins
            S.add("pe", bc, reads=["grow", "ones1"], writes=[("bank", bb)])
            S.add("dve", lambda e: e.tensor_copy(out=bcs[:].rearrange("p a c -> p (a c)"), in_=bps[:, 0:384]),
                  reads=[("bank", bb)], writes=["bcs"])
            cb = nb()
            cps = banks[cb]

            def trr(e):
                for q in range(3):
                    ins = e.matmul(cps[:, q:q + 1], lhsT=R(9 + q), rhs=ones1[0:1, 0:1], start=True, stop=True,
                                   skip_group_check=True)
                return ins
            S.add("pe", trr, reads=["grow", "ones1"], writes=[("bank", cb)])
            S.add("dve", lambda e: e.tensor_copy(out=gcol[:, 3:6], in_=cps[:, 0:3]), reads=[("bank", cb)], writes=["gcol"])
            S.add("act", lambda e: e.activation(out=T0[:], in_=DD[:], func=AF.Exp, bias=gcol[:, 4:5]),
                  reads=["DD", "gcol"], writes=["T0"])
            S.add("act", lambda e: e.activation(out=T1[:], in_=DD[:], func=AF.Exp, bias=gcol[:, 5:6]),
                  reads=["DD", "gcol"], writes=["T1", "h2f"])
            S.add("act", lambda e: e.activation(out=T2[:], in_=BP[:], func=AF.Exp, bias=gcol[:, 3:4]),
                  reads=["BP", "gcol"], writes=["T2", "h2f"])
            kb = nb()
            kps = banks[kb]

            def trt(e):
                for q, t in enumerate((T0, T1, T2)):
                    ins = e.transpose(out=kps[:, q * 128:(q + 1) * 128], in_=t[:], identity=identf[:])
                return ins
            S.add("pe", trt, reads=["T0", "T1", "T2", "h2f", "identf"], writes=[("bank", kb)])
            S.add("dve", lambda e: e.tensor_copy(out=tokT[:].rearrange("p a c -> p (a c)"), in_=kps[:, 0:384]),
                  reads=[("bank", kb)], writes=["tokT"])

            ucnt = [0]

            def proj_conv(i, cg, p, etmp, ekey, save_only=False):
                b = nb()
                pb = banks[b]

                def mm(e):
                    for c in range(4):
                        fk = cg * 4 + c
                        for k in range(8):
                            ins = e.matmul(pb[:, c * 128:(c + 1) * 128], lhsT=win[:, k, fk * 128:(fk + 1) * 128],
                                           rhs=hT[i][:, k, :], start=(k == 0), stop=(k == 7), skip_group_check=True)
                    return ins
                S.add("pe", mm, reads=[("hT", i)] + WA, writes=[("bank", b)], cost=2.6)
                u = ucnt[0] % 2
                ucnt[0] += 1
                S.add("dve", lambda e: e.tensor_copy(out=ubc[u][:, :, 0:3], in_=halo_c[:, cg * 4:(cg + 1) * 4, :]),
                      reads=[("halo_c", cg)], writes=[("ubc", u)])
                S.add("act", lambda e: e.activation(out=ubc[u][:, :, 3:131], in_=pb[:, :].rearrange("p (c t) -> p c t", c=4),
                                                    func=AF.Copy),
                      reads=[("bank", b)], writes=[("ubc", u)], cost=0.6)
                S.add("dve", lambda e: e.tensor_copy(out=halo_c[:, cg * 4:(cg + 1) * 4, :], in_=ubc[u][:, :, 128:131]),
                      reads=[("ubc", u)], writes=[("halo_c", cg)])
                if save_only:
                    return
                b2 = nb()
                pb2 = banks[b2]

                def cv(e):
                    for c in range(4):
                        fk = cg * 4 + c
                        for j in range(4):
                            ins = e.matmul(pb2[:, c * 128:(c + 1) * 128], lhsT=dconv[:, fk, j, :], rhs=ubc[u][:, c, j:j + 128],
                                           start=(j == 0), stop=(j == 3), skip_group_check=True)
                    return ins
                S.add("pe", cv, reads=[("ubc", u)] + [("dconv", cg * 4 + c) for c in range(4)], writes=[("bank", b2)], cost=1.4)
                S.add("act", lambda e: e.activation(out=etmp, in_=pb2[:, :], func=AF.Exp, scale=-1.0),
                      reads=[("bank", b2)], writes=[ekey], cost=0.6)
                S.add("act", lambda e: e.activation(out=etmp, in_=etmp, func=AF.Ln, bias=1.0),
                      reads=[ekey], writes=[ekey], cost=0.6)
                S.add("act", lambda e: e.activation(out=etmp, in_=etmp, func=AF.Exp, scale=-1.0),
                      reads=[ekey], writes=[ekey], cost=0.6)
                S.add("dve", lambda e: e.tensor_tensor(out=qkT[p][:, cg * 4:(cg + 1) * 4, :].rearrange("p c t -> p (c t)"),
                                                       in0=pb2[:, :], in1=etmp, op=ALU.mult),
                      reads=[("bank", b2), ekey], writes=[("qkT", p, cg * 4 + c) for c in range(4)], cost=0.6)

            def proj_v(i, p):
                b = nb()
                pb = banks[b]

                def mm(e):
                    for k in range(8):
                        ins = e.matmul(pb[:, :], lhsT=hT[i][:, k, :], rhs=win[:, k, 1024:1536], start=(k == 0), stop=(k == 7))
                    return ins
                S.add("pe", mm, reads=[("hT", i)] + WA, writes=[("bank", b)], cost=2.5)
                S.add("act", lambda e: e.activation(out=v1[p][:, :, 0:128], in_=pb[:, :].rearrange("p (h d) -> p h d", h=4),
                                                    func=AF.Copy),
                      reads=[("bank", b)], writes=[("v1", p)])

            def k_tok(p):
                b = nb()
                pb = banks[b][:].bitcast(BF16)

                def tr(e):
                    for h in range(4):
                        ins = e.transpose(out=pb[:, h * 128:(h + 1) * 128], in_=qkT[p][:, 4 + h, :], identity=identb[:])
                    return ins
                S.add("pe", tr, reads=[("qkT", p, 4 + h) for h in range(4)] + ["identb"], writes=[("bank", b)])
                S.add("dve", lambda e: e.tensor_copy(out=kTok[p][:].rearrange("p h d -> p (h d)"), in_=pb[:, 0:512]),
                      reads=[("bank", b)], writes=[("kTok", p)])

            def pool_proj(i, g, dst_fn):
                b = nb()
                pb = banks[b]

                def mm(e):
                    for k in range(8):
                        ins = e.matmul(pb[:, 0:128], lhsT=win[:, k, 2056 + g * 128:2056 + (g + 1) * 128],
                                       rhs=hT[i][:, k, :], start=(k == 0), stop=(k == 7))
                    return ins
                S.add("pe", mm, reads=[("hT", i)] + WA, writes=[("bank", b)])
                return b, pb

            def state_update(ci, p):
                for h in range(4):
                    ch = ci * 4 + h
                    w_ = wv[h % 2]
                    S.add("act", lambda e, h=h, ch=ch, w_=w_: e.activation(out=w_[:], in_=v1[p][:, h, :], func=AF.Copy,
                                                                           scale=tokT[:, 1, ch:ch + 1]),
                          reads=[("v1", p), "tokT"], writes=[("wv", h % 2)])
                    b = nb()
                    pb = banks[b]
                    S.add("pe", lambda e, h=h, w_=w_, pb=pb: e.matmul(pb[:, 0:129], lhsT=kTok[p][:, h, :], rhs=w_[:],
                                                                      start=True, stop=True),
                          reads=[("kTok", p), ("wv", h % 2)], writes=[("bank", b)])
                    S.add("dve", lambda e, h=h, ch=ch: e.tensor_scalar(out=Ctmp[:, h, :], in0=Cst[:, h, :],
                                                                       scalar1=bcs[:, 0, ch:ch + 1], scalar2=None,
                                                                       op0=ALU.mult),
                          reads=[("Cst", h), "bcs"], writes=[("Ctmp", h)])
                    S.add("dve", lambda e, h=h, ch=ch, pb=pb: e.scalar_tensor_tensor(out=Cst[:, h, :], in0=pb[:, 0:129],
                                                                                     scalar=bcs[:, 1, ch:ch + 1],
                                                                                     in1=Ctmp[:, h, :], op0=ALU.mult,
                                                                                     op1=ALU.add),
                          reads=[("bank", b), ("Ctmp", h), "bcs"], writes=[("Cst", h)])

            def front_prefix(ci, p):
                i = norm_T(ci, False)
                et, ek = h2Tf[:, 4:8, :].rearrange("p k t -> p (k t)"), ("h2Tf", 4)
                proj_conv(i, 1, p, et, ek)
                if ci == 15:
                    proj_conv(i, 0, p, et, ek, save_only=True)
                    for g in range(4):
                        b, pb = pool_proj(i, g, None)
                        S.add("act", lambda e, g=g, pb=pb: e.activation(out=phalo[:, g, :], in_=pb[:, 112:128], func=AF.Copy),
                              reads=[("bank", b)], writes=[("phalo", g)])
                proj_v(i, p)
                k_tok(p)

            for ci in range(16):
                front_prefix(ci, ci % 2)
                if ci > 0:
                    state_update(ci - 1, (ci - 1) % 2)
            ckpt(4)

            def front_own(ti, p):
                ci = 16 + ti
                i = norm_T(ci, False)
                et, ek = xin[0][:, 0:512], ("xin", 0)
                proj_conv(i, 0, p, et, ek)
                proj_conv(i, 1, p, et, ek)
                proj_v(i, p)
                b = nb()
                pb = banks[b]

                def mm(e):
                    for k in range(8):
                        ins = e.matmul(pb[:, :], lhsT=hT[i][:, k, :], rhs=win[:, k, 1536:2048], start=(k == 0), stop=(k == 7))
                    return ins
                S.add("pe", mm, reads=[("hT", i)] + WA, writes=[("bank", b)], cost=2.5)
                S.add("act", lambda e: e.activation(out=et, in_=pb[:, :], func=AF.Exp, scale=-1.0),
                      reads=[("bank", b)], writes=[ek], cost=0.6)
                S.add("dve", lambda e: e.tensor_scalar(out=et, in0=et, scalar1=1.0, scalar2=None, op0=ALU.add),
                      reads=[ek], writes=[ek], cost=0.5)
                def rcp(e):
                    with nc.allow_low_precision("sigmoid gate is stored in bf16 (matmul-operand precision)"):
                        return e.reciprocal(out=osig[p][:], in_=et)
                S.add("dve", rcp, reads=[ek], writes=[("osig", p)], cost=0.5)
                k_tok(p)
                for g in range(4):
                    b, pbg = pool_proj(i, g, None)
                    A_ = pu[0]
                    S.add("dve", lambda e, g=g: e.tensor_copy(out=A_[:, 0:16], in_=phalo[:, g, :]),
                          reads=[("phalo", g)], writes=["puA"])
                    S.add("act", lambda e, pbg=pbg: e.activation(out=A_[:, 16:144], in_=pbg[:, 0:128], func=AF.Copy),
                          reads=[("bank", b)], writes=["puA"])
                    S.add("dve", lambda e, g=g: e.tensor_copy(out=phalo[:, g, :], in_=A_[:, 128:144]),
                          reads=["puA"], writes=[("phalo", g)])
                    src_t, src_k = A_, "puA"
                    sh, lo = 1, 1
                    for s_ in range(g + 1):
                        dst_t = pu[1 + (s_ % 3)]
                        dk = "pu%d" % (1 + (s_ % 3))
                        S.add("dve", lambda e, src_t=src_t, dst_t=dst_t, sh=sh, lo=lo: e.tensor_tensor(
                            out=dst_t[:, lo:144], in0=src_t[:, lo:144], in1=src_t[:, lo - sh:144 - sh], op=ALU.add),
                            reads=[src_k], writes=[dk])
                        src_t, src_k = dst_t, dk
                        sh *= 2
                        lo = 2 * sh - 1
                    wdw = float(2 ** (g + 1))
                    if ti == 0:
                        S.add("dve", lambda e, src_t=src_t, g=g: e.tensor_tensor(out=src_t[:, 16:32], in0=src_t[:, 16:32],
                                                                                 in1=corr_s[:, g, :], op=ALU.mult),
                              reads=[src_k, "corr"], writes=[src_k])
                    S.add("dve", lambda e, src_t=src_t, wdw=wdw: e.scalar_tensor_tensor(
                        out=pooledT[:], in0=src_t[:, 16:144], scalar=1.0 / wdw, in1=A_[:, 16:144], op0=ALU.mult,
                        op1=ALU.subtract),
                        reads=[src_k, "puA"], writes=["pooledT"])
                    b2 = nb()
                    pb2 = banks[b2]
                    S.add("pe", lambda e, g=g, pb2=pb2: e.matmul(pb2[:, 0:128], lhsT=poolw[:, g, :], rhs=pooledT[:],
                                                                 start=True, stop=True),
                          reads=["pooledT", "poolw"], writes=[("bank", b2)])
                    S.add("act", lambda e, g=g, pb2=pb2: e.activation(out=yT[p][:, 4 + g, :], in_=pb2[:, 0:128], func=AF.Copy,
                                                                      scale=pscale_s[:, g:g + 1]),
                          reads=[("bank", b2), "pscale"], writes=[("yT", p, 4 + g)])

            def back_own(ti, p):
                ci = 16 + ti
                for h in range(4):
                    ch = ci * 4 + h
                    b = nb()
                    pb = banks[b]
                    S.add("pe", lambda e, h=h, pb=pb: e.matmul(pb[:, 0:128], lhsT=qkT[p][:, 4 + h, :], rhs=qkT[p][:, h, :],
                                                               start=True, stop=True),
                          reads=[("qkT", p, 4 + h), ("qkT", p, h)], writes=[("bank", b)])
                    s_ = sTp[h % 2]
                    S.add("dve", lambda e, ch=ch, pb=pb, s_=s_: e.scalar_tensor_tensor(
                        out=s_[:], in0=pb[:, 0:128], scalar=tokT[:, 0, ch:ch + 1], in1=maskb[:], op0=ALU.mult, op1=ALU.mult),
                        reads=[("bank", b), "tokT", "maskb"], writes=[("sTp", h % 2)])
                    S.add("act", lambda e, h=h, ch=ch: e.activation(out=Cs[:, h, :], in_=Cst[:, h, :], func=AF.Copy,
                                                                    scale=bcs[:, 2, ch:ch + 1]),
                          reads=[("Cst", h), "bcs"], writes=[("Cs", h)])
                    b2 = nb()
                    pb2 = banks[b2]

                    def nd(e, h=h, pb2=pb2, s_=s_):
                        e.matmul(pb2[:, 0:129], lhsT=s_[:], rhs=v1[p][:, h, :], start=True, stop=False)
                        return e.matmul(pb2[:, 0:129], lhsT=qkT[p][:, h, :], rhs=Cs[:, h, :], start=False, stop=True)
                    S.add("pe", nd, reads=[("sTp", h % 2), ("v1", p), ("qkT", p, h), ("Cs", h)], writes=[("bank", b2)])
                    S.add("dve", lambda e, h=h, pb2=pb2: e.tensor_scalar(out=dmx[:, h:h + 1], in0=pb2[:, 128:129], scalar1=-1.0,
                                                                         scalar2=None, op0=ALU.mult),
                          reads=[("bank", b2)], writes=[("dmx", h)])
                    S.add("dve", lambda e, h=h, pb2=pb2: e.tensor_tensor(out=dmx[:, h:h + 1], in0=pb2[:, 128:129],
                                                                         in1=dmx[:, h:h + 1], op=ALU.max),
                          reads=[("bank", b2), ("dmx", h)], writes=[("dmx", h)])
                    S.add("dve", lambda e, h=h, ch=ch: e.tensor_scalar(
                        out=dmx[:, h:h + 1], in0=dmx[:, h:h + 1], scalar1=tokT[:, 2, ch:ch + 1], scalar2=None,
                        op0=ALU.max),
                        reads=[("dmx", h), "tokT"], writes=[("dmx", h)])
                    S.add("dve", lambda e, h=h: e.reciprocal(out=dmx[:, 4 + h:5 + h], in_=dmx[:, h:h + 1]),
                          reads=[("dmx", h)], writes=[("dmx", 4 + h)])
                    S.add("act", lambda e, h=h, pb2=pb2: e.activation(out=hm[:, h, :], in_=pb2[:, 0:128], func=AF.Copy,
                                                                      scale=dmx[:, 4 + h:5 + h]),
                          reads=[("bank", b2), ("dmx", 4 + h)], writes=[("hm", h)])
                    S.add("dve", lambda e, h=h: e.bn_stats(out=bst[:, h, :], in_=hm[:, h, :]),
                          reads=[("hm", h)], writes=[("bst", h)])
                    S.add("dve", lambda e, h=h: e.bn_aggr(out=mv[:, h, :], in_=bst[:, h, :]),
                          reads=[("bst", h)], writes=[("mv", h)])
                state_update(ci, p)
                S.add("act", lambda e: e.activation(out=lnr[:], in_=mv[:, :, 1], func=AF.Ln, bias=EPS),
                      reads=[("mv", h) for h in range(4)], writes=["lnr"])
                S.add("act", lambda e: e.activation(out=lnr[:], in_=lnr[:], func=AF.Exp, scale=-0.5),
                      reads=["lnr"], writes=["lnr"])
                for h in range(4):
                    S.add("dve", lambda e, h=h: e.tensor_scalar(out=h2f[:, h * 128:(h + 1) * 128], in0=hm[:, h, :],
                                                                scalar1=mv[:, h, 0:1], scalar2=lnr[:, h:h + 1],
                                                                op0=ALU.subtract, op1=ALU.mult),
                          reads=[("hm", h), ("mv", h), "lnr"], writes=["h2f"])
                S.add("dve", lambda e: e.tensor_tensor(out=ymf, in0=ymf, in1=hngb_s[:], op=ALU.mult),
                      reads=["h2f", "hngb"], writes=["h2f"])
                S.add("dve", lambda e: e.tensor_tensor(out=ym[:], in0=ymf, in1=osig[p][:], op=ALU.mult),
                      reads=["h2f", ("osig", p)], writes=["ym"])
                b = nb()
                pbb = banks[b][:].bitcast(BF16)

                def tr(e, pbb=pbb):
                    for h in range(4):
                        ins = e.transpose(out=pbb[:, h * 128:(h + 1) * 128], in_=ym[:, h * 128:(h + 1) * 128],
                                          identity=identb[:])
                    return ins
                S.add("pe", tr, reads=["ym", "identb"], writes=[("bank", b)])
                S.add("act", lambda e, pbb=pbb: e.activation(out=yT[p][:, 0:4, :].rearrange("p h t -> p (h t)"), in_=pbb[:, 0:512],
                                                             func=AF.Copy),
                      reads=[("bank", b)], writes=[("yT", p, h) for h in range(4)])
                for hf in range(2):
                    b = nb()
                    pb = banks[b]

                    def mm(e, hf=hf, pb=pb):
                        for k in range(8):
                            ins = e.matmul(pb[:, :], lhsT=yT[p][:, k, :], rhs=wout[:, k, hf * 512:(hf + 1) * 512],
                                           start=(k == 0), stop=(k == 7))
                        return ins
                    S.add("pe", mm, reads=[("yT", p, k) for k in range(8)] + ["wout"], writes=[("bank", b)], cost=2.5)
                    S.add("dve", lambda e, hf=hf, pb=pb: e.tensor_tensor(
                        out=x1[:, ti, hf * 512:(hf + 1) * 512], in0=pb[:, :], in1=x1[:, ti, hf * 512:(hf + 1) * 512], op=ALU.add),
                        reads=[("bank", b), ("x1", ti)], writes=[("x1", ti)])
                S.add("act", lambda e: e.activation(out=h2f[:], in_=x1[:, ti, :], func=AF.Square,
                                                    accum_out=ssq[:, 32 + ti:33 + ti]),
                      reads=[("x1", ti)], writes=["h2f", ("ssq", 32 + ti)])
                S.add("act", lambda e: e.activation(out=ssq[:, 32 + ti:33 + ti], in_=ssq[:, 32 + ti:33 + ti],
                                                    func=AF.Ln, scale=1.0 / D, bias=EPS),
                      reads=[("ssq", 32 + ti)], writes=[("ssq", 32 + ti)])
                S.add("act", lambda e: e.activation(out=rstd2[:, ti:ti + 1], in_=ssq[:, 32 + ti:33 + ti],
                                                    func=AF.Exp, scale=-0.5),
                      reads=[("ssq", 32 + ti)], writes=[("rstd2", ti)])
                S.add("act", lambda e: e.activation(out=h2f[:], in_=x1[:, ti, :], func=AF.Copy, scale=rstd2[:, ti:ti + 1]),
                      reads=[("x1", ti), ("rstd2", ti)], writes=["h2f"])
                for hf in range(2):
                    b = nb()
                    pb = banks[b]

                    def tr(e, hf=hf, pb=pb):
                        for j in range(4):
                            k = hf * 4 + j
                            ins = e.transpose(out=pb[:, j * 128:(j + 1) * 128], in_=h2f[:, k * 128:(k + 1) * 128],
                                              identity=identf[:])
                        return ins
                    S.add("pe", tr, reads=["h2f", "identf"], writes=[("bank", b)])
                    for j in range(4):
                        k = hf * 4 + j
                        S.add("act" if hf else "dve",
                              (lambda e, k=k, j=j, pb=pb: e.activation(out=h2Tf[:, k, :], in_=pb[:, j * 128:(j + 1) * 128],
                                                                       func=AF.Copy, scale=g2c_s[:, k:k + 1])) if hf else
                              (lambda e, k=k, j=j, pb=pb: e.tensor_scalar(out=h2Tf[:, k, :], in0=pb[:, j * 128:(j + 1) * 128],
                                                                          scalar1=g2c_s[:, k:k + 1], scalar2=None,
                                                                          op0=ALU.mult)),
                              reads=[("bank", b), "g2c"], writes=[("h2Tf", k)])
                b = nb()
                pb = banks[b]

                def rmm(e, pb=pb):
                    for k in range(8):
                        ins = e.matmul(pb[:, 0:NE], lhsT=h2Tf[:, k, :], rhs=wr[:, k, :], start=(k == 0), stop=(k == 7))
                    return ins
                S.add("pe", rmm, reads=[("h2Tf", k) for k in range(8)] + ["wr"], writes=[("bank", b)])
                S.add("dve", lambda e, pb=pb: e.tensor_tensor(out=lg[:], in0=pb[:, 0:NE], in1=brb_s[:], op=ALU.add),
                      reads=[("bank", b), "brb"], writes=["lg"])
                S.add("dve", lambda e: e.max(out=top8[:], in_=lg[:]), reads=["lg"], writes=["top8"])
                S.add("dve", lambda e: e.tensor_scalar(out=rt[:, 0, :], in0=lg[:], scalar1=top8[:, 3:4], scalar2=None,
                                                       op0=ALU.is_ge),
                      reads=["lg", "top8"], writes=["rt0"])
                S.add("dve", lambda e: e.tensor_scalar(out=rsm[:, 0:1], in0=top8[:, 0:1], scalar1=-1.0, scalar2=None,
                                                       op0=ALU.mult),
                      reads=["top8"], writes=["rsm0"])
                S.add("act", lambda e: e.activation(out=rt[:, 1, :], in_=lg[:], func=AF.Exp, bias=rsm[:, 0:1]),
                      reads=["lg", "rsm0"], writes=["rt1"])
                S.add("dve", lambda e: e.tensor_tensor(out=rt[:, 2, :], in0=rt[:, 1, :], in1=rt[:, 0, :], op=ALU.mult),
                      reads=["rt0", "rt1"], writes=["rt2"])
                S.add("dve", lambda e: e.tensor_reduce(out=rsm[:, 1:2], in_=rt[:, 2, :], axis=AX.X, op=ALU.add),
                      reads=["rt2"], writes=["rsm1"])
                S.add("dve", lambda e: e.reciprocal(out=rsm[:, 2:3], in_=rsm[:, 1:2]), reads=["rsm1"], writes=["rsm2"])
                S.add("dve", lambda e: e.tensor_scalar(out=G[:, ti, :], in0=rt[:, 2, :], scalar1=rsm[:, 2:3],
                                                       scalar2=None, op0=ALU.mult),
                      reads=["rt2", "rsm2"], writes=[("G", ti)])

            front_own(0, 0)
            state_update(15, 1)
            for h in range(4):
                S.add("dve", lambda e, h=h: e.tensor_scalar(out=Cst[:, h, :], in0=Cst[:, h, :], scalar1=flag_s[:, 0:1],
                                                            scalar2=None, op0=ALU.mult),
                      reads=[("Cst", h), "flag"], writes=[("Cst", h)])
            for ti in range(NT):
                if ti + 1 < NT:
                    front_own(ti + 1, (ti + 1) % 2)
                back_own(ti, ti % 2)
            S.stopped = False
            if debug:
                for ti in range(NT):
                    S.add("sp", lambda e, ti=ti: e.dma_start(out=dbg[ti * 128:(ti + 1) * 128, :], in_=x1[:, ti, :]),
                          reads=[("x1", ti)], dma=True)
                    S.add("sp", lambda e, ti=ti: e.dma_start(out=dbgG[ti * 128:(ti + 1) * 128, :], in_=G[:, ti, :]),
                          reads=[("G", ti)], dma=True)
            dm = dummies()
            dpb = banks[0]
            dm["pe"] = lambda e: e.matmul(dpb[0:1, 0:1], lhsT=ones1[0:1, 0:1], rhs=ones1[0:1, 0:1], start=True, stop=True,
                                          skip_group_check=True)
            S.emit(dm)

        with contextlib.ExitStack() as sbk:
            if debug and os.environ.get("KSKIPB"):
                return nc
            S = Sched(nc, semst, "B")
            S.ignore_cost = True
            bank_i = [0]

            def nb():
                i = bank_i[0] % 8
                bank_i[0] += 1
                return i
            h2 = sb(sbk, "h2", [128, NT, D], BF16)
            g2b_s = sb(sbk, "g2b_s", [128, D], F32)
            bgT_s = sb(sbk, "bgT_s", [128, NE, 8], F32)
            buT_s = sb(sbk, "buT_s", [128, NE, 8], F32)
            bdn = sb(sbk, "bdn", [NE, D], F32)
            GTs = [sb(sbk, f"GTs{i}", [NE, 128], F32) for i in range(2)]
            slot = sb(sbk, "slot", [128, NT, NE], F32)
            Ghl = sb(sbk, "Ghl", [128, NT, NE, 2], BF16)
            Mb = sb(sbk, "Mb", [128, NT, NE], BF16)
            Mf = [sb(sbk, f"Mf{i}", [128, NE], F32) for i in range(2)]
            Gr = [sb(sbk, f"Gr{i}", [128, NE], F32) for i in range(2)]
            iota_s = sb(sbk, "iota_s", [128, 128], F32)
            ltf = sb(sbk, "ltf", [128, 128], F32)
            lts = sb(sbk, "lts", [128, 128], BF16)
            onesb = sb(sbk, "onesb", [128, 128], BF16)
            P = [sb(sbk, "P0", [128, NT, 128], BF16)] * 2
            PT = [sb(sbk, f"PT{i}", [128, 4, 512], BF16) for i in range(2)]
            gsel = [sb(sbk, f"gsel{i}", [128, 4], F32) for i in range(2)]
            xTs = sb(sbk, "xTs", [128, 8, 512], BF16)
            aT = sb(sbk, "aT", [128, 8, 512], BF16)
            ysc = [sb(sbk, f"ysc{i}", [128, D], BF16) for i in range(2)]
            gc = [sb(sbk, "gc0", [128, 512], F32)] * 2
            sg = [sb(sbk, "sg0", [128, 512], F32)] * 2
            uc = [sb(sbk, "uc0", [128, 512], F32)] * 2
            ones1b = sb(sbk, "ones1b", [1, 8], F32)
            for dst, src, k in ((bgT_s, bgT, "bgT"), (buT_s, buT, "buT"), (bdn, b_down, "bdn"), (g2b_s, g2b, "g2b"),
                                (iota_s, iotaj, "iota"), (ltf, ltstrict, "ltf")):
                S.add("sp", lambda e, dst=dst, src=src: e.dma_start(out=dst[:], in_=src), writes=[k], dma=True)
            S.add("dve", lambda e: e.memset(ones1b[:], 1.0), writes=["ones1b"])
            S.add("dve", lambda e: e.memset(onesb[:], 1.0), writes=["onesb"])
            S.add("dve", lambda e: e.tensor_copy(out=lts[:], in_=ltf[:]), reads=["ltf"], writes=["lts"])
            S.add("dve", lambda e: e.tensor_scalar(out=buT_s[:], in0=buT_s[:], scalar1=1.0, scalar2=None, op0=ALU.add),
                  reads=["buT"], writes=["buT"])
            wsl = [wa[:, s * 8192:(s + 1) * 8192].rearrange("p (k f) -> p k f", k=8) for s in range(3)]
            wsrc = []
            for e_ in range(NE):
                wsrc += [w_gate[e_], w_up[e_], w_down[e_]]

            def wload(mi):
                s = mi % 3
                S.add("pool", lambda e, mi=mi, s=s: e.dma_start(out=wsl[s], in_=wsrc[mi].rearrange("(k p) f -> p k f", p=128)),
                      writes=[("ws", s)], dma=True, cost=14.0)
            if ne_run > 0:
                wload(0); wload(1); wload(2)
            for ti in range(NT):
                i = ti % 2
                S.add("dve", lambda e, ti=ti: e.scalar_tensor_tensor(out=h2[:, ti, :], in0=x1[:, ti, :],
                                                                     scalar=rstd2[:, ti:ti + 1], in1=g2b_s[:],
                                                                     op0=ALU.mult, op1=ALU.mult),
                      reads=[("x1", ti), "g2b", ("rstd2", ti)], writes=[("h2", ti)])
                S.add("dve", lambda e, ti=ti: e.tensor_scalar(out=Mb[:, ti, :], in0=G[:, ti, :], scalar1=0.0, scalar2=None,
                                                              op0=ALU.is_gt),
                      reads=[("G", ti)], writes=[("Mb", ti)])
                S.add("dve", lambda e, ti=ti: e.tensor_copy(out=Ghl[:, ti, :, 0], in_=G[:, ti, :]),
                      reads=[("G", ti)], writes=[("Ghl", ti)])
                S.add("dve", lambda e, ti=ti, i=i: e.tensor_tensor(out=Gr[i][:], in0=G[:, ti, :], in1=Ghl[:, ti, :, 0],
                                                                   op=ALU.subtract),
                      reads=[("G", ti), ("Ghl", ti)], writes=[("Gr", i)])
                S.add("dve", lambda e, ti=ti, i=i: e.tensor_copy(out=Ghl[:, ti, :, 1], in_=Gr[i][:]),
                      reads=[("Gr", i)], writes=[("Ghl", ti)])
                b = nb()
                pb = banks[b]
                S.add("pe", lambda e, ti=ti, pb=pb: e.transpose(out=pb[0:NE, 0:128], in_=G[:, ti, :], identity=identf[:]),
                      reads=[("G", ti)], writes=[("bank", b)])
                S.add("act", lambda e, i=i, pb=pb: e.activation(out=GTs[i][:], in_=pb[0:NE, 0:128], func=AF.Copy),
                      reads=[("bank", b)], writes=[("GTs", i)])
                for hf in range(2):
                    b = nb()
                    pb = banks[b]
                    S.add("pe", lambda e, i=i, hf=hf, pb=pb: e.matmul(pb[:, :], lhsT=GTs[i][:], rhs=bdn[:, hf * 512:(hf + 1) * 512],
                                                                      start=True, stop=True),
                          reads=[("GTs", i), "bdn"], writes=[("bank", b)])
                    S.add("dve", lambda e, ti=ti, hf=hf, pb=pb: e.tensor_tensor(
                        out=x1[:, ti, hf * 512:(hf + 1) * 512], in0=pb[:, :], in1=x1[:, ti, hf * 512:(hf + 1) * 512], op=ALU.add),
                        reads=[("bank", b), ("x1", ti)], writes=[("x1", ti)])
            for ti in range(NT):
                i = ti % 2
                prev = list(range(ti % 4, ti, 4))
                b = nb()
                pb = banks[b]

                def rk(e, ti=ti, prev=prev, pb=pb):
                    ins = e.matmul(pb[:, 0:NE], lhsT=lts[:], rhs=Mb[:, ti, :], start=True, stop=(not prev))
                    for tj in prev:
                        ins = e.matmul(pb[:, 0:NE], lhsT=onesb[:], rhs=Mb[:, tj, :], start=False, stop=(tj == prev[-1]))
                    return ins
                S.add("pe", rk, reads=[("Mb", tj) for tj in prev + [ti]] + ["lts", "onesb"], writes=[("bank", b)])
                S.add("dve", lambda e, ti=ti, i=i: e.tensor_scalar(out=Mf[i][:], in0=G[:, ti, :], scalar1=0.0, scalar2=None,
                                                                   op0=ALU.is_gt),
                      reads=[("G", ti)], writes=[("Mf", i)])
                S.add("dve", lambda e, ti=ti, i=i, pb=pb: e.scalar_tensor_tensor(out=slot[:, ti, :], in0=pb[:, 0:NE], scalar=1.0,
                                                                                 in1=Mf[i][:], op0=ALU.add, op1=ALU.mult),
                      reads=[("bank", b), ("Mf", i)], writes=[("slot", ti)])
                S.add("dve", lambda e, ti=ti: e.tensor_scalar(out=slot[:, ti, :], in0=slot[:, ti, :], scalar1=-1.0, scalar2=None,
                                                              op0=ALU.add),
                      reads=[("slot", ti)], writes=[("slot", ti)])
            H2 = [("h2", ti) for ti in range(NT)]
            for e_ in range(ne_run):
                sG, sU, sD = 0, 1, 2
                pi = e_ % 2
                Pe, PTe, gse = P[0], PT[pi], gsel[pi]
                for ti in range(NT):
                    S.add("dve", lambda e, ti=ti, e_=e_, Pe=Pe: e.tensor_scalar(out=Pe[:, ti, :], in0=iota_s[:],
                                                                               scalar1=slot[:, ti, e_:e_ + 1], scalar2=None,
                                                                               op0=ALU.is_equal),
                          reads=["iota", ("slot", ti)], writes=[("P", 0, ti % 4)], cost=0.2)
                for kc in range(8):
                    b = nb()
                    pb = banks[b]

                    def ga(e, kc=kc, pb=pb, Pe=Pe):
                        for g in range(4):
                            for r in range(4):
                                ins = e.matmul(pb[:, g * 128:(g + 1) * 128], lhsT=h2[:, 4 * r + g, kc * 128:(kc + 1) * 128],
                                               rhs=Pe[:, 4 * r + g, :], start=(r == 0), stop=(r == 3), skip_group_check=True)
                        return ins
                    S.add("pe", ga, reads=H2 + [("P", 0, g) for g in range(4)], writes=[("bank", b)], cost=1.6)
                    if kc % 2:
                        S.add("act", lambda e, kc=kc, pb=pb: e.activation(out=xTs[:, kc, :], in_=pb[:, :], func=AF.Copy),
                              reads=[("bank", b)], writes=[("xTs", kc)])
                    else:
                        S.add("dve", lambda e, kc=kc, pb=pb: e.tensor_copy(out=xTs[:, kc, :], in_=pb[:, :]),
                              reads=[("bank", b)], writes=[("xTs", kc)])
                for g in range(4):
                    b = nb()
                    pbb = banks[b][:].bitcast(BF16)

                    def trp(e, g=g, pbb=pbb, Pe=Pe):
                        for r in range(4):
                            ins = e.transpose(out=pbb[:, r * 128:(r + 1) * 128], in_=Pe[:, 4 * r + g, :], identity=identb[:])
                        return ins
                    S.add("pe", trp, reads=[("P", 0, g)], writes=[("bank", b)])
                    S.add("act", lambda e, g=g, pbb=pbb, PTe=PTe: e.activation(out=PTe[:, g, :], in_=pbb[:, 0:512], func=AF.Copy),
                          reads=[("bank", b)], writes=[("PT", pi, g)])
                b = nb()
                pbg = banks[b]

                def gs(e, pbg=pbg, Pe=Pe, e_=e_):
                    for g in range(4):
                        for r in range(4):
                            ins = e.matmul(pbg[:, 2 * g:2 * g + 2], lhsT=Pe[:, 4 * r + g, :], rhs=Ghl[:, 4 * r + g, e_, :],
                                           start=(r == 0), stop=(r == 3), skip_group_check=True)
                    return ins
                S.add("pe", gs, reads=[("P", 0, g) for g in range(4)] + [("Ghl", ti) for ti in range(NT)], writes=[("bank", b)])
                S.add("dve", lambda e, pbg=pbg, gse=gse: e.tensor_reduce(out=gse[:], in_=pbg[:, 0:8].rearrange("p (g two) -> p g two", two=2),
                                                                        axis=AX.X, op=ALU.add),
                      reads=[("bank", b)], writes=[("gsel", pi)])
                for fc in range(8):
                    j = 0
                    bg_, bu_ = nb(), nb()
                    pg, pu_ = banks[bg_], banks[bu_]

                    def mmg(e, fc=fc, pg=pg):
                        for k in range(8):
                            ins = e.matmul(pg[:, :], lhsT=wsl[sG][:, k, fc * 128:(fc + 1) * 128], rhs=xTs[:, k, :],
                                           start=(k == 0), stop=(k == 7))
                        return ins

                    def mmu(e, fc=fc, pu_=pu_):
                        for k in range(8):
                            ins = e.matmul(pu_[:, :], lhsT=wsl[sU][:, k, fc * 128:(fc + 1) * 128], rhs=xTs[:, k, :],
                                           start=(k == 0), stop=(k == 7))
                        return ins
                    XT = [("xTs", k) for k in range(8)]
                    S.add("pe", mmg, reads=[("ws", sG)] + XT, writes=[("bank", bg_)], cost=2.5)
                    S.add("pe", mmu, reads=[("ws", sU)] + XT, writes=[("bank", bu_)], cost=2.5)
                    S.add("dve", lambda e, fc=fc, e_=e_, pg=pg, j=j: e.tensor_scalar(
                        out=gc[j][:], in0=pg[:, :], scalar1=bgT_s[:, e_, fc:fc + 1], scalar2=7.0, op0=ALU.add, op1=ALU.min),
                        reads=[("bank", bg_), "bgT"], writes=[("gc", j)], cost=0.6)
                    S.add("act", lambda e, j=j: e.activation(out=sg[j][:], in_=gc[j][:], func=AF.Sigmoid, scale=1.702),
                          reads=[("gc", j)], writes=[("sg", j)], cost=0.6)
                    S.add("act", lambda e, fc=fc, e_=e_, pu_=pu_, j=j: e.activation(out=uc[j][:], in_=pu_[:, :], func=AF.Identity,
                                                                                    bias=buT_s[:, e_, fc:fc + 1]),
                          reads=[("bank", bu_), "buT"], writes=[("uc", j)], cost=0.7)
                    S.add("dve", lambda e, j=j: e.tensor_scalar(out=uc[j][:], in0=uc[j][:], scalar1=-6.0, scalar2=8.0,
                                                                op0=ALU.max, op1=ALU.min),
                          reads=[("uc", j)], writes=[("uc", j)], cost=0.6)
                    S.add("dve", lambda e, j=j: e.tensor_tensor(out=gc[j][:], in0=gc[j][:], in1=sg[j][:], op=ALU.mult),
                          reads=[("gc", j), ("sg", j)], writes=[("gc", j)], cost=0.6)
                    S.add("dve", lambda e, j=j, fc=fc: e.tensor_tensor(out=aT[:, fc, :], in0=gc[j][:], in1=uc[j][:], op=ALU.mult),
                          reads=[("gc", j), ("uc", j)], writes=[("aT", fc)], cost=0.6)
                if e_ + 1 < ne_run:
                    wload(3 * (e_ + 1)); wload(3 * (e_ + 1) + 1)
                AT = [("aT", k) for k in range(8)]
                for g in range(4):
                    yi = g % 2
                    for hf in range(2):
                        b = nb()
                        pb = banks[b]

                        def mmd(e, g=g, hf=hf, pb=pb):
                            for k in range(8):
                                ins = e.matmul(pb[:, :], lhsT=aT[:, k, g * 128:(g + 1) * 128],
                                               rhs=wsl[sD][:, k, hf * 512:(hf + 1) * 512], start=(k == 0), stop=(k == 7))
                            return ins
                        S.add("pe", mmd, reads=AT + [("ws", sD)], writes=[("bank", b)], cost=2.5)
                        S.add("act", lambda e, g=g, hf=hf, pb=pb, yi=yi, gse=gse: e.activation(
                            out=ysc[yi][:, hf * 512:(hf + 1) * 512], in_=pb[:, :], func=AF.Copy, scale=gse[:, g:g + 1]),
                            reads=[("bank", b), ("gsel", pi)], writes=[("ysc", yi)], cost=0.6)
                    for r in range(4):
                        ti = 4 * r + g
                        for hf in range(2):
                            b = nb()
                            pb = banks[b]
                            S.add("pe", lambda e, g=g, r=r, hf=hf, pb=pb, yi=yi, PTe=PTe: e.matmul(
                                pb[:, :], lhsT=PTe[:, g, r * 128:(r + 1) * 128], rhs=ysc[yi][:, hf * 512:(hf + 1) * 512],
                                start=True, stop=True),
                                reads=[("PT", pi, g), ("ysc", yi)], writes=[("bank", b)], cost=0.32)
                            S.add("dve", lambda e, ti=ti, hf=hf, pb=pb: e.tensor_tensor(
                                out=x1[:, ti, hf * 512:(hf + 1) * 512], in0=pb[:, :], in1=x1[:, ti, hf * 512:(hf + 1) * 512],
                                op=ALU.add),
                                reads=[("bank", b), ("x1", ti)], writes=[("x1", ti)], cost=0.6)
                if e_ + 1 < ne_run:
                    wload(3 * (e_ + 1) + 2)
            for ti in range(NT):
                S.add("act", lambda e, ti=ti: e.activation(out=h2[:, ti, :], in_=x1[:, ti, :], func=AF.Square,
                                                           accum_out=rstd2[:, ti:ti + 1]),
                      reads=[("x1", ti)], writes=[("h2", ti), ("rstd2", ti)])
                S.add("act", lambda e, ti=ti: e.activation(out=rstd2[:, ti:ti + 1], in_=rstd2[:, ti:ti + 1], func=AF.Ln,
                                                           scale=1.0 / D, bias=EPS),
                      reads=[("rstd2", ti)], writes=[("rstd2", ti)])
                S.add("act", lambda e, ti=ti: e.activation(out=rstd2[:, ti:ti + 1], in_=rstd2[:, ti:ti + 1], func=AF.Exp,
                                                           scale=-0.5),
                      reads=[("rstd2", ti)], writes=[("rstd2", ti)])
                S.add("dve", lambda e, ti=ti: e.scalar_tensor_tensor(out=x1[:, ti, :], in0=x1[:, ti, :],
                                                                     scalar=rstd2[:, ti:ti + 1], in1=gfb_s[:],
                                                                     op0=ALU.mult, op1=ALU.mult),
                      reads=[("x1", ti), ("rstd2", ti), "gfb"], writes=[("x1", ti)])
                S.add("sp", lambda e, ti=ti: e.dma_start(out=out[ti * 128:(ti + 1) * 128, :], in_=x1[:, ti, :]),
                      reads=[("x1", ti)], dma=True)
            dm = dummies()
            dpb = banks[0]
            dm["pe"] = lambda e: e.matmul(dpb[0:1, 0:1], lhsT=ones1b[0:1, 0:1], rhs=ones1b[0:1, 0:1], start=True, stop=True,
                                          skip_group_check=True)
            S.emit(dm)
    return nc


_NC = None


def _prep(inputs):
    f = lambda a: np.ascontiguousarray(np.asarray(a, dtype=np.float32))
    x = f(inputs["x"])
    rep = lambda v, n=128: f(np.broadcast_to(np.asarray(v, np.float32).reshape(1, -1), (n, np.asarray(v).size)))
    col = lambda v: f(np.asarray(v, np.float32).reshape(8, 128).T)
    common = {
        "w_in": f(inputs["w_in"][0]), "w_out": f(inputs["w_out"][0]), "pool_w": f(inputs["pool_w"][0]),
        "g1c": col(inputs["norm1_g"][0]), "g2c": col(inputs["norm2_g"][0]), "gfb": rep(inputs["normf_g"]),
        "convT": f(np.asarray(inputs["conv_w"][0], np.float32).T.reshape(8, 128, 4).transpose(1, 0, 2)),
        "igb": f(np.tile(np.asarray(inputs["ig_b"][0], np.float32), 32).reshape(128, 1)),
        "fgbn": f(np.tile(np.asarray(inputs["fg_b"][0], np.float32), 32).reshape(128, 1)),
        "hngb": rep(inputs["head_norm_g"][0]),
        "pscale": f(np.asarray(inputs["pool_scale"][0], np.float32).reshape(4, 128).T),
        "w_router": f(inputs["w_router"][0]), "brb": rep(inputs["b_router"][0]),
        "w_gate": f(inputs["w_gate"][0]), "w_up": f(inputs["w_up"][0]), "w_down": f(inputs["w_down"][0]),
        "bgT": f(np.asarray(inputs["b_gate"][0], np.float32).reshape(NE, 8, 128).transpose(2, 0, 1)),
        "buT": f(np.asarray(inputs["b_up"][0], np.float32).reshape(NE, 8, 128).transpose(2, 0, 1)),
        "b_down": f(inputs["b_down"][0]),
        "ident": np.eye(128, dtype=np.float32),
        "maskrl": np.triu(np.ones((128, 128), np.float32)),
        "g2b": rep(inputs["norm2_g"][0]),
        "iotaj": np.ascontiguousarray(np.broadcast_to(np.arange(128, dtype=np.float32)[None, :], (128, 128))),
        "ltstrict": np.triu(np.ones((128, 128), np.float32), 1),
    }
    rm = np.ones((128, 128), np.float32)
    rm[:, 0] = 0.0
    common["resetm"] = rm
    corr_even = np.ones((128, 4, 16), np.float32)
    for g, w in enumerate((2, 4, 8, 16)):
        t = np.arange(16)
        corr_even[:, g, :] = (w / np.minimum(t + 1, w)).astype(np.float32)[None, :]
    maps = []
    for c in range(8):
        b, half = c // 2, c % 2
        m = dict(common)
        m["xo"] = f(x[b, half * TOK:(half + 1) * TOK])
        m["xp"] = f(x[b, 0:TOK]) if half else np.zeros((TOK, D), np.float32)
        m["flag"] = np.full((128, 1), float(half), np.float32)
        m["corr"] = np.ones((128, 4, 16), np.float32) if half else corr_even
        maps.append(m)
    return maps


def kernel(**inputs):
    global _NC
    debug = bool(os.environ.get("KDEBUG"))
    nc = build(debug)
    maps = _prep(inputs)
    res = run_bass_kernel_spmd(nc, maps, core_ids=list(range(8)))
    outp = np.zeros((4, 4096, D), np.float32)
    for c in range(8):
        outp[c // 2, (c % 2) * TOK:(c % 2 + 1) * TOK] = res.results[c]["out"]
    return outp
```

```python
import contextlib
import math
import os
import numpy as np
import concourse.bass as bass
import concourse.mybir as mybir
from concourse.alu_op_type import AluOpType as ALU
from concourse.bass_utils import run_bass_kernel_spmd

F32 = mybir.dt.float32
BF16 = mybir.dt.bfloat16
AF = mybir.ActivationFunctionType
AX = mybir.AxisListType

D = 1024
TOK = 2048
NT = TOK // 128
NE = 32
NIN = 2568
EPS = 1e-5
LNSC = math.log(128.0 ** -0.5)


class Op:
    __slots__ = ("eng", "fn", "deps", "odeps", "signal", "sem", "val", "is_dma", "idx", "cost", "lat", "fin", "nrem", "users")


class Sched:
    ENG = ("pe", "dve", "act", "pool", "sp")

    def __init__(self, nc, semstack, tag, ndma_sems=6):
        self.nc, self.semstack, self.tag = nc, semstack, tag
        self.ops = {e: [] for e in self.ENG}
        self.last_w, self.readers = {}, {}
        self.ndma = ndma_sems
        self.dma_count = {e: 0 for e in self.ENG}
        self.dma_prev = {}
        self.n = 0
        self.stopped = False
        self.ignore_cost = False

    COST = {"pe": 0.55, "dve": 0.35, "act": 0.35, "pool": 1.0, "sp": 0.1}

    def add(self, eng, fn, reads=(), writes=(), dma=False, cost=None):
        op = Op()
        if self.stopped:
            op.deps, op.signal, op.is_dma, op.eng = [], False, dma, eng
            return op
        if self.ignore_cost:
            cost = None
        op.cost = cost if cost is not None else (0.1 if dma else self.COST[eng])
        op.lat = (cost if cost is not None else 3.0) if dma else op.cost
        op.eng, op.fn, op.is_dma, op.signal = eng, fn, dma, False
        op.idx = self.n
        self.n += 1
        deps = []
        for r in reads:
            w = self.last_w.get(r)
            if w is not None:
                deps.append(w)
            if isinstance(r, tuple) and r[0] == "bank":
                deps.extend(o for o in self.readers.get(r, ()) if o.eng != eng)
        for w_ in writes:
            w = self.last_w.get(w_)
            if w is not None:
                deps.append(w)
            deps.extend(self.readers.get(w_, ()))
        for r in reads:
            self.readers.setdefault(r, []).append(op)
        for w_ in writes:
            self.last_w[w_] = op
            self.readers[w_] = []
        if dma:
            k = self.dma_count[eng]
            self.dma_count[eng] += 1
            slot = (eng, k % self.ndma)
            op.sem = slot
            prev = self.dma_prev.get(slot)
            if prev is not None:
                deps.append(prev)
            self.dma_prev[slot] = op
            op.signal = True
        ded, oded, seen = [], [], set()
        for d in deps:
            if d is op or id(d) in seen:
                continue
            seen.add(id(d))
            oded.append(d)
            if (not d.is_dma) and d.eng == eng and eng == "pe":
                continue
            ded.append(d)
        op.deps = ded
        op.odeps = oded
        self.ops[eng].append(op)
        return op

    def reorder(self):
        import heapq
        allops = [op for e in self.ENG for op in self.ops[e]]
        for op in allops:
            op.users, op.nrem, op.fin = [], len(op.odeps), None
        for op in allops:
            for d in op.odeps:
                d.users.append(op)
        SYNC = 0.6
        fut = {e: [] for e in self.ENG}
        now = {e: [] for e in self.ENG}
        t = {e: 0.0 for e in self.ENG}
        new = {e: [] for e in self.ENG}

        def push(op):
            r = 0.0
            for d in op.odeps:
                f = d.fin + (0.0 if (d.eng == op.eng and not d.is_dma) else SYNC)
                if f > r:
                    r = f
            heapq.heappush(fut[op.eng], (r, op.idx, op))
        for op in allops:
            if op.nrem == 0:
                push(op)
        left = len(allops)
        while left:
            best = None
            for e in self.ENG:
                while fut[e] and fut[e][0][0] <= t[e]:
                    r, i, op = heapq.heappop(fut[e])
                    heapq.heappush(now[e], (i, op))
                if now[e]:
                    st = t[e]
                elif fut[e]:
                    st = fut[e][0][0]
                else:
                    continue
                if best is None or st < best[0]:
                    best = (st, e)
            st, e = best
            if now[e]:
                i, op = heapq.heappop(now[e])
            else:
                r, i, op = heapq.heappop(fut[e])
            new[e].append(op)
            t[e] = st + op.cost
            op.fin = st + op.lat
            left -= 1
            for u in op.users:
                u.nrem -= 1
                if u.nrem == 0:
                    push(u)
        self.ops = new
        self.est = max(t.values())

    def emit(self, dummies, final_waits=()):
        nc = self.nc
        if os.environ.get("KNOSCHED") is None:
            self.reorder()
        for e, fn in dummies.items():
            o = self.add(e, fn, writes=[("bank", 0)] if e == "pe" else ())
            o.signal = True
        for e in self.ENG:
            for op in self.ops[e]:
                for d in op.deps:
                    d.signal = True
        esem = {e: self.semstack.enter_context(nc.semaphore(f"s{self.tag}_{e}")) for e in self.ENG}
        dsem = {}
        for e in self.ENG:
            for i in range(min(self.ndma, self.dma_count[e])):
                dsem[(e, i)] = self.semstack.enter_context(nc.semaphore(f"d{self.tag}_{e}{i}"))
        finals = {}
        for e in self.ENG:
            c, dc = 0, {}
            for op in self.ops[e]:
                if op.is_dma:
                    dc[op.sem] = dc.get(op.sem, 0) + 16
                    op.val = dc[op.sem]
                    op.sem = dsem[op.sem]
                    finals[id(op.sem)] = (op.sem, op.val)
                elif op.signal:
                    c += 1
                    op.val = c
                    op.sem = esem[e]
                    finals[id(op.sem)] = (op.sem, op.val)
        with nc.Block() as block:
            def run(e, eng):
                waited = {}
                for op in self.ops[e]:
                    for d in op.deps:
                        key = id(d.sem)
                        if waited.get(key, 0) >= d.val:
                            continue
                        waited[key] = d.val
                        eng.wait_ge(d.sem, d.val)
                    ins = op.fn(eng)
                    if op.signal:
                        ins.then_inc(op.sem, 16 if op.is_dma else 1)
                for sem, val in finals.values():
                    if waited.get(id(sem), 0) < val:
                        eng.wait_ge(sem, val)

            @block.tensor
            def _(eng):
                run("pe", eng)

            @block.vector
            def _(eng):
                run("dve", eng)

            @block.scalar
            def _(eng):
                run("act", eng)

            @block.gpsimd
            def _(eng):
                run("pool", eng)

            @block.sync
            def _(eng):
                run("sp", eng)


def build(debug=False):
    nc = bass.Bass("TRN2", target_bir_lowering=False)

    def din(name, shape):
        return nc.dram_tensor(name, list(shape), F32, kind="ExternalInput").ap()

    xo = din("xo", [TOK, D]); xp = din("xp", [TOK, D])
    w_in = din("w_in", [D, NIN]); w_out = din("w_out", [D, D]); pool_w = din("pool_w", [4, 128, 128])
    g1c = din("g1c", [128, 8]); g2c = din("g2c", [128, 8]); gfb = din("gfb", [128, D])
    convT = din("convT", [128, 8, 4]); igb = din("igb", [128, 1]); fgbn = din("fgbn", [128, 1])
    hngb = din("hngb", [128, 512]); pscale = din("pscale", [128, 4])
    w_router = din("w_router", [D, NE]); brb = din("brb", [128, NE])
    w_gate = din("w_gate", [NE, D, D]); w_up = din("w_up", [NE, D, D]); w_down = din("w_down", [NE, D, D])
    bgT = din("bgT", [128, NE, 8]); buT = din("buT", [128, NE, 8]); b_down = din("b_down", [NE, D])
    ident = din("ident", [128, 128]); maskrl = din("maskrl", [128, 128]); corr = din("corr", [128, 4, 16])
    flag = din("flag", [128, 1]); resetm = din("resetm", [128, 128])
    g2b = din("g2b", [128, D]); iotaj = din("iotaj", [128, 128]); ltstrict = din("ltstrict", [128, 128])
    out = nc.dram_tensor("out", [TOK, D], F32, kind="ExternalOutput").ap()
    dbg = nc.dram_tensor("dbg", [TOK, D], F32, kind="ExternalOutput").ap() if debug else None
    dbgG = nc.dram_tensor("dbgG", [TOK, NE], F32, kind="ExternalOutput").ap() if debug else None
    ne_run = int(os.environ.get("KNE", NE)) if debug else NE

    with contextlib.ExitStack() as st, contextlib.ExitStack() as semst:
        def sb(stack, name, shape, dt):
            return stack.enter_context(nc.sbuf_tensor(name, list(shape), dt))

        def ps(stack, name, shape, dt):
            return stack.enter_context(nc.psum_tensor(name, list(shape), dt))

        x1 = sb(st, "x1", [128, NT, D], F32)
        wa = sb(st, "wa", [128, 3 * 8192], BF16)
        G = sb(st, "G", [128, NT, NE], F32)
        rstd2 = sb(st, "rstd2", [128, NT], F32)
        identf = sb(st, "identf", [128, 128], F32)
        identb = sb(st, "identb", [128, 128], BF16)
        gfb_s = sb(st, "gfb_s", [128, D], F32)
        g2c_s = sb(st, "g2c_s", [128, 8], F32)
        dum = sb(st, "dum", [128, 8], F32)
        banks = [ps(st, f"bank{i}", [128, 512], F32) for i in range(8)]

        def dummies():
            return {
                "dve": lambda e: e.memset(dum[:, 0:1], 0.0),
                "act": lambda e: e.activation(out=dum[:, 1:2], in_=identf[:, 0:1], func=AF.Copy),
                "pool": lambda e: e.memset(dum[:, 3:4], 0.0),
            }

        with contextlib.ExitStack() as sa:
            S = Sched(nc, semst, "A")
            bank_i = [0]
            reserved = set()

            def nb():
                while True:
                    i = bank_i[0] % 8
                    bank_i[0] += 1
                    if i not in reserved:
                        return i

            win = wa[:, 0:8 * NIN].rearrange("p (k f) -> p k f", k=8)
            wout = sb(sa, "wout", [128, 8, D], BF16)
            dconv = sb(sa, "dconv", [128, 8, 4, 128], BF16)
            convT_s = sb(sa, "convT_s", [128, 8, 4], F32)
            g1c_s = sb(sa, "g1c_s", [128, 8], F32)
            wg = sb(sa, "wg", [128, 8, 8], BF16)
            hngb_s = sb(sa, "hngb_s", [128, 512], F32)
            poolw = sb(sa, "poolw", [128, 4, 128], BF16)
            pscale_s = sb(sa, "pscale_s", [128, 4], F32)
            wr = sb(sa, "wr", [128, 8, NE], F32)
            brb_s = sb(sa, "brb_s", [128, NE], F32)
            maskb = sb(sa, "maskb", [128, 128], BF16)
            maskf = sb(sa, "maskf", [128, 128], F32)
            corr_s = sb(sa, "corr_s", [128, 4, 16], F32)
            flag_s = sb(sa, "flag_s", [128, 1], F32)
            igb_s = sb(sa, "igb_s", [128, 1], F32)
            fgbn_s = sb(sa, "fgbn_s", [128, 1], F32)
            resetm_s = sb(sa, "resetm_s", [128, 128], F32)
            ones1 = sb(sa, "ones1", [1, 128], F32)
            rstd1 = sb(sa, "rstd1", [128, 32], F32)
            ssq = sb(sa, "ssq", [128, 48], F32)
            xin = [sb(sa, "xin0", [128, D], F32)] * 2
            hs = [sb(sa, f"hs{i}", [128, D], BF16) for i in range(2)]
            hT = [sb(sa, f"hT{i}", [128, 8, 128], BF16) for i in range(2)]
            gcol = sb(sa, "gcol", [128, 8], F32)
            grow = sb(sa, "grow", [1, 12, 128], F32)
            tokT = sb(sa, "tokT", [128, 3, 128], F32)
            bcs = sb(sa, "bcs", [128, 3, 128], F32)
            ubc = [sb(sa, f"ubc{i}", [128, 4, 3 + 128], BF16) for i in range(2)]
            halo_c = sb(sa, "halo_c", [128, 8, 3], BF16)
            qkT = [sb(sa, f"qkT{i}", [128, 8, 128], BF16) for i in range(2)]
            kTok = [sb(sa, f"kTok{i}", [128, 4, 128], BF16) for i in range(2)]
            v1 = [sb(sa, f"v1{i}", [128, 4, 129], BF16) for i in range(2)]
            osig = [sb(sa, f"osig{i}", [128, 512], BF16) for i in range(2)]
            Cst = sb(sa, "Cst", [128, 4, 129], F32)
            Ctmp = sb(sa, "Ctmp", [128, 4, 129], F32)
            Cs = sb(sa, "Cs", [128, 4, 129], BF16)
            sTp = [sb(sa, f"sTp{i}", [128, 128], BF16) for i in range(2)]
            wv = [sb(sa, f"wv{i}", [128, 129], BF16) for i in range(2)]
            dmx = sb(sa, "dmx", [128, 8], F32)
            hm = sb(sa, "hm", [128, 4, 128], F32)
            bst = sb(sa, "bst", [128, 4, 6], F32)
            mv = sb(sa, "mv", [128, 4, 2], F32)
            lnr = sb(sa, "lnr", [128, 4], F32)
            ym = sb(sa, "ym", [128, 512], BF16)
            yT = [sb(sa, f"yT{i}", [128, 8, 128], BF16) for i in range(2)]
            pu = [sb(sa, f"pu{i}", [128, 16 + 128], F32) for i in range(4)]
            phalo = sb(sa, "phalo", [128, 4, 16], F32)
            pooledT = sb(sa, "pooledT", [128, 128], BF16)
            h2f = sb(sa, "h2f", [128, D], F32)
            ymf = h2f[:, 0:512]
            gm = [hm[:, 0, :], hm[:, 1, :], hm[:, 2, :], hm[:, 3, :], h2f[:, 0:128], h2f[:, 128:256]]
            h2Tf = sb(sa, "h2Tf", [128, 8, 128], F32)
            gsb = h2Tf[:, 0:2, :]
            lg = sb(sa, "lg", [128, NE], F32)
            top8 = sb(sa, "top8", [128, 8], F32)
            rt = sb(sa, "rt", [128, 4, NE], F32)
            rsm = sb(sa, "rsm", [128, 4], F32)

            klim = float(os.environ.get("KSTAGE", "99")) if debug else 99

            def ckpt(k):
                if klim <= k:
                    S.stopped = True
            def ld(eng, dst, src, wkey, rk=()):
                return S.add(eng, lambda e: e.dma_start(out=dst, in_=src), reads=rk, writes=[wkey], dma=True)

            ld("sp", identf[:], ident, "identf")
            S.add("dve", lambda e: e.tensor_copy(out=identb[:], in_=identf[:]), reads=["identf"], writes=["identb"])
            ld("sp", maskf[:], maskrl, "maskf")
            S.add("dve", lambda e: e.tensor_copy(out=maskb[:], in_=maskf[:]), reads=["maskf"], writes=["maskb"])
            for dst, src, k in ((g1c_s, g1c, "g1c"), (g2c_s, g2c, "g2c"), (gfb_s, gfb, "gfb"), (convT_s, convT, "convT"),
                                (hngb_s, hngb, "hngb"), (pscale_s, pscale, "pscale"), (brb_s, brb, "brb"),
                                (corr_s, corr, "corr"), (flag_s, flag, "flag"), (igb_s, igb, "igb"),
                                (fgbn_s, fgbn, "fgbn"), (resetm_s, resetm, "resetm")):
                ld("sp", dst[:], src, k)
            ld("sp", wr[:], w_router.rearrange("(k p) e -> p k e", p=128), "wr")
            S.add("dve", lambda e: e.tensor_scalar(out=fgbn_s[:], in0=fgbn_s[:], scalar1=-1.0, scalar2=None, op0=ALU.mult),
                  reads=["fgbn"], writes=["fgbn"])
            w_in_v = w_in.rearrange("(k p) f -> p k f", p=128)
            ld("pool", wg[:], w_in_v[:, :, 2048:2056], "wg")
            for k in range(8):
                S.add("dve", lambda e, k=k: e.tensor_scalar(out=wg[:, k, :], in0=wg[:, k, :], scalar1=g1c_s[:, k:k + 1],
                                                            scalar2=None, op0=ALU.mult),
                      reads=["g1c", "wg"], writes=["wg"], cost=0.1)
            for k in range(8):
                ld("pool", win[:, k, :], w_in_v[:, k, :], ("wa", k))
            ld("pool", wout[:], w_out.rearrange("(k p) f -> p k f", p=128), "wout")
            ld("pool", poolw[:], pool_w.rearrange("g c d -> c g d"), "poolw")
            for k in range(8):
                if k % 2:
                    S.add("dve", lambda e, k=k: e.tensor_scalar(out=win[:, k, :], in0=win[:, k, :], scalar1=g1c_s[:, k:k + 1],
                                                                scalar2=None, op0=ALU.mult),
                          reads=["g1c", ("wa", k)], writes=[("wa", k)])
                else:
                    S.add("act", lambda e, k=k: e.activation(out=win[:, k, :], in_=win[:, k, :], func=AF.Copy,
                                                             scale=g1c_s[:, k:k + 1]),
                          reads=["g1c", ("wa", k)], writes=[("wa", k)])
            for k in range(8):
                for j in range(4):
                    S.add("dve", lambda e, k=k, j=j: e.tensor_scalar(out=dconv[:, k, j, :], in0=identf[:],
                                                                       scalar1=convT_s[:, k, j:j + 1], scalar2=None,
                                                                       op0=ALU.mult),
                          reads=["identf", "convT"], writes=[("dconv", k)])
            S.add("dve", lambda e: e.memset(ones1[:], 1.0), writes=["ones1"])
            S.add("dve", lambda e: e.memset(Cst[:], 0.0), writes=[("Cst", h) for h in range(4)])
            S.add("dve", lambda e: e.memset(halo_c[:], 0.0), writes=[("halo_c", 0), ("halo_c", 1)])
            S.add("dve", lambda e: e.memset(phalo[:], 0.0), writes=[("phalo", g) for g in range(4)])
            S.add("dve", lambda e: e.memset(v1[0][:], 1.0), writes=[("v1", 0)])
            S.add("dve", lambda e: e.memset(v1[1][:], 1.0), writes=[("v1", 1)])
            WA = [("wa", k) for k in range(8)]

            ckpt(1)
            cnt = [0]

            def norm_T(ci, first):
                i = cnt[0] % 2
                cnt[0] += 1
                own = ci >= 16
                if own:
                    xt, xkey = x1[:, ci - 16, :], ("x1", ci - 16)
                elif ci % 2 == 0:
                    xt, xkey = xin[0][:], ("xin", 0)
                else:
                    xt, xkey = h2f[:], "h2f"
                if first or not own:
                    src = (xo if own else xp)[(ci % 16) * 128:(ci % 16 + 1) * 128, :]
                    S.add("sp", lambda e: e.dma_start(out=xt, in_=src), writes=[xkey], dma=True)
                if first:
                    S.add("act", lambda e: e.activation(out=hs[i][:], in_=xt, func=AF.Square,
                                                        accum_out=ssq[:, ci:ci + 1]),
                          reads=[xkey], writes=[("hs", i), ("ssq", ci)], cost=1.0)
                    S.add("act", lambda e: e.activation(out=ssq[:, ci:ci + 1], in_=ssq[:, ci:ci + 1], func=AF.Ln,
                                                        scale=1.0 / D, bias=EPS),
                          reads=[("ssq", ci)], writes=[("ssq", ci)])
                    S.add("act", lambda e: e.activation(out=rstd1[:, ci:ci + 1], in_=ssq[:, ci:ci + 1], func=AF.Exp,
                                                        scale=-0.5),
                          reads=[("ssq", ci)], writes=[("rstd1", ci)])
                if first:
                    S.add("dve", lambda e: e.tensor_scalar(out=hs[i][:], in0=xt, scalar1=rstd1[:, ci:ci + 1],
                                                           scalar2=None, op0=ALU.mult),
                          reads=[xkey, ("rstd1", ci)], writes=[("hs", i)], cost=1.1)
                else:
                    S.add("act", lambda e: e.activation(out=hs[i][:], in_=xt, func=AF.Copy, scale=rstd1[:, ci:ci + 1]),
                          reads=[xkey, ("rstd1", ci)], writes=[("hs", i)], cost=1.1)
                b = nb()
                pb = banks[b][:].bitcast(BF16)

                def tr(e):
                    for k in range(8):
                        ins = e.transpose(out=pb[:, k * 128:(k + 1) * 128], in_=hs[i][:, k * 128:(k + 1) * 128],
                                          identity=identb[:])
                    return ins
                S.add("pe", tr, reads=[("hs", i), "identb"], writes=[("bank", b)], cost=0.8)
                S.add("dve", lambda e: e.tensor_copy(out=hT[i][:].rearrange("p k t -> p (k t)"), in_=pb[:, 0:1024]),
                      reads=[("bank", b)], writes=[("hT", i)], cost=0.6)
                return i

            gb = nb()
            reserved.add(gb)
            gall = banks[gb][:, 0:256].rearrange("p (c a) -> p c a", a=8)
            for ci in range(32):
                i = norm_T(ci, True)

                def gm_(e, i=i, ci=ci):
                    for k in range(8):
                        ins = e.matmul(gall[:, ci, :], lhsT=hT[i][:, k, :], rhs=wg[:, k, :],
                                       start=(k == 0), stop=(k == 7), skip_group_check=True)
                    return ins
                S.add("pe", gm_, reads=[("hT", i), "wg"], writes=[("bank", gb)])
            ckpt(2)
            for a in range(2):
                S.add("dve", lambda e, a=a: e.tensor_copy(out=gsb[:, a, :].rearrange("p (c h) -> p c h", h=4),
                                                          in_=gall[:, :, a * 4:(a + 1) * 4]),
                      reads=[("bank", gb)], writes=[("gsb", a)])
            reserved.discard(gb)
            tb = nb()
            tps = banks[tb]

            def trg(e):
                e.transpose(out=tps[:, 0:128], in_=gsb[:, 0, :], identity=identf[:])
                return e.transpose(out=tps[:, 128:256], in_=gsb[:, 1, :], identity=identf[:])
            S.add("pe", trg, reads=[("gsb", 0), ("gsb", 1), "identf"], writes=[("bank", tb)])
            IG, BP, DD, T0, T1, T2 = gm
            S.add("act", lambda e: e.activation(out=IG[:], in_=tps[:, 0:128], func=AF.Identity, bias=igb_s[:, 0:1]),
                  reads=[("bank", tb), "igb"], writes=["IG"])
            S.add("act", lambda e: e.activation(out=T0[:], in_=tps[:, 128:256], func=AF.Exp, scale=-1.0,
                                                bias=fgbn_s[:, 0:1]),
                  reads=[("bank", tb), "fgbn"], writes=["T0"])
            S.add("act", lambda e: e.activation(out=T0[:], in_=T0[:], func=AF.Ln, bias=1.0),
                  reads=["T0"], writes=["T0"])
            S.add("dve", lambda e: e.tensor_tensor_scan(out=BP[:], data0=resetm_s[:], data1=T0[:], initial=0.0,
                                                        op0=ALU.mult, op1=ALU.add),
                  reads=["T0", "resetm"], writes=["BP"])
            S.add("dve", lambda e: e.tensor_tensor(out=DD[:], in0=IG[:], in1=BP[:], op=ALU.add),
                  reads=["IG", "BP"], writes=["DD"])
            S.add("dve", lambda e: e.tensor_reduce(out=gcol[:, 0:1], in_=DD[:], axis=AX.X, op=ALU.max),
                  reads=["DD"], writes=["gcol"])
            S.add("dve", lambda e: e.tensor_scalar(out=gcol[:, 1:2], in0=BP[:, 127:128], scalar1=-1.0, scalar2=None,
                                                   op0=ALU.mult),
                  reads=["BP", "gcol"], writes=["gcol"])
            S.add("dve", lambda e: e.tensor_tensor(out=gcol[:, 2:3], in0=gcol[:, 0:1], in1=gcol[:, 1:2], op=ALU.add),
                  reads=["gcol"], writes=["gcol"])
            rb = nb()
            rps = banks[rb]

            def trc(e):
                for q in range(3):
                    ins = e.transpose(out=rps[0:1, q * 128:(q + 1) * 128], in_=gcol[:, q:q + 1], identity=identf[:])
                return ins
            S.add("pe", trc, reads=["gcol", "identf"], writes=[("bank", rb)])
            S.add("dve", lambda e: e.tensor_copy(out=grow[:, 0:3, :].rearrange("p a c -> p (a c)"),
                                                 in_=rps[0:1, 0:384]),
                  reads=[("bank", rb)], writes=["grow"])
            R = lambda q: grow[0:1, q, :]
            for h in range(4):
                S.add("dve", lambda e, h=h: e.tensor_tensor_scan(out=grow[0:1, 3, h:64:4], data0=grow[0:1, 1, h:64:4],
                                                                 data1=grow[0:1, 2, h:64:4], initial=0.0,
                                                                 op0=ALU.add, op1=ALU.max),
                      reads=["grow"], writes=["grow"])
            S.add("dve", lambda e: e.memset(grow[0:1, 4, 0:4], 0.0), reads=["grow"], writes=["grow"])
            S.add("dve", lambda e: e.tensor_copy(out=grow[0:1, 4, 4:64], in_=grow[0:1, 3, 0:60]),
                  reads=["grow"], writes=["grow"])
            S.add("dve", lambda e: e.tensor_scalar(out=grow[0:1, 4, 64:68], in0=grow[0:1, 3, 60:64],
                                                   scalar1=flag_s[0:1, 0:1], scalar2=None, op0=ALU.mult),
                  reads=["grow", "flag"], writes=["grow"])
            for h in range(4):
                S.add("dve", lambda e, h=h: e.tensor_tensor_scan(out=grow[0:1, 3, 64 + h:128:4],
                                                                 data0=grow[0:1, 1, 64 + h:128:4],
                                                                 data1=grow[0:1, 2, 64 + h:128:4],
                                                                 initial=grow[0:1, 4, 64 + h:65 + h],
                                                                 op0=ALU.add, op1=ALU.max),
                      reads=["grow"], writes=["grow"])
            S.add("dve", lambda e: e.tensor_copy(out=grow[0:1, 4, 68:128], in_=grow[0:1, 3, 64:124]),
                  reads=["grow"], writes=["grow"])
            S.add("dve", lambda e: e.tensor_tensor(out=R(5), in0=R(4), in1=R(0), op=ALU.max), reads=["grow"], writes=["grow"])
            S.add("dve", lambda e: e.tensor_tensor(out=R(9), in0=R(1), in1=R(4), op=ALU.add), reads=["grow"], writes=["grow"])
            S.add("dve", lambda e: e.tensor_tensor(out=R(9), in0=R(9), in1=R(3), op=ALU.subtract), reads=["grow"], writes=["grow"])
            S.add("act", lambda e: e.activation(out=R(6), in_=R(9), func=AF.Exp), reads=["grow"], writes=["grow"])
            S.add("dve", lambda e: e.tensor_tensor(out=R(9), in0=R(2), in1=R(3), op=ALU.subtract), reads=["grow"], writes=["grow"])
            S.add("act", lambda e: e.activation(out=R(7), in_=R(9), func=AF.Exp), reads=["grow"], writes=["grow"])
            S.add("dve", lambda e: e.tensor_tensor(out=R(9), in0=R(4), in1=R(5), op=ALU.subtract), reads=["grow"], writes=["grow"])
            S.add("act", lambda e: e.activation(out=R(8), in_=R(9), func=AF.Exp), reads=["grow"], writes=["grow"])
            S.add("dve", lambda e: e.tensor_scalar(out=R(10), in0=R(5), scalar1=-1.0, scalar2=LNSC, op0=ALU.mult, op1=ALU.add),
                  reads=["grow"], writes=["grow"])
            S.add("dve", lambda e: e.tensor_scalar(out=R(11), in0=R(0), scalar1=-1.0, scalar2=LNSC, op0=ALU.mult, op1=ALU.add),
                  reads=["grow"], writes=["grow"])
            S.add("dve", lambda e: e.tensor_scalar(out=R(9), in0=R(5), scalar1=-1.0, scalar2=None, op0=ALU.mult),
                  reads=["grow"], writes=["grow"])
            bb = nb()
            bps = banks[bb]

            def bc(e):
                for q in range(3):
                    ins = e.matmul(bps[:, q * 128:(q + 1) * 128], lhsT=ones1[0:1, :], rhs=R(6 + q), start=True, stop=True,
                                   skip_group_check=True)
                return ins
            S.add("pe", bc, reads=["grow", "ones1"], writes=[("bank", bb)])
            S.add("dve", lambda e: e.tensor_copy(out=bcs[:].rearrange("p a c -> p (a c)"), in_=bps[:, 0:384]),
                  reads=[("bank", bb)], writes=["bcs"])
            cb = nb()
            cps = banks[cb]

            def trr(e):
                for q in range(3):
                    ins = e.matmul(cps[:, q:q + 1], lhsT=R(9 + q), rhs=ones1[0:1, 0:1], start=True, stop=True,
                                   skip_group_check=True)
                return ins
            S.add("pe", trr, reads=["grow", "ones1"], writes=[("bank", cb)])
            S.add("dve", lambda e: e.tensor_copy(out=gcol[:, 3:6], in_=cps[:, 0:3]), reads=[("bank", cb)], writes=["gcol"])
            S.add("act", lambda e: e.activation(out=T0[:], in_=DD[:], func=AF.Exp, bias=gcol[:, 4:5]),
                  reads=["DD", "gcol"], writes=["T0"])
            S.add("act", lambda e: e.activation(out=T1[:], in_=DD[:], func=AF.Exp, bias=gcol[:, 5:6]),
                  reads=["DD", "gcol"], writes=["T1", "h2f"])
            S.add("act", lambda e: e.activation(out=T2[:], in_=BP[:], func=AF.Exp, bias=gcol[:, 3:4]),
                  reads=["BP", "gcol"], writes=["T2", "h2f"])
            kb = nb()
            kps = banks[kb]

            def trt(e):
                for q, t in enumerate((T0, T1, T2)):
                    ins = e.transpose(out=kps[:, q * 128:(q + 1) * 128], in_=t[:], identity=identf[:])
                return ins
            S.add("pe", trt, reads=["T0", "T1", "T2", "h2f", "identf"], writes=[("bank", kb)])
            S.add("dve", lambda e: e.tensor_copy(out=tokT[:].rearrange("p a c -> p (a c)"), in_=kps[:, 0:384]),
                  reads=[("bank", kb)], writes=["tokT"])

            ucnt = [0]

            def proj_conv(i, cg, p, etmp, ekey, save_only=False):
                b = nb()
                pb = banks[b]

                def mm(e):
                    for c in range(4):
                        fk = cg * 4 + c
                        for k in range(8):
                            ins = e.matmul(pb[:, c * 128:(c + 1) * 128], lhsT=win[:, k, fk * 128:(fk + 1) * 128],
                                           rhs=hT[i][:, k, :], start=(k == 0), stop=(k == 7), skip_group_check=True)
                    return ins
                S.add("pe", mm, reads=[("hT", i)] + WA, writes=[("bank", b)], cost=2.6)
                u = ucnt[0] % 2
                ucnt[0] += 1
                S.add("dve", lambda e: e.tensor_copy(out=ubc[u][:, :, 0:3], in_=halo_c[:, cg * 4:(cg + 1) * 4, :]),
                      reads=[("halo_c", cg)], writes=[("ubc", u)])
                S.add("act", lambda e: e.activation(out=ubc[u][:, :, 3:131], in_=pb[:, :].rearrange("p (c t) -> p c t", c=4),
                                                    func=AF.Copy),
                      reads=[("bank", b)], writes=[("ubc", u)], cost=0.6)
                S.add("dve", lambda e: e.tensor_copy(out=halo_c[:, cg * 4:(cg + 1) * 4, :], in_=ubc[u][:, :, 128:131]),
                      reads=[("ubc", u)], writes=[("halo_c", cg)])
                if save_only:
                    return
                b2 = nb()
                pb2 = banks[b2]

                def cv(e):
                    for c in range(4):
                        fk = cg * 4 + c
                        for j in range(4):
                            ins = e.matmul(pb2[:, c * 128:(c + 1) * 128], lhsT=dconv[:, fk, j, :], rhs=ubc[u][:, c, j:j + 128],
                                           start=(j == 0), stop=(j == 3), skip_group_check=True)
                    return ins
                S.add("pe", cv, reads=[("ubc", u)] + [("dconv", cg * 4 + c) for c in range(4)], writes=[("bank", b2)], cost=1.4)
                S.add("act", lambda e: e.activation(out=etmp, in_=pb2[:, :], func=AF.Exp, scale=-1.0),
                      reads=[("bank", b2)], writes=[ekey], cost=0.6)
                S.add("act", lambda e: e.activation(out=etmp, in_=etmp, func=AF.Ln, bias=1.0),
                      reads=[ekey], writes=[ekey], cost=0.6)
                S.add("act", lambda e: e.activation(out=etmp, in_=etmp, func=AF.Exp, scale=-1.0),
                      reads=[ekey], writes=[ekey], cost=0.6)
                S.add("dve", lambda e: e.tensor_tensor(out=qkT[p][:, cg * 4:(cg + 1) * 4, :].rearrange("p c t -> p (c t)"),
                                                       in0=pb2[:, :], in1=etmp, op=ALU.mult),
                      reads=[("bank", b2), ekey], writes=[("qkT", p, cg * 4 + c) for c in range(4)], cost=0.6)

            def proj_v(i, p):
                b = nb()
                pb = banks[b]

                def mm(e):
                    for k in range(8):
                        ins = e.matmul(pb[:, :], lhsT=hT[i][:, k, :], rhs=win[:, k, 1024:1536], start=(k == 0), stop=(k == 7))
                    return ins
                S.add("pe", mm, reads=[("hT", i)] + WA, writes=[("bank", b)], cost=2.5)
                S.add("act", lambda e: e.activation(out=v1[p][:, :, 0:128], in_=pb[:, :].rearrange("p (h d) -> p h d", h=4),
                                                    func=AF.Copy),
                      reads=[("bank", b)], writes=[("v1", p)])

            def k_tok(p):
                b = nb()
                pb = banks[b][:].bitcast(BF16)

                def tr(e):
                    for h in range(4):
                        ins = e.transpose(out=pb[:, h * 128:(h + 1) * 128], in_=qkT[p][:, 4 + h, :], identity=identb[:])
                    return ins
                S.add("pe", tr, reads=[("qkT", p, 4 + h) for h in range(4)] + ["identb"], writes=[("bank", b)])
                S.add("dve", lambda e: e.tensor_copy(out=kTok[p][:].rearrange("p h d -> p (h d)"), in_=pb[:, 0:512]),
                      reads=[("bank", b)], writes=[("kTok", p)])

            def pool_proj(i, g, dst_fn):
                b = nb()
                pb = banks[b]

                def mm(e):
                    for k in range(8):
                        ins = e.matmul(pb[:, 0:128], lhsT=win[:, k, 2056 + g * 128:2056 + (g + 1) * 128],
                                       rhs=hT[i][:, k, :], start=(k == 0), stop=(k == 7))
                    return ins
                S.add("pe", mm, reads=[("hT", i)] + WA, writes=[("bank", b)])
                return b, pb

            def state_update(ci, p):
                for h in range(4):
                    ch = ci * 4 + h
                    w_ = wv[h % 2]
                    S.add("act", lambda e, h=h, ch=ch, w_=w_: e.activation(out=w_[:], in_=v1[p][:, h, :], func=AF.Copy,
                                                                           scale=tokT[:, 1, ch:ch + 1]),
                          reads=[("v1", p), "tokT"], writes=[("wv", h % 2)])
                    b = nb()
                    pb = banks[b]
                    S.add("pe", lambda e, h=h, w_=w_, pb=pb: e.matmul(pb[:, 0:129], lhsT=kTok[p][:, h, :], rhs=w_[:],
                                                                      start=True, stop=True),
                          reads=[("kTok", p), ("wv", h % 2)], writes=[("bank", b)])
                    S.add("dve", lambda e, h=h, ch=ch: e.tensor_scalar(out=Ctmp[:, h, :], in0=Cst[:, h, :],
                                                                       scalar1=bcs[:, 0, ch:ch + 1], scalar2=None,
                                                                       op0=ALU.mult),
                          reads=[("Cst", h), "bcs"], writes=[("Ctmp", h)])
                    S.add("dve", lambda e, h=h, ch=ch, pb=pb: e.scalar_tensor_tensor(out=Cst[:, h, :], in0=pb[:, 0:129],
                                                                                     scalar=bcs[:, 1, ch:ch + 1],
                                                                                     in1=Ctmp[:, h, :], op0=ALU.mult,
                                                                                     op1=ALU.add),
                          reads=[("bank", b), ("Ctmp", h), "bcs"], writes=[("Cst", h)])

            def front_prefix(ci, p):
                i = norm_T(ci, False)
                et, ek = h2Tf[:, 4:8, :].rearrange("p k t -> p (k t)"), ("h2Tf", 4)
                proj_conv(i, 1, p, et, ek)
                if ci == 15:
                    proj_conv(i, 0, p, et, ek, save_only=True)
                    for g in range(4):
                        b, pb = pool_proj(i, g, None)
                        S.add("act", lambda e, g=g, pb=pb: e.activation(out=phalo[:, g, :], in_=pb[:, 112:128], func=AF.Copy),
                              reads=[("bank", b)], writes=[("phalo", g)])
                proj_v(i, p)
                k_tok(p)

            for ci in range(16):
                front_prefix(ci, ci % 2)
                if ci > 0:
                    state_update(ci - 1, (ci - 1) % 2)
            ckpt(4)

            def front_own(ti, p):
                ci = 16 + ti
                i = norm_T(ci, False)
                et, ek = xin[0][:, 0:512], ("xin", 0)
                proj_conv(i, 0, p, et, ek)
                proj_conv(i, 1, p, et, ek)
                proj_v(i, p)
                b = nb()
                pb = banks[b]

                def mm(e):
                    for k in range(8):
                        ins = e.matmul(pb[:, :], lhsT=hT[i][:, k, :], rhs=win[:, k, 1536:2048], start=(k == 0), stop=(k == 7))
                    return ins
                S.add("pe", mm, reads=[("hT", i)] + WA, writes=[("bank", b)], cost=2.5)
                S.add("act", lambda e: e.activation(out=et, in_=pb[:, :], func=AF.Exp, scale=-1.0),
                      reads=[("bank", b)], writes=[ek], cost=0.6)
                S.add("dve", lambda e: e.tensor_scalar(out=et, in0=et, scalar1=1.0, scalar2=None, op0=ALU.add),
                      reads=[ek], writes=[ek], cost=0.5)
                def rcp(e):
                    with nc.allow_low_precision("sigmoid gate is stored in bf16 (matmul-operand precision)"):
                        return e.reciprocal(out=osig[p][:], in_=et)
                S.add("dve", rcp, reads=[ek], writes=[("osig", p)], cost=0.5)
                k_tok(p)
                for g in range(4):
                    b, pbg = pool_proj(i, g, None)
                    A_ = pu[0]
                    S.add("dve", lambda e, g=g: e.tensor_copy(out=A_[:, 0:16], in_=phalo[:, g, :]),
                          reads=[("phalo", g)], writes=["puA"])
                    S.add("act", lambda e, pbg=pbg: e.activation(out=A_[:, 16:144], in_=pbg[:, 0:128], func=AF.Copy),
                          reads=[("bank", b)], writes=["puA"])
                    S.add("dve", lambda e, g=g: e.tensor_copy(out=phalo[:, g, :], in_=A_[:, 128:144]),
                          reads=["puA"], writes=[("phalo", g)])
                    src_t, src_k = A_, "puA"
                    sh, lo = 1, 1
                    for s_ in range(g + 1):
                        dst_t = pu[1 + (s_ % 3)]
                        dk = "pu%d" % (1 + (s_ % 3))
                        S.add("dve", lambda e, src_t=src_t, dst_t=dst_t, sh=sh, lo=lo: e.tensor_tensor(
                            out=dst_t[:, lo:144], in0=src_t[:, lo:144], in1=src_t[:, lo - sh:144 - sh], op=ALU.add),
                            reads=[src_k], writes=[dk])
                        src_t, src_k = dst_t, dk
                        sh *= 2
                        lo = 2 * sh - 1
                    wdw = float(2 ** (g + 1))
                    if ti == 0:
                        S.add("dve", lambda e, src_t=src_t, g=g: e.tensor_tensor(out=src_t[:, 16:32], in0=src_t[:, 16:32],
                                                                                 in1=corr_s[:, g, :], op=ALU.mult),
                              reads=[src_k, "corr"], writes=[src_k])
                    S.add("dve", lambda e, src_t=src_t, wdw=wdw: e.scalar_tensor_tensor(
                        out=pooledT[:], in0=src_t[:, 16:144], scalar=1.0 / wdw, in1=A_[:, 16:144], op0=ALU.mult,
                        op1=ALU.subtract),
                        reads=[src_k, "puA"], writes=["pooledT"])
                    b2 = nb()
                    pb2 = banks[b2]
                    S.add("pe", lambda e, g=g, pb2=pb2: e.matmul(pb2[:, 0:128], lhsT=poolw[:, g, :], rhs=pooledT[:],
                                                                 start=True, stop=True),
                          reads=["pooledT", "poolw"], writes=[("bank", b2)])
                    S.add("act", lambda e, g=g, pb2=pb2: e.activation(out=yT[p][:, 4 + g, :], in_=pb2[:, 0:128], func=AF.Copy,
                                                                      scale=pscale_s[:, g:g + 1]),
                          reads=[("bank", b2), "pscale"], writes=[("yT", p, 4 + g)])

            def back_own(ti, p):
                ci = 16 + ti
                for h in range(4):
                    ch = ci * 4 + h
                    b = nb()
                    pb = banks[b]
                    S.add("pe", lambda e, h=h, pb=pb: e.matmul(pb[:, 0:128], lhsT=qkT[p][:, 4 + h, :], rhs=qkT[p][:, h, :],
                                                               start=True, stop=True),
                          reads=[("qkT", p, 4 + h), ("qkT", p, h)], writes=[("bank", b)])
                    s_ = sTp[h % 2]
                    S.add("dve", lambda e, ch=ch, pb=pb, s_=s_: e.scalar_tensor_tensor(
                        out=s_[:], in0=pb[:, 0:128], scalar=tokT[:, 0, ch:ch + 1], in1=maskb[:], op0=ALU.mult, op1=ALU.mult),
                        reads=[("bank", b), "tokT", "maskb"], writes=[("sTp", h % 2)])
                    S.add("act", lambda e, h=h, ch=ch: e.activation(out=Cs[:, h, :], in_=Cst[:, h, :], func=AF.Copy,
                                                                    scale=bcs[:, 2, ch:ch + 1]),
                          reads=[("Cst", h), "bcs"], writes=[("Cs", h)])
                    b2 = nb()
                    pb2 = banks[b2]

                    def nd(e, h=h, pb2=pb2, s_=s_):
                        e.matmul(pb2[:, 0:129], lhsT=s_[:], rhs=v1[p][:, h, :], start=True, stop=False)
                        return e.matmul(pb2[:, 0:129], lhsT=qkT[p][:, h, :], rhs=Cs[:, h, :], start=False, stop=True)
                    S.add("pe", nd, reads=[("sTp", h % 2), ("v1", p), ("qkT", p, h), ("Cs", h)], writes=[("bank", b2)])
                    S.add("dve", lambda e, h=h, pb2=pb2: e.tensor_scalar(out=dmx[:, h:h + 1], in0=pb2[:, 128:129], scalar1=-1.0,
                                                                         scalar2=None, op0=ALU.mult),
                          reads=[("bank", b2)], writes=[("dmx", h)])
                    S.add("dve", lambda e, h=h, pb2=pb2: e.tensor_tensor(out=dmx[:, h:h + 1], in0=pb2[:, 128:129],
                                                                         in1=dmx[:, h:h + 1], op=ALU.max),
                          reads=[("bank", b2), ("dmx", h)], writes=[("dmx", h)])
                    S.add("dve", lambda e, h=h, ch=ch: e.tensor_scalar(
                        out=dmx[:, h:h + 1], in0=dmx[:, h:h + 1], scalar1=tokT[:, 2, ch:ch + 1], scalar2=None,
                        op0=ALU.max),
                        reads=[("dmx", h), "tokT"], writes=[("dmx", h)])
                    S.add("dve", lambda e, h=h: e.reciprocal(out=dmx[:, 4 + h:5 + h], in_=dmx[:, h:h + 1]),
                          reads=[("dmx", h)], writes=[("dmx", 4 + h)])
                    S.add("act", lambda e, h=h, pb2=pb2: e.activation(out=hm[:, h, :], in_=pb2[:, 0:128], func=AF.Copy,
                                                                      scale=dmx[:, 4 + h:5 + h]),
                          reads=[("bank", b2), ("dmx", 4 + h)], writes=[("hm", h)])
                    S.add("dve", lambda e, h=h: e.bn_stats(out=bst[:, h, :], in_=hm[:, h, :]),
                          reads=[("hm", h)], writes=[("bst", h)])
                    S.add("dve", lambda e, h=h: e.bn_aggr(out=mv[:, h, :], in_=bst[:, h, :]),
                          reads=[("bst", h)], writes=[("mv", h)])
                state_update(ci, p)
                S.add("act", lambda e: e.activation(out=lnr[:], in_=mv[:, :, 1], func=AF.Ln, bias=EPS),
                      reads=[("mv", h) for h in range(4)], writes=["lnr"])
                S.add("act", lambda e: e.activation(out=lnr[:], in_=lnr[:], func=AF.Exp, scale=-0.5),
                      reads=["lnr"], writes=["lnr"])
                for h in range(4):
                    S.add("dve", lambda e, h=h: e.tensor_scalar(out=h2f[:, h * 128:(h + 1) * 128], in0=hm[:, h, :],
                                                                scalar1=mv[:, h, 0:1], scalar2=lnr[:, h:h + 1],
                                                                op0=ALU.subtract, op1=ALU.mult),
                          reads=[("hm", h), ("mv", h), "lnr"], writes=["h2f"])
                S.add("dve", lambda e: e.tensor_tensor(out=ymf, in0=ymf, in1=hngb_s[:], op=ALU.mult),
                      reads=["h2f", "hngb"], writes=["h2f"])
                S.add("dve", lambda e: e.tensor_tensor(out=ym[:], in0=ymf, in1=osig[p][:], op=ALU.mult),
                      reads=["h2f", ("osig", p)], writes=["ym"])
                b = nb()
                pbb = banks[b][:].bitcast(BF16)

                def tr(e, pbb=pbb):
                    for h in range(4):
                        ins = e.transpose(out=pbb[:, h * 128:(h + 1) * 128], in_=ym[:, h * 128:(h + 1) * 128],
                                          identity=identb[:])
                    return ins
                S.add("pe", tr, reads=["ym", "identb"], writes=[("bank", b)])
                S.add("act", lambda e, pbb=pbb: e.activation(out=yT[p][:, 0:4, :].rearrange("p h t -> p (h t)"), in_=pbb[:, 0:512],
                                                             func=AF.Copy),
                      reads=[("bank", b)], writes=[("yT", p, h) for h in range(4)])
                for hf in range(2):
                    b = nb()
                    pb = banks[b]

                    def mm(e, hf=hf, pb=pb):
                        for k in range(8):
                            ins = e.matmul(pb[:, :], lhsT=yT[p][:, k, :], rhs=wout[:, k, hf * 512:(hf + 1) * 512],
                                           start=(k == 0), stop=(k == 7))
                        return ins
                    S.add("pe", mm, reads=[("yT", p, k) for k in range(8)] + ["wout"], writes=[("bank", b)], cost=2.5)
                    S.add("dve", lambda e, hf=hf, pb=pb: e.tensor_tensor(
                        out=x1[:, ti, hf * 512:(hf + 1) * 512], in0=pb[:, :], in1=x1[:, ti, hf * 512:(hf + 1) * 512], op=ALU.add),
                        reads=[("bank", b), ("x1", ti)], writes=[("x1", ti)])
                S.add("act", lambda e: e.activation(out=h2f[:], in_=x1[:, ti, :], func=AF.Square,
                                                    accum_out=ssq[:, 32 + ti:33 + ti]),
                      reads=[("x1", ti)], writes=["h2f", ("ssq", 32 + ti)])
                S.add("act", lambda e: e.activation(out=ssq[:, 32 + ti:33 + ti], in_=ssq[:, 32 + ti:33 + ti],
                                                    func=AF.Ln, scale=1.0 / D, bias=EPS),
                      reads=[("ssq", 32 + ti)], writes=[("ssq", 32 + ti)])
                S.add("act", lambda e: e.activation(out=rstd2[:, ti:ti + 1], in_=ssq[:, 32 + ti:33 + ti],
                                                    func=AF.Exp, scale=-0.5),
                      reads=[("ssq", 32 + ti)], writes=[("rstd2", ti)])
                S.add("act", lambda e: e.activation(out=h2f[:], in_=x1[:, ti, :], func=AF.Copy, scale=rstd2[:, ti:ti + 1]),
                      reads=[("x1", ti), ("rstd2", ti)], writes=["h2f"])
                for hf in range(2):
                    b = nb()
                    pb = banks[b]

                    def tr(e, hf=hf, pb=pb):
                        for j in range(4):
                            k = hf * 4 + j
                            ins = e.transpose(out=pb[:, j * 128:(j + 1) * 128], in_=h2f[:, k * 128:(k + 1) * 128],
                                              identity=identf[:])
                        return ins
                    S.add("pe", tr, reads=["h2f", "identf"], writes=[("bank", b)])
                    for j in range(4):
                        k = hf * 4 + j
                        S.add("act" if hf else "dve",
                              (lambda e, k=k, j=j, pb=pb: e.activation(out=h2Tf[:, k, :], in_=pb[:, j * 128:(j + 1) * 128],
                                                                       func=AF.Copy, scale=g2c_s[:, k:k + 1])) if hf else
                              (lambda e, k=k, j=j, pb=pb: e.tensor_scalar(out=h2Tf[:, k, :], in0=pb[:, j * 128:(j + 1) * 128],
                                                                          scalar1=g2c_s[:, k:k + 1], scalar2=None,
                                                                          op0=ALU.mult)),
                              reads=[("bank", b), "g2c"], writes=[("h2Tf", k)])
                b = nb()
                pb = banks[b]

                def rmm(e, pb=pb):
                    for k in range(8):
                        ins = e.matmul(pb[:, 0:NE], lhsT=h2Tf[:, k, :], rhs=wr[:, k, :], start=(k == 0), stop=(k == 7))
                    return ins
                S.add("pe", rmm, reads=[("h2Tf", k) for k in range(8)] + ["wr"], writes=[("bank", b)])
                S.add("dve", lambda e, pb=pb: e.tensor_tensor(out=lg[:], in0=pb[:, 0:NE], in1=brb_s[:], op=ALU.add),
                      reads=[("bank", b), "brb"], writes=["lg"])
                S.add("dve", lambda e: e.max(out=top8[:], in_=lg[:]), reads=["lg"], writes=["top8"])
                S.add("dve", lambda e: e.tensor_scalar(out=rt[:, 0, :], in0=lg[:], scalar1=top8[:, 3:4], scalar2=None,
                                                       op0=ALU.is_ge),
                      reads=["lg", "top8"], writes=["rt0"])
                S.add("dve", lambda e: e.tensor_scalar(out=rsm[:, 0:1], in0=top8[:, 0:1], scalar1=-1.0, scalar2=None,
                                                       op0=ALU.mult),
                      reads=["top8"], writes=["rsm0"])
                S.add("act", lambda e: e.activation(out=rt[:, 1, :], in_=lg[:], func=AF.Exp, bias=rsm[:, 0:1]),
                      reads=["lg", "rsm0"], writes=["rt1"])
                S.add("dve", lambda e: e.tensor_tensor(out=rt[:, 2, :], in0=rt[:, 1, :], in1=rt[:, 0, :], op=ALU.mult),
                      reads=["rt0", "rt1"], writes=["rt2"])
                S.add("dve", lambda e: e.tensor_reduce(out=rsm[:, 1:2], in_=rt[:, 2, :], axis=AX.X, op=ALU.add),
                      reads=["rt2"], writes=["rsm1"])
                S.add("dve", lambda e: e.reciprocal(out=rsm[:, 2:3], in_=rsm[:, 1:2]), reads=["rsm1"], writes=["rsm2"])
                S.add("dve", lambda e: e.tensor_scalar(out=G[:, ti, :], in0=rt[:, 2, :], scalar1=rsm[:, 2:3],
                                                       scalar2=None, op0=ALU.mult),
                      reads=["rt2", "rsm2"], writes=[("G", ti)])

            front_own(0, 0)
            state_update(15, 1)
            for h in range(4):
                S.add("dve", lambda e, h=h: e.tensor_scalar(out=Cst[:, h, :], in0=Cst[:, h, :], scalar1=flag_s[:, 0:1],
                                                            scalar2=None, op0=ALU.mult),
                      reads=[("Cst", h), "flag"], writes=[("Cst", h)])
            for ti in range(NT):
                if ti + 1 < NT:
                    front_own(ti + 1, (ti + 1) % 2)
                back_own(ti, ti % 2)
            S.stopped = False
            if debug:
                for ti in range(NT):
                    S.add("sp", lambda e, ti=ti: e.dma_start(out=dbg[ti * 128:(ti + 1) * 128, :], in_=x1[:, ti, :]),
                          reads=[("x1", ti)], dma=True)
                    S.add("sp", lambda e, ti=ti: e.dma_start(out=dbgG[ti * 128:(ti + 1) * 128, :], in_=G[:, ti, :]),
                          reads=[("G", ti)], dma=True)
            dm = dummies()
            dpb = banks[0]
            dm["pe"] = lambda e: e.matmul(dpb[0:1, 0:1], lhsT=ones1[0:1, 0:1], rhs=ones1[0:1, 0:1], start=True, stop=True,
                                          skip_group_check=True)
            S.emit(dm)

        with contextlib.ExitStack() as sbk:
            if debug and os.environ.get("KSKIPB"):
                return nc
            S = Sched(nc, semst, "B")
            S.ignore_cost = True
            bank_i = [0]

            def nb():
                i = bank_i[0] % 8
                bank_i[0] += 1
                return i
            h2 = sb(sbk, "h2", [128, NT, D], BF16)
            g2b_s = sb(sbk, "g2b_s", [128, D], F32)
            bgT_s = sb(sbk, "bgT_s", [128, NE, 8], F32)
            buT_s = sb(sbk, "buT_s", [128, NE, 8], F32)
            bdn = sb(sbk, "bdn", [NE, D], F32)
            GTs = [sb(sbk, f"GTs{i}", [NE, 128], F32) for i in range(2)]
            slot = sb(sbk, "slot", [128, NT, NE], F32)
            Ghl = sb(sbk, "Ghl", [128, NT, NE, 2], BF16)
            Mb = sb(sbk, "Mb", [128, NT, NE], BF16)
            Mf = [sb(sbk, f"Mf{i}", [128, NE], F32) for i in range(2)]
            Gr = [sb(sbk, f"Gr{i}", [128, NE], F32) for i in range(2)]
            iota_s = sb(sbk, "iota_s", [128, 128], F32)
            ltf = sb(sbk, "ltf", [128, 128], F32)
            lts = sb(sbk, "lts", [128, 128], BF16)
            onesb = sb(sbk, "onesb", [128, 128], BF16)
            P = [sb(sbk, "P0", [128, NT, 128], BF16)] * 2
            PT = [sb(sbk, f"PT{i}", [128, 4, 512], BF16) for i in range(2)]
            gsel = [sb(sbk, f"gsel{i}", [128, 4], F32) for i in range(2)]
            xTs = sb(sbk, "xTs", [128, 8, 512], BF16)
            aT = sb(sbk, "aT", [128, 8, 512], BF16)
            ysc = [sb(sbk, f"ysc{i}", [128, D], BF16) for i in range(2)]
            gc = [sb(sbk, "gc0", [128, 512], F32)] * 2
            sg = [sb(sbk, "sg0", [128, 512], F32)] * 2
            uc = [sb(sbk, "uc0", [128, 512], F32)] * 2
            ones1b = sb(sbk, "ones1b", [1, 8], F32)
            for dst, src, k in ((bgT_s, bgT, "bgT"), (buT_s, buT, "buT"), (bdn, b_down, "bdn"), (g2b_s, g2b, "g2b"),
                                (iota_s, iotaj, "iota"), (ltf, ltstrict, "ltf")):
                S.add("sp", lambda e, dst=dst, src=src: e.dma_start(out=dst[:], in_=src), writes=[k], dma=True)
            S.add("dve", lambda e: e.memset(ones1b[:], 1.0), writes=["ones1b"])
            S.add("dve", lambda e: e.memset(onesb[:], 1.0), writes=["onesb"])
            S.add("dve", lambda e: e.tensor_copy(out=lts[:], in_=ltf[:]), reads=["ltf"], writes=["lts"])
            S.add("dve", lambda e: e.tensor_scalar(out=buT_s[:], in0=buT_s[:], scalar1=1.0, scalar2=None, op0=ALU.add),
                  reads=["buT"], writes=["buT"])
            wsl = [wa[:, s * 8192:(s + 1) * 8192].rearrange("p (k f) -> p k f", k=8) for s in range(3)]
            wsrc = []
            for e_ in range(NE):
                wsrc += [w_gate[e_], w_up[e_], w_down[e_]]

            def wload(mi):
                s = mi % 3
                S.add("pool", lambda e, mi=mi, s=s: e.dma_start(out=wsl[s], in_=wsrc[mi].rearrange("(k p) f -> p k f", p=128)),
                      writes=[("ws", s)], dma=True, cost=14.0)
            if ne_run > 0:
                wload(0); wload(1); wload(2)
            for ti in range(NT):
                i = ti % 2
                S.add("dve", lambda e, ti=ti: e.scalar_tensor_tensor(out=h2[:, ti, :], in0=x1[:, ti, :],
                                                                     scalar=rstd2[:, ti:ti + 1], in1=g2b_s[:],
                                                                     op0=ALU.mult, op1=ALU.mult),
                      reads=[("x1", ti), "g2b", ("rstd2", ti)], writes=[("h2", ti)])
                S.add("dve", lambda e, ti=ti: e.tensor_scalar(out=Mb[:, ti, :], in0=G[:, ti, :], scalar1=0.0, scalar2=None,
                                                              op0=ALU.is_gt),
                      reads=[("G", ti)], writes=[("Mb", ti)])
                S.add("dve", lambda e, ti=ti: e.tensor_copy(out=Ghl[:, ti, :, 0], in_=G[:, ti, :]),
                      reads=[("G", ti)], writes=[("Ghl", ti)])
                S.add("dve", lambda e, ti=ti, i=i: e.tensor_tensor(out=Gr[i][:], in0=G[:, ti, :], in1=Ghl[:, ti, :, 0],
                                                                   op=ALU.subtract),
                      reads=[("G", ti), ("Ghl", ti)], writes=[("Gr", i)])
                S.add("dve", lambda e, ti=ti, i=i: e.tensor_copy(out=Ghl[:, ti, :, 1], in_=Gr[i][:]),
                      reads=[("Gr", i)], writes=[("Ghl", ti)])
                b = nb()
                pb = banks[b]
                S.add("pe", lambda e, ti=ti, pb=pb: e.transpose(out=pb[0:NE, 0:128], in_=G[:, ti, :], identity=identf[:]),
                      reads=[("G", ti)], writes=[("bank", b)])
                S.add("act", lambda e, i=i, pb=pb: e.activation(out=GTs[i][:], in_=pb[0:NE, 0:128], func=AF.Copy),
                      reads=[("bank", b)], writes=[("GTs", i)])
                for hf in range(2):
                    b = nb()
                    pb = banks[b]
                    S.add("pe", lambda e, i=i, hf=hf, pb=pb: e.matmul(pb[:, :], lhsT=GTs[i][:], rhs=bdn[:, hf * 512:(hf + 1) * 512],
                                                                      start=True, stop=True),
                          reads=[("GTs", i), "bdn"], writes=[("bank", b)])
                    S.add("dve", lambda e, ti=ti, hf=hf, pb=pb: e.tensor_tensor(
                        out=x1[:, ti, hf * 512:(hf + 1) * 512], in0=pb[:, :], in1=x1[:, ti, hf * 512:(hf + 1) * 512], op=ALU.add),
                        reads=[("bank", b), ("x1", ti)], writes=[("x1", ti)])
            for ti in range(NT):
                i = ti % 2
                prev = list(range(ti % 4, ti, 4))
                b = nb()
                pb = banks[b]

                def rk(e, ti=ti, prev=prev, pb=pb):
                    ins = e.matmul(pb[:, 0:NE], lhsT=lts[:], rhs=Mb[:, ti, :], start=True, stop=(not prev))
                    for tj in prev:
                        ins = e.matmul(pb[:, 0:NE], lhsT=onesb[:], rhs=Mb[:, tj, :], start=False, stop=(tj == prev[-1]))
                    return ins
                S.add("pe", rk, reads=[("Mb", tj) for tj in prev + [ti]] + ["lts", "onesb"], writes=[("bank", b)])
                S.add("dve", lambda e, ti=ti, i=i: e.tensor_scalar(out=Mf[i][:], in0=G[:, ti, :], scalar1=0.0, scalar2=None,
                                                                   op0=ALU.is_gt),
                      reads=[("G", ti)], writes=[("Mf", i)])
                S.add("dve", lambda e, ti=ti, i=i, pb=pb: e.scalar_tensor_tensor(out=slot[:, ti, :], in0=pb[:, 0:NE], scalar=1.0,
                                                                                 in1=Mf[i][:], op0=ALU.add, op1=ALU.mult),
                      reads=[("bank", b), ("Mf", i)], writes=[("slot", ti)])
                S.add("dve", lambda e, ti=ti: e.tensor_scalar(out=slot[:, ti, :], in0=slot[:, ti, :], scalar1=-1.0, scalar2=None,
                                                              op0=ALU.add),
                      reads=[("slot", ti)], writes=[("slot", ti)])
            H2 = [("h2", ti) for ti in range(NT)]
            for e_ in range(ne_run):
                sG, sU, sD = 0, 1, 2
                pi = e_ % 2
                Pe, PTe, gse = P[0], PT[pi], gsel[pi]
                for ti in range(NT):
                    S.add("dve", lambda e, ti=ti, e_=e_, Pe=Pe: e.tensor_scalar(out=Pe[:, ti, :], in0=iota_s[:],
                                                                               scalar1=slot[:, ti, e_:e_ + 1], scalar2=None,
                                                                               op0=ALU.is_equal),
                          reads=["iota", ("slot", ti)], writes=[("P", 0, ti % 4)], cost=0.2)
                for kc in range(8):
                    b = nb()
                    pb = banks[b]

                    def ga(e, kc=kc, pb=pb, Pe=Pe):
                        for g in range(4):
                            for r in range(4):
                                ins = e.matmul(pb[:, g * 128:(g + 1) * 128], lhsT=h2[:, 4 * r + g, kc * 128:(kc + 1) * 128],
                                               rhs=Pe[:, 4 * r + g, :], start=(r == 0), stop=(r == 3), skip_group_check=True)
                        return ins
                    S.add("pe", ga, reads=H2 + [("P", 0, g) for g in range(4)], writes=[("bank", b)], cost=1.6)
                    if kc % 2:
                        S.add("act", lambda e, kc=kc, pb=pb: e.activation(out=xTs[:, kc, :], in_=pb[:, :], func=AF.Copy),
                              reads=[("bank", b)], writes=[("xTs", kc)])
                    else:
                        S.add("dve", lambda e, kc=kc, pb=pb: e.tensor_copy(out=xTs[:, kc, :], in_=pb[:, :]),
                              reads=[("bank", b)], writes=[("xTs", kc)])
                for g in range(4):
                    b = nb()
                    pbb = banks[b][:].bitcast(BF16)

                    def trp(e, g=g, pbb=pbb, Pe=Pe):
                        for r in range(4):
                            ins = e.transpose(out=pbb[:, r * 128:(r + 1) * 128], in_=Pe[:, 4 * r + g, :], identity=identb[:])
                        return ins
                    S.add("pe", trp, reads=[("P", 0, g)], writes=[("bank", b)])
                    S.add("act", lambda e, g=g, pbb=pbb, PTe=PTe: e.activation(out=PTe[:, g, :], in_=pbb[:, 0:512], func=AF.Copy),
                          reads=[("bank", b)], writes=[("PT", pi, g)])
                b = nb()
                pbg = banks[b]

                def gs(e, pbg=pbg, Pe=Pe, e_=e_):
                    for g in range(4):
                        for r in range(4):
                            ins = e.matmul(pbg[:, 2 * g:2 * g + 2], lhsT=Pe[:, 4 * r + g, :], rhs=Ghl[:, 4 * r + g, e_, :],
                                           start=(r == 0), stop=(r == 3), skip_group_check=True)
                    return ins
                S.add("pe", gs, reads=[("P", 0, g) for g in range(4)] + [("Ghl", ti) for ti in range(NT)], writes=[("bank", b)])
                S.add("dve", lambda e, pbg=pbg, gse=gse: e.tensor_reduce(out=gse[:], in_=pbg[:, 0:8].rearrange("p (g two) -> p g two", two=2),
                                                                        axis=AX.X, op=ALU.add),
                      reads=[("bank", b)], writes=[("gsel", pi)])
                for fc in range(8):
                    j = 0
                    bg_, bu_ = nb(), nb()
                    pg, pu_ = banks[bg_], banks[bu_]

                    def mmg(e, fc=fc, pg=pg):
                        for k in range(8):
                            ins = e.matmul(pg[:, :], lhsT=wsl[sG][:, k, fc * 128:(fc + 1) * 128], rhs=xTs[:, k, :],
                                           start=(k == 0), stop=(k == 7))
                        return ins

                    def mmu(e, fc=fc, pu_=pu_):
                        for k in range(8):
                            ins = e.matmul(pu_[:, :], lhsT=wsl[sU][:, k, fc * 128:(fc + 1) * 128], rhs=xTs[:, k, :],
                                           start=(k == 0), stop=(k == 7))
                        return ins
                    XT = [("xTs", k) for k in range(8)]
                    S.add("pe", mmg, reads=[("ws", sG)] + XT, writes=[("bank", bg_)], cost=2.5)
                    S.add("pe", mmu, reads=[("ws", sU)] + XT, writes=[("bank", bu_)], cost=2.5)
                    S.add("dve", lambda e, fc=fc, e_=e_, pg=pg, j=j: e.tensor_scalar(
                        out=gc[j][:], in0=pg[:, :], scalar1=bgT_s[:, e_, fc:fc + 1], scalar2=7.0, op0=ALU.add, op1=ALU.min),
                        reads=[("bank", bg_), "bgT"], writes=[("gc", j)], cost=0.6)
                    S.add("act", lambda e, j=j: e.activation(out=sg[j][:], in_=gc[j][:], func=AF.Sigmoid, scale=1.702),
                          reads=[("gc", j)], writes=[("sg", j)], cost=0.6)
                    S.add("act", lambda e, fc=fc, e_=e_, pu_=pu_, j=j: e.activation(out=uc[j][:], in_=pu_[:, :], func=AF.Identity,
                                                                                    bias=buT_s[:, e_, fc:fc + 1]),
                          reads=[("bank", bu_), "buT"], writes=[("uc", j)], cost=0.7)
                    S.add("dve", lambda e, j=j: e.tensor_scalar(out=uc[j][:], in0=uc[j][:], scalar1=-6.0, scalar2=8.0,
                                                                op0=ALU.max, op1=ALU.min),
                          reads=[("uc", j)], writes=[("uc", j)], cost=0.6)
                    S.add("dve", lambda e, j=j: e.tensor_tensor(out=gc[j][:], in0=gc[j][:], in1=sg[j][:], op=ALU.mult),
                          reads=[("gc", j), ("sg", j)], writes=[("gc", j)], cost=0.6)
                    S.add("dve", lambda e, j=j, fc=fc: e.tensor_tensor(out=aT[:, fc, :], in0=gc[j][:], in1=uc[j][:], op=ALU.mult),
                          reads=[("gc", j), ("uc", j)], writes=[("aT", fc)], cost=0.6)
                if e_ + 1 < ne_run:
                    wload(3 * (e_ + 1)); wload(3 * (e_ + 1) + 1)
                AT = [("aT", k) for k in range(8)]
                for g in range(4):
                    yi = g % 2
                    for hf in range(2):
                        b = nb()
                        pb = banks[b]

                        def mmd(e, g=g, hf=hf, pb=pb):
                            for k in range(8):
                                ins = e.matmul(pb[:, :], lhsT=aT[:, k, g * 128:(g + 1) * 128],
                                               rhs=wsl[sD][:, k, hf * 512:(hf + 1) * 512], start=(k == 0), stop=(k == 7))
                            return ins
                        S.add("pe", mmd, reads=AT + [("ws", sD)], writes=[("bank", b)], cost=2.5)
                        S.add("act", lambda e, g=g, hf=hf, pb=pb, yi=yi, gse=gse: e.activation(
                            out=ysc[yi][:, hf * 512:(hf + 1) * 512], in_=pb[:, :], func=AF.Copy, scale=gse[:, g:g + 1]),
                            reads=[("bank", b), ("gsel", pi)], writes=[("ysc", yi)], cost=0.6)
                    for r in range(4):
                        ti = 4 * r + g
                        for hf in range(2):
                            b = nb()
                            pb = banks[b]
                            S.add("pe", lambda e, g=g, r=r, hf=hf, pb=pb, yi=yi, PTe=PTe: e.matmul(
                                pb[:, :], lhsT=PTe[:, g, r * 128:(r + 1) * 128], rhs=ysc[yi][:, hf * 512:(hf + 1) * 512],
                                start=True, stop=True),
                                reads=[("PT", pi, g), ("ysc", yi)], writes=[("bank", b)], cost=0.32)
                            S.add("dve", lambda e, ti=ti, hf=hf, pb=pb: e.tensor_tensor(
                                out=x1[:, ti, hf * 512:(hf + 1) * 512], in0=pb[:, :], in1=x1[:, ti, hf * 512:(hf + 1) * 512],
                                op=ALU.add),
                                reads=[("bank", b), ("x1", ti)], writes=[("x1", ti)], cost=0.6)
                if e_ + 1 < ne_run:
                    wload(3 * (e_ + 1) + 2)
            for ti in range(NT):
                S.add("act", lambda e, ti=ti: e.activation(out=h2[:, ti, :], in_=x1[:, ti, :], func=AF.Square,
                                                           accum_out=rstd2[:, ti:ti + 1]),
                      reads=[("x1", ti)], writes=[("h2", ti), ("rstd2", ti)])
                S.add("act", lambda e, ti=ti: e.activation(out=rstd2[:, ti:ti + 1], in_=rstd2[:, ti:ti + 1], func=AF.Ln,
                                                           scale=1.0 / D, bias=EPS),
                      reads=[("rstd2", ti)], writes=[("rstd2", ti)])
                S.add("act", lambda e, ti=ti: e.activation(out=rstd2[:, ti:ti + 1], in_=rstd2[:, ti:ti + 1], func=AF.Exp,
                                                           scale=-0.5),
                      reads=[("rstd2", ti)], writes=[("rstd2", ti)])
                S.add("dve", lambda e, ti=ti: e.scalar_tensor_tensor(out=x1[:, ti, :], in0=x1[:, ti, :],
                                                                     scalar=rstd2[:, ti:ti + 1], in1=gfb_s[:],
                                                                     op0=ALU.mult, op1=ALU.mult),
                      reads=[("x1", ti), ("rstd2", ti), "gfb"], writes=[("x1", ti)])
                S.add("sp", lambda e, ti=ti: e.dma_start(out=out[ti * 128:(ti + 1) * 128, :], in_=x1[:, ti, :]),
                      reads=[("x1", ti)], dma=True)
            dm = dummies()
            dpb = banks[0]
            dm["pe"] = lambda e: e.matmul(dpb[0:1, 0:1], lhsT=ones1b[0:1, 0:1], rhs=ones1b[0:1, 0:1], start=True, stop=True,
                                          skip_group_check=True)
            S.emit(dm)
    return nc


_NC = None


def _prep(inputs):
    f = lambda a: np.ascontiguousarray(np.asarray(a, dtype=np.float32))
    x = f(inputs["x"])
    rep = lambda v, n=128: f(np.broadcast_to(np.asarray(v, np.float32).reshape(1, -1), (n, np.asarray(v).size)))
    col = lambda v: f(np.asarray(v, np.float32).reshape(8, 128).T)
    common = {
        "w_in": f(inputs["w_in"][0]), "w_out": f(inputs["w_out"][0]), "pool_w": f(inputs["pool_w"][0]),
        "g1c": col(inputs["norm1_g"][0]), "g2c": col(inputs["norm2_g"][0]), "gfb": rep(inputs["normf_g"]),
        "convT": f(np.asarray(inputs["conv_w"][0], np.float32).T.reshape(8, 128, 4).transpose(1, 0, 2)),
        "igb": f(np.tile(np.asarray(inputs["ig_b"][0], np.float32), 32).reshape(128, 1)),
        "fgbn": f(np.tile(np.asarray(inputs["fg_b"][0], np.float32), 32).reshape(128, 1)),
        "hngb": rep(inputs["head_norm_g"][0]),
        "pscale": f(np.asarray(inputs["pool_scale"][0], np.float32).reshape(4, 128).T),
        "w_router": f(inputs["w_router"][0]), "brb": rep(inputs["b_router"][0]),
        "w_gate": f(inputs["w_gate"][0]), "w_up": f(inputs["w_up"][0]), "w_down": f(inputs["w_down"][0]),
        "bgT": f(np.asarray(inputs["b_gate"][0], np.float32).reshape(NE, 8, 128).transpose(2, 0, 1)),
        "buT": f(np.asarray(inputs["b_up"][0], np.float32).reshape(NE, 8, 128).transpose(2, 0, 1)),
        "b_down": f(inputs["b_down"][0]),
        "ident": np.eye(128, dtype=np.float32),
        "maskrl": np.triu(np.ones((128, 128), np.float32)),
        "g2b": rep(inputs["norm2_g"][0]),
        "iotaj": np.ascontiguousarray(np.broadcast_to(np.arange(128, dtype=np.float32)[None, :], (128, 128))),
        "ltstrict": np.triu(np.ones((128, 128), np.float32), 1),
    }
    rm = np.ones((128, 128), np.float32)
    rm[:, 0] = 0.0
    common["resetm"] = rm
    corr_even = np.ones((128, 4, 16), np.float32)
    for g, w in enumerate((2, 4, 8, 16)):
        t = np.arange(16)
        corr_even[:, g, :] = (w / np.minimum(t + 1, w)).astype(np.float32)[None, :]
    maps = []
    for c in range(8):
        b, half = c // 2, c % 2
        m = dict(common)
        m["xo"] = f(x[b, half * TOK:(half + 1) * TOK])
        m["xp"] = f(x[b, 0:TOK]) if half else np.zeros((TOK, D), np.float32)
        m["flag"] = np.full((128, 1), float(half), np.float32)
        m["corr"] = np.ones((128, 4, 16), np.float32) if half else corr_even
        maps.append(m)
    return maps


def kernel(**inputs):
    global _NC
    debug = bool(os.environ.get("KDEBUG"))
    nc = build(debug)
    maps = _prep(inputs)
    res = run_bass_kernel_spmd(nc, maps, core_ids=list(range(8)))
    outp = np.zeros((4, 4096, D), np.float32)
    for c in range(8):
        outp[c // 2, (c % 2) * TOK:(c % 2 + 1) * TOK] = res.results[c]["out"]
    return outp
```

```python
import contextlib
import math
import os
import numpy as np
import concourse.bass as bass
import concourse.mybir as mybir
from concourse.alu_op_type import AluOpType as ALU
from concourse.bass_utils import run_bass_kernel_spmd

F32 = mybir.dt.float32
BF16 = mybir.dt.bfloat16
AF = mybir.ActivationFunctionType
AX = mybir.AxisListType

D = 1024
TOK = 2048
NT = TOK // 128
NE = 32
NIN = 2568
EPS = 1e-5
LNSC = math.log(128.0 ** -0.5)


class Op:
    __slots__ = ("eng", "fn", "deps", "odeps", "signal", "sem", "val", "is_dma", "idx", "cost", "lat", "fin", "nrem", "users")


class Sched:
    ENG = ("pe", "dve", "act", "pool", "sp")

    def __init__(self, nc, semstack, tag, ndma_sems=6):
        self.nc, self.semstack, self.tag = nc, semstack, tag
        self.ops = {e: [] for e in self.ENG}
        self.last_w, self.readers = {}, {}
        self.ndma = ndma_sems
        self.dma_count = {e: 0 for e in self.ENG}
        self.dma_prev = {}
        self.n = 0
        self.stopped = False
        self.ignore_cost = False

    COST = {"pe": 0.55, "dve": 0.35, "act": 0.35, "pool": 1.0, "sp": 0.1}

    def add(self, eng, fn, reads=(), writes=(), dma=False, cost=None):
        op = Op()
        if self.stopped:
            op.deps, op.signal, op.is_dma, op.eng = [], False, dma, eng
            return op
        if self.ignore_cost:
            cost = None
        op.cost = cost if cost is not None else (0.1 if dma else self.COST[eng])
        op.lat = (cost if cost is not None else 3.0) if dma else op.cost
        op.eng, op.fn, op.is_dma, op.signal = eng, fn, dma, False
        op.idx = self.n
        self.n += 1
        deps = []
        for r in reads:
            w = self.last_w.get(r)
            if w is not None:
                deps.append(w)
            if isinstance(r, tuple) and r[0] == "bank":
                deps.extend(o for o in self.readers.get(r, ()) if o.eng != eng)
        for w_ in writes:
            w = self.last_w.get(w_)
            if w is not None:
                deps.append(w)
            deps.extend(self.readers.get(w_, ()))
        for r in reads:
            self.readers.setdefault(r, []).append(op)
        for w_ in writes:
            self.last_w[w_] = op
            self.readers[w_] = []
        if dma:
            k = self.dma_count[eng]
            self.dma_count[eng] += 1
            slot = (eng, k % self.ndma)
            op.sem = slot
            prev = self.dma_prev.get(slot)
            if prev is not None:
                deps.append(prev)
            self.dma_prev[slot] = op
            op.signal = True
        ded, oded, seen = [], [], set()
        for d in deps:
            if d is op or id(d) in seen:
                continue
            seen.add(id(d))
            oded.append(d)
            if (not d.is_dma) and d.eng == eng and eng == "pe":
                continue
            ded.append(d)
        op.deps = ded
        op.odeps = oded
        self.ops[eng].append(op)
        return op

    def reorder(self):
        import heapq
        allops = [op for e in self.ENG for op in self.ops[e]]
        for op in allops:
            op.users, op.nrem, op.fin = [], len(op.odeps), None
        for op in allops:
            for d in op.odeps:
                d.users.append(op)
        SYNC = 0.6
        fut = {e: [] for e in self.ENG}
        now = {e: [] for e in self.ENG}
        t = {e: 0.0 for e in self.ENG}
        new = {e: [] for e in self.ENG}

        def push(op):
            r = 0.0
            for d in op.odeps:
                f = d.fin + (0.0 if (d.eng == op.eng and not d.is_dma) else SYNC)
                if f > r:
                    r = f
            heapq.heappush(fut[op.eng], (r, op.idx, op))
        for op in allops:
            if op.nrem == 0:
                push(op)
        left = len(allops)
        while left:
            best = None
            for e in self.ENG:
                while fut[e] and fut[e][0][0] <= t[e]:
                    r, i, op = heapq.heappop(fut[e])
                    heapq.heappush(now[e], (i, op))
                if now[e]:
                    st = t[e]
                elif fut[e]:
                    st = fut[e][0][0]
                else:
                    continue
                if best is None or st < best[0]:
                    best = (st, e)
            st, e = best
            if now[e]:
                i, op = heapq.heappop(now[e])
            else:
                r, i, op = heapq.heappop(fut[e])
            new[e].append(op)
            t[e] = st + op.cost
            op.fin = st + op.lat
            left -= 1
            for u in op.users:
                u.nrem -= 1
                if u.nrem == 0:
                    push(u)
        self.ops = new
        self.est = max(t.values())

    def emit(self, dummies, final_waits=()):
        nc = self.nc
        if os.environ.get("KNOSCHED") is None:
            self.reorder()
        for e, fn in dummies.items():
            o = self.add(e, fn, writes=[("bank", 0)] if e == "pe" else ())
            o.signal = True
        for e in self.ENG:
            for op in self.ops[e]:
                for d in op.deps:
                    d.signal = True
        esem = {e: self.semstack.enter_context(nc.semaphore(f"s{self.tag}_{e}")) for e in self.ENG}
        dsem = {}
        for e in self.ENG:
            for i in range(min(self.ndma, self.dma_count[e])):
                dsem[(e, i)] = self.semstack.enter_context(nc.semaphore(f"d{self.tag}_{e}{i}"))
        finals = {}
        for e in self.ENG:
            c, dc = 0, {}
            for op in self.ops[e]:
                if op.is_dma:
                    dc[op.sem] = dc.get(op.sem, 0) + 16
                    op.val = dc[op.sem]
                    op.sem = dsem[op.sem]
                    finals[id(op.sem)] = (op.sem, op.val)
                elif op.signal:
                    c += 1
                    op.val = c
                    op.sem = esem[e]
                    finals[id(op.sem)] = (op.sem, op.val)
        with nc.Block() as block:
            def run(e, eng):
                waited = {}
                for op in self.ops[e]:
                    for d in op.deps:
                        key = id(d.sem)
                        if waited.get(key, 0) >= d.val:
                            continue
                        waited[key] = d.val
                        eng.wait_ge(d.sem, d.val)
                    ins = op.fn(eng)
                    if op.signal:
                        ins.then_inc(op.sem, 16 if op.is_dma else 1)
                for sem, val in finals.values():
                    if waited.get(id(sem), 0) < val:
                        eng.wait_ge(sem, val)

            @block.tensor
            def _(eng):
                run("pe", eng)

            @block.vector
            def _(eng):
                run("dve", eng)

            @block.scalar
            def _(eng):
                run("act", eng)

            @block.gpsimd
            def _(eng):
                run("pool", eng)

            @block.sync
            def _(eng):
                run("sp", eng)


def build(debug=False):
    nc = bass.Bass("TRN2", target_bir_lowering=False)

    def din(name, shape):
        return nc.dram_tensor(name, list(shape), F32, kind="ExternalInput").ap()

    xo = din("xo", [TOK, D]); xp = din("xp", [TOK, D])
    w_in = din("w_in", [D, NIN]); w_out = din("w_out", [D, D]); pool_w = din("pool_w", [4, 128, 128])
    g1c = din("g1c", [128, 8]); g2c = din("g2c", [128, 8]); gfb = din("gfb", [128, D])
    convT = din("convT", [128, 8, 4]); igb = din("igb", [128, 1]); fgbn = din("fgbn", [128, 1])
    hngb = din("hngb", [128, 512]); pscale = din("pscale", [128, 4])
    w_router = din("w_router", [D, NE]); brb = din("brb", [128, NE])
    w_gate = din("w_gate", [NE, D, D]); w_up = din("w_up", [NE, D, D]); w_down = din("w_down", [NE, D, D])
    bgT = din("bgT", [128, NE, 8]); buT = din("buT", [128, NE, 8]); b_down = din("b_down", [NE, D])
    ident = din("ident", [128, 128]); maskrl = din("maskrl", [128, 128]); corr = din("corr", [128, 4, 16])
    flag = din("flag", [128, 1]); resetm = din("resetm", [128, 128])
    g2b = din("g2b", [128, D]); iotaj = din("iotaj", [128, 128]); ltstrict = din("ltstrict", [128, 128])
    out = nc.dram_tensor("out", [TOK, D], F32, kind="ExternalOutput").ap()
    dbg = nc.dram_tensor("dbg", [TOK, D], F32, kind="ExternalOutput").ap() if debug else None
    dbgG = nc.dram_tensor("dbgG", [TOK, NE], F32, kind="ExternalOutput").ap() if debug else None
    ne_run = int(os.environ.get("KNE", NE)) if debug else NE

    with contextlib.ExitStack() as st, contextlib.ExitStack() as semst:
        def sb(stack, name, shape, dt):
            return stack.enter_context(nc.sbuf_tensor(name, list(shape), dt))

        def ps(stack, name, shape, dt):
            return stack.enter_context(nc.psum_tensor(name, list(shape), dt))

        x1 = sb(st, "x1", [128, NT, D], F32)
        wa = sb(st, "wa", [128, 3 * 8192], BF16)
        G = sb(st, "G", [128, NT, NE], F32)
        rstd2 = sb(st, "rstd2", [128, NT], F32)
        identf = sb(st, "identf", [128, 128], F32)
        identb = sb(st, "identb", [128, 128], BF16)
        gfb_s = sb(st, "gfb_s", [128, D], F32)
        g2c_s = sb(st, "g2c_s", [128, 8], F32)
        dum = sb(st, "dum", [128, 8], F32)
        banks = [ps(st, f"bank{i}", [128, 512], F32) for i in range(8)]

        def dummies():
            return {
                "dve": lambda e: e.memset(dum[:, 0:1], 0.0),
                "act": lambda e: e.activation(out=dum[:, 1:2], in_=identf[:, 0:1], func=AF.Copy),
                "pool": lambda e: e.memset(dum[:, 3:4], 0.0),
            }

        with contextlib.ExitStack() as sa:
            S = Sched(nc, semst, "A")
            bank_i = [0]
            reserved = set()

            def nb():
                while True:
                    i = bank_i[0] % 8
                    bank_i[0] += 1
                    if i not in reserved:
                        return i

            win = wa[:, 0:8 * NIN].rearrange("p (k f) -> p k f", k=8)
            wout = sb(sa, "wout", [128, 8, D], BF16)
            dconv = sb(sa, "dconv", [128, 8, 4, 128], BF16)
            convT_s = sb(sa, "convT_s", [128, 8, 4], F32)
            g1c_s = sb(sa, "g1c_s", [128, 8], F32)
            wg = sb(sa, "wg", [128, 8, 8], BF16)
            hngb_s = sb(sa, "hngb_s", [128, 512], F32)
            poolw = sb(sa, "poolw", [128, 4, 128], BF16)
            pscale_s = sb(sa, "pscale_s", [128, 4], F32)
            wr = sb(sa, "wr", [128, 8, NE], F32)
            brb_s = sb(sa, "brb_s", [128, NE], F32)
            maskb = sb(sa, "maskb", [128, 128], BF16)
            maskf = sb(sa, "maskf", [128, 128], F32)
            corr_s = sb(sa, "corr_s", [128, 4, 16], F32)
            flag_s = sb(sa, "flag_s", [128, 1], F32)
            igb_s = sb(sa, "igb_s", [128, 1], F32)
            fgbn_s = sb(sa, "fgbn_s", [128, 1], F32)
            resetm_s = sb(sa, "resetm_s", [128, 128], F32)
            ones1 = sb(sa, "ones1", [1, 128], F32)
            rstd1 = sb(sa, "rstd1", [128, 32], F32)
            ssq = sb(sa, "ssq", [128, 48], F32)
            xin = [sb(sa, "xin0", [128, D], F32)] * 2
            hs = [sb(sa, f"hs{i}", [128, D], BF16) for i in range(2)]
            hT = [sb(sa, f"hT{i}", [128, 8, 128], BF16) for i in range(2)]
            gcol = sb(sa, "gcol", [128, 8], F32)
            grow = sb(sa, "grow", [1, 12, 128], F32)
            tokT = sb(sa, "tokT", [128, 3, 128], F32)
            bcs = sb(sa, "bcs", [128, 3, 128], F32)
            ubc = [sb(sa, f"ubc{i}", [128, 4, 3 + 128], BF16) for i in range(2)]
            halo_c = sb(sa, "halo_c", [128, 8, 3], BF16)
            qkT = [sb(sa, f"qkT{i}", [128, 8, 128], BF16) for i in range(2)]
            kTok = [sb(sa, f"kTok{i}", [128, 4, 128], BF16) for i in range(2)]
            v1 = [sb(sa, f"v1{i}", [128, 4, 129], BF16) for i in range(2)]
            osig = [sb(sa, f"osig{i}", [128, 512], BF16) for i in range(2)]
            Cst = sb(sa, "Cst", [128, 4, 129], F32)
            Ctmp = sb(sa, "Ctmp", [128, 4, 129], F32)
            Cs = sb(sa, "Cs", [128, 4, 129], BF16)
            sTp = [sb(sa, f"sTp{i}", [128, 128], BF16) for i in range(2)]
            wv = [sb(sa, f"wv{i}", [128, 129], BF16) for i in range(2)]
            dmx = sb(sa, "dmx", [128, 8], F32)
            hm = sb(sa, "hm", [128, 4, 128], F32)
            bst = sb(sa, "bst", [128, 4, 6], F32)
            mv = sb(sa, "mv", [128, 4, 2], F32)
            lnr = sb(sa, "lnr", [128, 4], F32)
            ym = sb(sa, "ym", [128, 512], BF16)
            yT = [sb(sa, f"yT{i}", [128, 8, 128], BF16) for i in range(2)]
            pu = [sb(sa, f"pu{i}", [128, 16 + 128], F32) for i in range(4)]
            phalo = sb(sa, "phalo", [128, 4, 16], F32)
            pooledT = sb(sa, "pooledT", [128, 128], BF16)
            h2f = sb(sa, "h2f", [128, D], F32)
            ymf = h2f[:, 0:512]
            gm = [hm[:, 0, :], hm[:, 1, :], hm[:, 2, :], hm[:, 3, :], h2f[:, 0:128], h2f[:, 128:256]]
            h2Tf = sb(sa, "h2Tf", [128, 8, 128], F32)
            gsb = h2Tf[:, 0:2, :]
            lg = sb(sa, "lg", [128, NE], F32)
            top8 = sb(sa, "top8", [128, 8], F32)
            rt = sb(sa, "rt", [128, 4, NE], F32)
            rsm = sb(sa, "rsm", [128, 4], F32)

            klim = float(os.environ.get("KSTAGE", "99")) if debug else 99

            def ckpt(k):
                if klim <= k:
                    S.stopped = True
            def ld(eng, dst, src, wkey, rk=()):
                return S.add(eng, lambda e: e.dma_start(out=dst, in_=src), reads=rk, writes=[wkey], dma=True)

            ld("sp", identf[:], ident, "identf")
            S.add("dve", lambda e: e.tensor_copy(out=identb[:], in_=identf[:]), reads=["identf"], writes=["identb"])
            ld("sp", maskf[:], maskrl, "maskf")
            S.add("dve", lambda e: e.tensor_copy(out=maskb[:], in_=maskf[:]), reads=["maskf"], writes=["maskb"])
            for dst, src, k in ((g1c_s, g1c, "g1c"), (g2c_s, g2c, "g2c"), (gfb_s, gfb, "gfb"), (convT_s, convT, "convT"),
                                (hngb_s, hngb, "hngb"), (pscale_s, pscale, "pscale"), (brb_s, brb, "brb"),
                                (corr_s, corr, "corr"), (flag_s, flag, "flag"), (igb_s, igb, "igb"),
                                (fgbn_s, fgbn, "fgbn"), (resetm_s, resetm, "resetm")):
                ld("sp", dst[:], src, k)
            ld("sp", wr[:], w_router.rearrange("(k p) e -> p k e", p=128), "wr")
            S.add("dve", lambda e: e.tensor_scalar(out=fgbn_s[:], in0=fgbn_s[:], scalar1=-1.0, scalar2=None, op0=ALU.mult),
                  reads=["fgbn"], writes=["fgbn"])
            w_in_v = w_in.rearrange("(k p) f -> p k f", p=128)
            ld("pool", wg[:], w_in_v[:, :, 2048:2056], "wg")
            for k in range(8):
                S.add("dve", lambda e, k=k: e.tensor_scalar(out=wg[:, k, :], in0=wg[:, k, :], scalar1=g1c_s[:, k:k + 1],
                                                            scalar2=None, op0=ALU.mult),
                      reads=["g1c", "wg"], writes=["wg"], cost=0.1)
            for k in range(8):
                ld("pool", win[:, k, :], w_in_v[:, k, :], ("wa", k))
            ld("pool", wout[:], w_out.rearrange("(k p) f -> p k f", p=128), "wout")
            ld("pool", poolw[:], pool_w.rearrange("g c d -> c g d"), "poolw")
            for k in range(8):
                if k % 2:
                    S.add("dve", lambda e, k=k: e.tensor_scalar(out=win[:, k, :], in0=win[:, k, :], scalar1=g1c_s[:, k:k + 1],
                                                                scalar2=None, op0=ALU.mult),
                          reads=["g1c", ("wa", k)], writes=[("wa", k)])
                else:
                    S.add("act", lambda e, k=k: e.activation(out=win[:, k, :], in_=win[:, k, :], func=AF.Copy,
                                                             scale=g1c_s[:, k:k + 1]),
                          reads=["g1c", ("wa", k)], writes=[("wa", k)])
            for k in range(8):
                for j in range(4):
                    S.add("dve", lambda e, k=k, j=j: e.tensor_scalar(out=dconv[:, k, j, :], in0=identf[:],
                                                                       scalar1=convT_s[:, k, j:j + 1], scalar2=None,
                                                                       op0=ALU.mult),
                          reads=["identf", "convT"], writes=[("dconv", k)])
            S.add("dve", lambda e: e.memset(ones1[:], 1.0), writes=["ones1"])
            S.add("dve", lambda e: e.memset(Cst[:], 0.0), writes=[("Cst", h) for h in range(4)])
            S.add("dve", lambda e: e.memset(halo_c[:], 0.0), writes=[("halo_c", 0), ("halo_c", 1)])
            S.add("dve", lambda e: e.memset(phalo[:], 0.0), writes=[("phalo", g) for g in range(4)])
            S.add("dve", lambda e: e.memset(v1[0][:], 1.0), writes=[("v1", 0)])
            S.add("dve", lambda e: e.memset(v1[1][:], 1.0), writes=[("v1", 1)])
            WA = [("wa", k) for k in range(8)]

            ckpt(1)
            cnt = [0]

            def norm_T(ci, first):
                i = cnt[0] % 2
                cnt[0] += 1
                own = ci >= 16
                if own:
                    xt, xkey = x1[:, ci - 16, :], ("x1", ci - 16)
                elif ci % 2 == 0:
                    xt, xkey = xin[0][:], ("xin", 0)
                else:
                    xt, xkey = h2f[:], "h2f"
                if first or not own:
                    src = (xo if own else xp)[(ci % 16) * 128:(ci % 16 + 1) * 128, :]
                    S.add("sp", lambda e: e.dma_start(out=xt, in_=src), writes=[xkey], dma=True)
                if first:
                    S.add("act", lambda e: e.activation(out=hs[i][:], in_=xt, func=AF.Square,
                                                        accum_out=ssq[:, ci:ci + 1]),
                          reads=[xkey], writes=[("hs", i), ("ssq", ci)], cost=1.0)
                    S.add("act", lambda e: e.activation(out=ssq[:, ci:ci + 1], in_=ssq[:, ci:ci + 1], func=AF.Ln,
                                                        scale=1.0 / D, bias=EPS),
                          reads=[("ssq", ci)], writes=[("ssq", ci)])
                    S.add("act", lambda e: e.activation(out=rstd1[:, ci:ci + 1], in_=ssq[:, ci:ci + 1], func=AF.Exp,
                                                        scale=-0.5),
                          reads=[("ssq", ci)], writes=[("rstd1", ci)])
                if first:
                    S.add("dve", lambda e: e.tensor_scalar(out=hs[i][:], in0=xt, scalar1=rstd1[:, ci:ci + 1],
                                                           scalar2=None, op0=ALU.mult),
                          reads=[xkey, ("rstd1", ci)], writes=[("hs", i)], cost=1.1)
                else:
                    S.add("act", lambda e: e.activation(out=hs[i][:], in_=xt, func=AF.Copy, scale=rstd1[:, ci:ci + 1]),
                          reads=[xkey, ("rstd1", ci)], writes=[("hs", i)], cost=1.1)
                b = nb()
                pb = banks[b][:].bitcast(BF16)

                def tr(e):
                    for k in range(8):
                        ins = e.transpose(out=pb[:, k * 128:(k + 1) * 128], in_=hs[i][:, k * 128:(k + 1) * 128],
                                          identity=identb[:])
                    return ins
                S.add("pe", tr, reads=[("hs", i), "identb"], writes=[("bank", b)], cost=0.8)
                S.add("dve", lambda e: e.tensor_copy(out=hT[i][:].rearrange("p k t -> p (k t)"), in_=pb[:, 0:1024]),
                      reads=[("bank", b)], writes=[("hT", i)], cost=0.6)
                return i

            gb = nb()
            reserved.add(gb)
            gall = banks[gb][:, 0:256].rearrange("p (c a) -> p c a", a=8)
            for ci in range(32):
                i = norm_T(ci, True)

                def gm_(e, i=i, ci=ci):
                    for k in range(8):
                        ins = e.matmul(gall[:, ci, :], lhsT=hT[i][:, k, :], rhs=wg[:, k, :],
                                       start=(k == 0), stop=(k == 7), skip_group_check=True)
                    return ins
                S.add("pe", gm_, reads=[("hT", i), "wg"], writes=[("bank", gb)])
            ckpt(2)
            for a in range(2):
                S.add("dve", lambda e, a=a: e.tensor_copy(out=gsb[:, a, :].rearrange("p (c h) -> p c h", h=4),
                                                          in_=gall[:, :, a * 4:(a + 1) * 4]),
                      reads=[("bank", gb)], writes=[("gsb", a)])
            reserved.discard(gb)
            tb = nb()
            tps = banks[tb]

            def trg(e):
                e.transpose(out=tps[:, 0:128], in_=gsb[:, 0, :], identity=identf[:])
                return e.transpose(out=tps[:, 128:256], in_=gsb[:, 1, :], identity=identf[:])
            S.add("pe", trg, reads=[("gsb", 0), ("gsb", 1), "identf"], writes=[("bank", tb)])
            IG, BP, DD, T0, T1, T2 = gm
            S.add("act", lambda e: e.activation(out=IG[:], in_=tps[:, 0:128], func=AF.Identity, bias=igb_s[:, 0:1]),
                  reads=[("bank", tb), "igb"], writes=["IG"])
            S.add("act", lambda e: e.activation(out=T0[:], in_=tps[:, 128:256], func=AF.Exp, scale=-1.0,
                                                bias=fgbn_s[:, 0:1]),
                  reads=[("bank", tb), "fgbn"], writes=["T0"])
            S.add("act", lambda e: e.activation(out=T0[:], in_=T0[:], func=AF.Ln, bias=1.0),
                  reads=["T0"], writes=["T0"])
            S.add("dve", lambda e: e.tensor_tensor_scan(out=BP[:], data0=resetm_s[:], data1=T0[:], initial=0.0,
                                                        op0=ALU.mult, op1=ALU.add),
                  reads=["T0", "resetm"], writes=["BP"])
            S.add("dve", lambda e: e.tensor_tensor(out=DD[:], in0=IG[:], in1=BP[:], op=ALU.add),
                  reads=["IG", "BP"], writes=["DD"])
            S.add("dve", lambda e: e.tensor_reduce(out=gcol[:, 0:1], in_=DD[:], axis=AX.X, op=ALU.max),
                  reads=["DD"], writes=["gcol"])
            S.add("dve", lambda e: e.tensor_scalar(out=gcol[:, 1:2], in0=BP[:, 127:128], scalar1=-1.0, scalar2=None,
                                                   op0=ALU.mult),
                  reads=["BP", "gcol"], writes=["gcol"])
            S.add("dve", lambda e: e.tensor_tensor(out=gcol[:, 2:3], in0=gcol[:, 0:1], in1=gcol[:, 1:2], op=ALU.add),
                  reads=["gcol"], writes=["gcol"])
            rb = nb()
            rps = banks[rb]

            def trc(e):
                for q in range(3):
                    ins = e.transpose(out=rps[0:1, q * 128:(q + 1) * 128], in_=gcol[:, q:q + 1], identity=identf[:])
                return ins
            S.add("pe", trc, reads=["gcol", "identf"], writes=[("bank", rb)])
            S.add("dve", lambda e: e.tensor_copy(out=grow[:, 0:3, :].rearrange("p a c -> p (a c)"),
                                                 in_=rps[0:1, 0:384]),
                  reads=[("bank", rb)], writes=["grow"])
            R = lambda q: grow[0:1, q, :]
            for h in range(4):
                S.add("dve", lambda e, h=h: e.tensor_tensor_scan(out=grow[0:1, 3, h:64:4], data0=grow[0:1, 1, h:64:4],
                                                                 data1=grow[0:1, 2, h:64:4], initial=0.0,
                                                                 op0=ALU.add, op1=ALU.max),
                      reads=["grow"], writes=["grow"])
            S.add("dve", lambda e: e.memset(grow[0:1, 4, 0:4], 0.0), reads=["grow"], writes=["grow"])
            S.add("dve", lambda e: e.tensor_copy(out=grow[0:1, 4, 4:64], in_=grow[0:1, 3, 0:60]),
                  reads=["grow"], writes=["grow"])
            S.add("dve", lambda e: e.tensor_scalar(out=grow[0:1, 4, 64:68], in0=grow[0:1, 3, 60:64],
                                                   scalar1=flag_s[0:1, 0:1], scalar2=None, op0=ALU.mult),
                  reads=["grow", "flag"], writes=["grow"])
            for h in range(4):
                S.add("dve", lambda e, h=h: e.tensor_tensor_scan(out=grow[0:1, 3, 64 + h:128:4],
                                                                 data0=grow[0:1, 1, 64 + h:128:4],
                                                                 data1=grow[0:1, 2, 64 + h:128:4],
                                                                 initial=grow[0:1, 4, 64 + h:65 + h],
                                                                 op0=ALU.add, op1=ALU.max),
                      reads=["grow"], writes=["grow"])
            S.add("dve", lambda e: e.tensor_copy(out=grow[0:1, 4, 68:128], in_=grow[0:1, 3, 64:124]),
                  reads=["grow"], writes=["grow"])
            S.add("dve", lambda e: e.tensor_tensor(out=R(5), in0=R(4), in1=R(0), op=ALU.max), reads=["grow"], writes=["grow"])
            S.add("dve", lambda e: e.tensor_tensor(out=R(9), in0=R(1), in1=R(4), op=ALU.add), reads=["grow"], writes=["grow"])
            S.add("dve", lambda e: e.tensor_tensor(out=R(9), in0=R(9), in1=R(3), op=ALU.subtract), reads=["grow"], writes=["grow"])
            S.add("act", lambda e: e.activation(out=R(6), in_=R(9), func=AF.Exp), reads=["grow"], writes=["grow"])
            S.add("dve", lambda e: e.tensor_tensor(out=R(9), in0=R(2), in1=R(3), op=ALU.subtract), reads=["grow"], writes=["grow"])
            S.add("act", lambda e: e.activation(out=R(7), in_=R(9), func=AF.Exp), reads=["grow"], writes=["grow"])
            S.add("dve", lambda e: e.tensor_tensor(out=R(9), in0=R(4), in1=R(5), op=ALU.subtract), reads=["grow"], writes=["grow"])
            S.add("act", lambda e: e.activation(out=R(8), in_=R(9), func=AF.Exp), reads=["grow"], writes=["grow"])
            S.add("dve", lambda e: e.tensor_scalar(out=R(10), in0=R(5), scalar1=-1.0, scalar2=LNSC, op0=ALU.mult, op1=ALU.add),
                  reads=["grow"], writes=["grow"])
            S.add("dve", lambda e: e.tensor_scalar(out=R(11), in0=R(0), scalar1=-1.0, scalar2=LNSC, op0=ALU.mult, op1=ALU.add),
                  reads=["grow"], writes=["grow"])
            S.add("dve", lambda e: e.tensor_scalar(out=R(9), in0=R(5), scalar1=-1.0, scalar2=None, op0=ALU.mult),
                  reads=["grow"], writes=["grow"])
            bb = nb()
            bps = banks[bb]

            def bc(e):
                for q in range(3):
                    ins = e.matmul(bps[:, q * 128:(q + 1) * 128], lhsT=ones1[0:1, :], rhs=R(6 + q), start=True, stop=True,
                                   skip_group_check=True)
                return ins
            S.add("pe", bc, reads=["grow", "ones1"], writes=[("bank", bb)])
            S.add("dve", lambda e: e.tensor_copy(out=bcs[:].rearrange("p a c -> p (a c)"), in_=bps[:, 0:384]),
                  reads=[("bank", bb)], writes=["bcs"])
            cb = nb()
            cps = banks[cb]

            def trr(e):
                for q in range(3):
                    ins = e.matmul(cps[:, q:q + 1], lhsT=R(9 + q), rhs=ones1[0:1, 0:1], start=True, stop=True,
                                   skip_group_check=True)
                return ins
            S.add("pe", trr, reads=["grow", "ones1"], writes=[("bank", cb)])
            S.add("dve", lambda e: e.tensor_copy(out=gcol[:, 3:6], in_=cps[:, 0:3]), reads=[("bank", cb)], writes=["gcol"])
            S.add("act", lambda e: e.activation(out=T0[:], in_=DD[:], func=AF.Exp, bias=gcol[:, 4:5]),
                  reads=["DD", "gcol"], writes=["T0"])
            S.add("act", lambda e: e.activation(out=T1[:], in_=DD[:], func=AF.Exp, bias=gcol[:, 5:6]),
                  reads=["DD", "gcol"], writes=["T1", "h2f"])
            S.add("act", lambda e: e.activation(out=T2[:], in_=BP[:], func=AF.Exp, bias=gcol[:, 3:4]),
                  reads=["BP", "gcol"], writes=["T2", "h2f"])
            kb = nb()
            kps = banks[kb]

            def trt(e):
                for q, t in enumerate((T0, T1, T2)):
                    ins = e.transpose(out=kps[:, q * 128:(q + 1) * 128], in_=t[:], identity=identf[:])
                return ins
            S.add("pe", trt, reads=["T0", "T1", "T2", "h2f", "identf"], writes=[("bank", kb)])
            S.add("dve", lambda e: e.tensor_copy(out=tokT[:].rearrange("p a c -> p (a c)"), in_=kps[:, 0:384]),
                  reads=[("bank", kb)], writes=["tokT"])

            ucnt = [0]

            def proj_conv(i, cg, p, etmp, ekey, save_only=False):
                b = nb()
                pb = banks[b]

                def mm(e):
                    for c in range(4):
                        fk = cg * 4 + c
                        for k in range(8):
                            ins = e.matmul(pb[:, c * 128:(c + 1) * 128], lhsT=win[:, k, fk * 128:(fk + 1) * 128],
                                           rhs=hT[i][:, k, :], start=(k == 0), stop=(k == 7), skip_group_check=True)
                    return ins
                S.add("pe", mm, reads=[("hT", i)] + WA, writes=[("bank", b)], cost=2.6)
                u = ucnt[0] % 2
                ucnt[0] += 1
                S.add("dve", lambda e: e.tensor_copy(out=ubc[u][:, :, 0:3], in_=halo_c[:, cg * 4:(cg + 1) * 4, :]),
                      reads=[("halo_c", cg)], writes=[("ubc", u)])
                S.add("act", lambda e: e.activation(out=ubc[u][:, :, 3:131], in_=pb[:, :].rearrange("p (c t) -> p c t", c=4),
                                                    func=AF.Copy),
                      reads=[("bank", b)], writes=[("ubc", u)], cost=0.6)
                S.add("dve", lambda e: e.tensor_copy(out=halo_c[:, cg * 4:(cg + 1) * 4, :], in_=ubc[u][:, :, 128:131]),
                      reads=[("ubc", u)], writes=[("halo_c", cg)])
                if save_only:
                    return
                b2 = nb()
                pb2 = banks[b2]

                def cv(e):
                    for c in range(4):
                        fk = cg * 4 + c
                        for j in range(4):
                            ins = e.matmul(pb2[:, c * 128:(c + 1) * 128], lhsT=dconv[:, fk, j, :], rhs=ubc[u][:, c, j:j + 128],
                                           start=(j == 0), stop=(j == 3), skip_group_check=True)
                    return ins
                S.add("pe", cv, reads=[("ubc", u)] + [("dconv", cg * 4 + c) for c in range(4)], writes=[("bank", b2)], cost=1.4)
                S.add("act", lambda e: e.activation(out=etmp, in_=pb2[:, :], func=AF.Exp, scale=-1.0),
                      reads=[("bank", b2)], writes=[ekey], cost=0.6)
                S.add("act", lambda e: e.activation(out=etmp, in_=etmp, func=AF.Ln, bias=1.0),
                      reads=[ekey], writes=[ekey], cost=0.6)
                S.add("act", lambda e: e.activation(out=etmp, in_=etmp, func=AF.Exp, scale=-1.0),
                      reads=[ekey], writes=[ekey], cost=0.6)
                S.add("dve", lambda e: e.tensor_tensor(out=qkT[p][:, cg * 4:(cg + 1) * 4, :].rearrange("p c t -> p (c t)"),
                                                       in0=pb2[:, :], in1=etmp, op=ALU.mult),
                      reads=[("bank", b2), ekey], writes=[("qkT", p, cg * 4 + c) for c in range(4)], cost=0.6)

            def proj_v(i, p):
                b = nb()
                pb = banks[b]

                def mm(e):
                    for k in range(8):
                        ins = e.matmul(pb[:, :], lhsT=hT[i][:, k, :], rhs=win[:, k, 1024:1536], start=(k == 0), stop=(k == 7))
                    return ins
                S.add("pe", mm, reads=[("hT", i)] + WA, writes=[("bank", b)], cost=2.5)
                S.add("act", lambda e: e.activation(out=v1[p][:, :, 0:128], in_=pb[:, :].rearrange("p (h d) -> p h d", h=4),
                                                    func=AF.Copy),
                      reads=[("bank", b)], writes=[("v1", p)])

            def k_tok(p):
                b = nb()
                pb = banks[b][:].bitcast(BF16)

                def tr(e):
                    for h in range(4):
                        ins = e.transpose(out=pb[:, h * 128:(h + 1) * 128], in_=qkT[p][:, 4 + h, :], identity=identb[:])
                    return ins
                S.add("pe", tr, reads=[("qkT", p, 4 + h) for h in range(4)] + ["identb"], writes=[("bank", b)])
                S.add("dve", lambda e: e.tensor_copy(out=kTok[p][:].rearrange("p h d -> p (h d)"), in_=pb[:, 0:512]),
                      reads=[("bank", b)], writes=[("kTok", p)])

            def pool_proj(i, g, dst_fn):
                b = nb()
                pb = banks[b]

                def mm(e):
                    for k in range(8):
                        ins = e.matmul(pb[:, 0:128], lhsT=win[:, k, 2056 + g * 128:2056 + (g + 1) * 128],
                                       rhs=hT[i][:, k, :], start=(k == 0), stop=(k == 7))
                    return ins
                S.add("pe", mm, reads=[("hT", i)] + WA, writes=[("bank", b)])
                return b, pb

            def state_update(ci, p):
                for h in range(4):
                    ch = ci * 4 + h
                    w_ = wv[h % 2]
                    S.add("act", lambda e, h=h, ch=ch, w_=w_: e.activation(out=w_[:], in_=v1[p][:, h, :], func=AF.Copy,
                                                                           scale=tokT[:, 1, ch:ch + 1]),
                          reads=[("v1", p), "tokT"], writes=[("wv", h % 2)])
                    b = nb()
                    pb = banks[b]
                    S.add("pe", lambda e, h=h, w_=w_, pb=pb: e.matmul(pb[:, 0:129], lhsT=kTok[p][:, h, :], rhs=w_[:],
                                                                      start=True, stop=True),
                          reads=[("kTok", p), ("wv", h % 2)], writes=[("bank", b)])
                    S.add("dve", lambda e, h=h, ch=ch: e.tensor_scalar(out=Ctmp[:, h, :], in0=Cst[:, h, :],
                                                                       scalar1=bcs[:, 0, ch:ch + 1], scalar2=None,
                                                                       op0=ALU.mult),
                          reads=[("Cst", h), "bcs"], writes=[("Ctmp", h)])
                    S.add("dve", lambda e, h=h, ch=ch, pb=pb: e.scalar_tensor_tensor(out=Cst[:, h, :], in0=pb[:, 0:129],
                                                                                     scalar=bcs[:, 1, ch:ch + 1],
                                                                                     in1=Ctmp[:, h, :], op0=ALU.mult,
                                                                                     op1=ALU.add),
                          reads=[("bank", b), ("Ctmp", h), "bcs"], writes=[("Cst", h)])

            def front_prefix(ci, p):
                i = norm_T(ci, False)
                et, ek = h2Tf[:, 4:8, :].rearrange("p k t -> p (k t)"), ("h2Tf", 4)
                proj_conv(i, 1, p, et, ek)
                if ci == 15:
                    proj_conv(i, 0, p, et, ek, save_only=True)
                    for g in range(4):
                        b, pb = pool_proj(i, g, None)
                        S.add("act", lambda e, g=g, pb=pb: e.activation(out=phalo[:, g, :], in_=pb[:, 112:128], func=AF.Copy),
                              reads=[("bank", b)], writes=[("phalo", g)])
                proj_v(i, p)
                k_tok(p)

            for ci in range(16):
                front_prefix(ci, ci % 2)
                if ci > 0:
                    state_update(ci - 1, (ci - 1) % 2)
            ckpt(4)

            def front_own(ti, p):
                ci = 16 + ti
                i = norm_T(ci, False)
                et, ek = xin[0][:, 0:512], ("xin", 0)
                proj_conv(i, 0, p, et, ek)
                proj_conv(i, 1, p, et, ek)
                proj_v(i, p)
                b = nb()
                pb = banks[b]

                def mm(e):
                    for k in range(8):
                        ins = e.matmul(pb[:, :], lhsT=hT[i][:, k, :], rhs=win[:, k, 1536:2048], start=(k == 0), stop=(k == 7))
                    return ins
                S.add("pe", mm, reads=[("hT", i)] + WA, writes=[("bank", b)], cost=2.5)
                S.add("act", lambda e: e.activation(out=et, in_=pb[:, :], func=AF.Exp, scale=-1.0),
                      reads=[("bank", b)], writes=[ek], cost=0.6)
                S.add("dve", lambda e: e.tensor_scalar(out=et, in0=et, scalar1=1.0, scalar2=None, op0=ALU.add),
                      reads=[ek], writes=[ek], cost=0.5)
                def rcp(e):
                    with nc.allow_low_precision("sigmoid gate is stored in bf16 (matmul-operand precision)"):
                        return e.reciprocal(out=osig[p][:], in_=et)
                S.add("dve", rcp, reads=[ek], writes=[("osig", p)], cost=0.5)
                k_tok(p)
                for g in range(4):
                    b, pbg = pool_proj(i, g, None)
                    A_ = pu[0]
                    S.add("dve", lambda e, g=g: e.tensor_copy(out=A_[:, 0:16], in_=phalo[:, g, :]),
                          reads=[("phalo", g)], writes=["puA"])
                    S.add("act", lambda e, pbg=pbg: e.activation(out=A_[:, 16:144], in_=pbg[:, 0:128], func=AF.Copy),
                          reads=[("bank", b)], writes=["puA"])
                    S.add("dve", lambda e, g=g: e.tensor_copy(out=phalo[:, g, :], in_=A_[:, 128:144]),
                          reads=["puA"], writes=[("phalo", g)])
                    src_t, src_k = A_, "puA"
                    sh, lo = 1, 1
                    for s_ in range(g + 1):
                        dst_t = pu[1 + (s_ % 3)]
                        dk = "pu%d" % (1 + (s_ % 3))
                        S.add("dve", lambda e, src_t=src_t, dst_t=dst_t, sh=sh, lo=lo: e.tensor_tensor(
                            out=dst_t[:, lo:144], in0=src_t[:, lo:144], in1=src_t[:, lo - sh:144 - sh], op=ALU.add),
                            reads=[src_k], writes=[dk])
                        src_t, src_k = dst_t, dk
                        sh *= 2
                        lo = 2 * sh - 1
                    wdw = float(2 ** (g + 1))
                    if ti == 0:
                        S.add("dve", lambda e, src_t=src_t, g=g: e.tensor_tensor(out=src_t[:, 16:32], in0=src_t[:, 16:32],
                                                                                 in1=corr_s[:, g, :], op=ALU.mult),
                              reads=[src_k, "corr"], writes=[src_k])
                    S.add("dve", lambda e, src_t=src_t, wdw=wdw: e.scalar_tensor_tensor(
                        out=pooledT[:], in0=src_t[:, 16:144], scalar=1.0 / wdw, in1=A_[:, 16:144], op0=ALU.mult,
                        op1=ALU.subtract),
                        reads=[src_k, "puA"], writes=["pooledT"])
                    b2 = nb()
                    pb2 = banks[b2]
                    S.add("pe", lambda e, g=g, pb2=pb2: e.matmul(pb2[:, 0:128], lhsT=poolw[:, g, :], rhs=pooledT[:],
                                                                 start=True, stop=True),
                          reads=["pooledT", "poolw"], writes=[("bank", b2)])
                    S.add("act", lambda e, g=g, pb2=pb2: e.activation(out=yT[p][:, 4 + g, :], in_=pb2[:, 0:128], func=AF.Copy,
                                                                      scale=pscale_s[:, g:g + 1]),
                          reads=[("bank", b2), "pscale"], writes=[("yT", p, 4 + g)])

            def back_own(ti, p):
                ci = 16 + ti
                for h in range(4):
                    ch = ci * 4 + h
                    b = nb()
                    pb = banks[b]
                    S.add("pe", lambda e, h=h, pb=pb: e.matmul(pb[:, 0:128], lhsT=qkT[p][:, 4 + h, :], rhs=qkT[p][:, h, :],
                                                               start=True, stop=True),
                          reads=[("qkT", p, 4 + h), ("qkT", p, h)], writes=[("bank", b)])
                    s_ = sTp[h % 2]
                    S.add("dve", lambda e, ch=ch, pb=pb, s_=s_: e.scalar_tensor_tensor(
                        out=s_[:], in0=pb[:, 0:128], scalar=tokT[:, 0, ch:ch + 1], in1=maskb[:], op0=ALU.mult, op1=ALU.mult),
                        reads=[("bank", b), "tokT", "maskb"], writes=[("sTp", h % 2)])
                    S.add("act", lambda e, h=h, ch=ch: e.activation(out=Cs[:, h, :], in_=Cst[:, h, :], func=AF.Copy,
                                                                    scale=bcs[:, 2, ch:ch + 1]),
                          reads=[("Cst", h), "bcs"], writes=[("Cs", h)])
                    b2 = nb()
                    pb2 = banks[b2]

                    def nd(e, h=h, pb2=pb2, s_=s_):
                        e.matmul(pb2[:, 0:129], lhsT=s_[:], rhs=v1[p][:, h, :], start=True, stop=False)
                        return e.matmul(pb2[:, 0:129], lhsT=qkT[p][:, h, :], rhs=Cs[:, h, :], start=False, stop=True)
                    S.add("pe", nd, reads=[("sTp", h % 2), ("v1", p), ("qkT", p, h), ("Cs", h)], writes=[("bank", b2)])
                    S.add("dve", lambda e, h=h, pb2=pb2: e.tensor_scalar(out=dmx[:, h:h + 1], in0=pb2[:, 128:129], scalar1=-1.0,
                                                                         scalar2=None, op0=ALU.mult),
                          reads=[("bank", b2)], writes=[("dmx", h)])
                    S.add("dve", lambda e, h=h, pb2=pb2: e.tensor_tensor(out=dmx[:, h:h + 1], in0=pb2[:, 128:129],
                                                                         in1=dmx[:, h:h + 1], op=ALU.max),
                          reads=[("bank", b2), ("dmx", h)], writes=[("dmx", h)])
                    S.add("dve", lambda e, h=h, ch=ch: e.tensor_scalar(
                        out=dmx[:, h:h + 1], in0=dmx[:, h:h + 1], scalar1=tokT[:, 2, ch:ch + 1], scalar2=None,
                        op0=ALU.max),
                        reads=[("dmx", h), "tokT"], writes=[("dmx", h)])
                    S.add("dve", lambda e, h=h: e.reciprocal(out=dmx[:, 4 + h:5 + h], in_=dmx[:, h:h + 1]),
                          reads=[("dmx", h)], writes=[("dmx", 4 + h)])
                    S.add("act", lambda e, h=h, pb2=pb2: e.activation(out=hm[:, h, :], in_=pb2[:, 0:128], func=AF.Copy,
                                                                      scale=dmx[:, 4 + h:5 + h]),
                          reads=[("bank", b2), ("dmx", 4 + h)], writes=[("hm", h)])
                    S.add("dve", lambda e, h=h: e.bn_stats(out=bst[:, h, :], in_=hm[:, h, :]),
                          reads=[("hm", h)], writes=[("bst", h)])
                    S.add("dve", lambda e, h=h: e.bn_aggr(out=mv[:, h, :], in_=bst[:, h, :]),
                          reads=[("bst", h)], writes=[("mv", h)])
                state_update(ci, p)
                S.add("act", lambda e: e.activation(out=lnr[:], in_=mv[:, :, 1], func=AF.Ln, bias=EPS),
                      reads=[("mv", h) for h in range(4)], writes=["lnr"])
                S.add("act", lambda e: e.activation(out=lnr[:], in_=lnr[:], func=AF.Exp, scale=-0.5),
                      reads=["lnr"], writes=["lnr"])
                for h in range(4):
                    S.add("dve", lambda e, h=h: e.tensor_scalar(out=h2f[:, h * 128:(h + 1) * 128], in0=hm[:, h, :],
                                                                scalar1=mv[:, h, 0:1], scalar2=lnr[:, h:h + 1],
                                                                op0=ALU.subtract, op1=ALU.mult),
                          reads=[("hm", h), ("mv", h), "lnr"], writes=["h2f"])
                S.add("dve", lambda e: e.tensor_tensor(out=ymf, in0=ymf, in1=hngb_s[:], op=ALU.mult),
                      reads=["h2f", "hngb"], writes=["h2f"])
                S.add("dve", lambda e: e.tensor_tensor(out=ym[:], in0=ymf, in1=osig[p][:], op=ALU.mult),
                      reads=["h2f", ("osig", p)], writes=["ym"])
                b = nb()
                pbb = banks[b][:].bitcast(BF16)

                def tr(e, pbb=pbb):
                    for h in range(4):
                        ins = e.transpose(out=pbb[:, h * 128:(h + 1) * 128], in_=ym[:, h * 128:(h + 1) * 128],
                                          identity=identb[:])
                    return ins
                S.add("pe", tr, reads=["ym", "identb"], writes=[("bank", b)])
                S.add("act", lambda e, pbb=pbb: e.activation(out=yT[p][:, 0:4, :].rearrange("p h t -> p (h t)"), in_=pbb[:, 0:512],
                                                             func=AF.Copy),
                      reads=[("bank", b)], writes=[("yT", p, h) for h in range(4)])
                for hf in range(2):
                    b = nb()
                    pb = banks[b]

                    def mm(e, hf=hf, pb=pb):
                        for k in range(8):
                            ins = e.matmul(pb[:, :], lhsT=yT[p][:, k, :], rhs=wout[:, k, hf * 512:(hf + 1) * 512],
                                           start=(k == 0), stop=(k == 7))
                        return ins
                    S.add("pe", mm, reads=[("yT", p, k) for k in range(8)] + ["wout"], writes=[("bank", b)], cost=2.5)
                    S.add("dve", lambda e, hf=hf, pb=pb: e.tensor_tensor(
                        out=x1[:, ti, hf * 512:(hf + 1) * 512], in0=pb[:, :], in1=x1[:, ti, hf * 512:(hf + 1) * 512], op=ALU.add),
                        reads=[("bank", b), ("x1", ti)], writes=[("x1", ti)])
                S.add("act", lambda e: e.activation(out=h2f[:], in_=x1[:, ti, :], func=AF.Square,
                                                    accum_out=ssq[:, 32 + ti:33 + ti]),
                      reads=[("x1", ti)], writes=["h2f", ("ssq", 32 + ti)])
                S.add("act", lambda e: e.activation(out=ssq[:, 32 + ti:33 + ti], in_=ssq[:, 32 + ti:33 + ti],
                                                    func=AF.Ln, scale=1.0 / D, bias=EPS),
                      reads=[("ssq", 32 + ti)], writes=[("ssq", 32 + ti)])
                S.add("act", lambda e: e.activation(out=rstd2[:, ti:ti + 1], in_=ssq[:, 32 + ti:33 + ti],
                                                    func=AF.Exp, scale=-0.5),
                      reads=[("ssq", 32 + ti)], writes=[("rstd2", ti)])
                S.add("act", lambda e: e.activation(out=h2f[:], in_=x1[:, ti, :], func=AF.Copy, scale=rstd2[:, ti:ti + 1]),
                      reads=[("x1", ti), ("rstd2", ti)], writes=["h2f"])
                for hf in range(2):
                    b = nb()
                    pb = banks[b]

                    def tr(e, hf=hf, pb=pb):
                        for j in range(4):
                            k = hf * 4 + j
                            ins = e.transpose(out=pb[:, j * 128:(j + 1) * 128], in_=h2f[:, k * 128:(k + 1) * 128],
                                              identity=identf[:])
                        return ins
                    S.add("pe", tr, reads=["h2f", "identf"], writes=[("bank", b)])
                    for j in range(4):
                        k = hf * 4 + j
                        S.add("act" if hf else "dve",
                              (lambda e, k=k, j=j, pb=pb: e.activation(out=h2Tf[:, k, :], in_=pb[:, j * 128:(j + 1) * 128],
                                                                       func=AF.Copy, scale=g2c_s[:, k:k + 1])) if hf else
                              (lambda e, k=k, j=j, pb=pb: e.tensor_scalar(out=h2Tf[:, k, :], in0=pb[:, j * 128:(j + 1) * 128],
                                                                          scalar1=g2c_s[:, k:k + 1], scalar2=None,
                                                                          op0=ALU.mult)),
                              reads=[("bank", b), "g2c"], writes=[("h2Tf", k)])
                b = nb()
                pb = banks[b]

                def rmm(e, pb=pb):
                    for k in range(8):
                        ins = e.matmul(pb[:, 0:NE], lhsT=h2Tf[:, k, :], rhs=wr[:, k, :], start=(k == 0), stop=(k == 7))
                    return ins
                S.add("pe", rmm, reads=[("h2Tf", k) for k in range(8)] + ["wr"], writes=[("bank", b)])
                S.add("dve", lambda e, pb=pb: e.tensor_tensor(out=lg[:], in0=pb[:, 0:NE], in1=brb_s[:], op=ALU.add),
                      reads=[("bank", b), "brb"], writes=["lg"])
                S.add("dve", lambda e: e.max(out=top8[:], in_=lg[:]), reads=["lg"], writes=["top8"])
                S.add("dve", lambda e: e.tensor_scalar(out=rt[:, 0, :], in0=lg[:], scalar1=top8[:, 3:4], scalar2=None,
                                                       op0=ALU.is_ge),
                      reads=["lg", "top8"], writes=["rt0"])
                S.add("dve", lambda e: e.tensor_scalar(out=rsm[:, 0:1], in0=top8[:, 0:1], scalar1=-1.0, scalar2=None,
                                                       op0=ALU.mult),
                      reads=["top8"], writes=["rsm0"])
                S.add("act", lambda e: e.activation(out=rt[:, 1, :], in_=lg[:], func=AF.Exp, bias=rsm[:, 0:1]),
                      reads=["lg", "rsm0"], writes=["rt1"])
                S.add("dve", lambda e: e.tensor_tensor(out=rt[:, 2, :], in0=rt[:, 1, :], in1=rt[:, 0, :], op=ALU.mult),
                      reads=["rt0", "rt1"], writes=["rt2"])
                S.add("dve", lambda e: e.tensor_reduce(out=rsm[:, 1:2], in_=rt[:, 2, :], axis=AX.X, op=ALU.add),
                      reads=["rt2"], writes=["rsm1"])
                S.add("dve", lambda e: e.reciprocal(out=rsm[:, 2:3], in_=rsm[:, 1:2]), reads=["rsm1"], writes=["rsm2"])
                S.add("dve", lambda e: e.tensor_scalar(out=G[:, ti, :], in0=rt[:, 2, :], scalar1=rsm[:, 2:3],
                                                       scalar2=None, op0=ALU.mult),
                      reads=["rt2", "rsm2"], writes=[("G", ti)])

            front_own(0, 0)
            state_update(15, 1)
            for h in range(4):
                S.add("dve", lambda e, h=h: e.tensor_scalar(out=Cst[:, h, :], in0=Cst[:, h, :], scalar1=flag_s[:, 0:1],
                                                            scalar2=None, op0=ALU.mult),
                      reads=[("Cst", h), "flag"], writes=[("Cst", h)])
            for ti in range(NT):
                if ti + 1 < NT:
                    front_own(ti + 1, (ti + 1) % 2)
                back_own(ti, ti % 2)
            S.stopped = False
            if debug:
                for ti in range(NT):
                    S.add("sp", lambda e, ti=ti: e.dma_start(out=dbg[ti * 128:(ti + 1) * 128, :], in_=x1[:, ti, :]),
                          reads=[("x1", ti)], dma=True)
                    S.add("sp", lambda e, ti=ti: e.dma_start(out=dbgG[ti * 128:(ti + 1) * 128, :], in_=G[:, ti, :]),
                          reads=[("G", ti)], dma=True)
            dm = dummies()
            dpb = banks[0]
            dm["pe"] = lambda e: e.matmul(dpb[0:1, 0:1], lhsT=ones1[0:1, 0:1], rhs=ones1[0:1, 0:1], start=True, stop=True,
                                          skip_group_check=True)
            S.emit(dm)

        with contextlib.ExitStack() as sbk:
            if debug and os.environ.get("KSKIPB"):
                return nc
            S = Sched(nc, semst, "B")
            S.ignore_cost = True
            bank_i = [0]

            def nb():
                i = bank_i[0] % 8
                bank_i[0] += 1
                return i
            h2 = sb(sbk, "h2", [128, NT, D], BF16)
            g2b_s = sb(sbk, "g2b_s", [128, D], F32)
            bgT_s = sb(sbk, "bgT_s", [128, NE, 8], F32)
            buT_s = sb(sbk, "buT_s", [128, NE, 8], F32)
            bdn = sb(sbk, "bdn", [NE, D], F32)
            GTs = [sb(sbk, f"GTs{i}", [NE, 128], F32) for i in range(2)]
            slot = sb(sbk, "slot", [128, NT, NE], F32)
            Ghl = sb(sbk, "Ghl", [128, NT, NE, 2], BF16)
            Mb = sb(sbk, "Mb", [128, NT, NE], BF16)
            Mf = [sb(sbk, f"Mf{i}", [128, NE], F32) for i in range(2)]
            Gr = [sb(sbk, f"Gr{i}", [128, NE], F32) for i in range(2)]
            iota_s = sb(sbk, "iota_s", [128, 128], F32)
            ltf = sb(sbk, "ltf", [128, 128], F32)
            lts = sb(sbk, "lts", [128, 128], BF16)
            onesb = sb(sbk, "onesb", [128, 128], BF16)
            P = [sb(sbk, "P0", [128, NT, 128], BF16)] * 2
            PT = [sb(sbk, f"PT{i}", [128, 4, 512], BF16) for i in range(2)]
            gsel = [sb(sbk, f"gsel{i}", [128, 4], F32) for i in range(2)]
            xTs = sb(sbk, "xTs", [128, 8, 512], BF16)
            aT = sb(sbk, "aT", [128, 8, 512], BF16)
            ysc = [sb(sbk, f"ysc{i}", [128, D], BF16) for i in range(2)]
            gc = [sb(sbk, "gc0", [128, 512], F32)] * 2
            sg = [sb(sbk, "sg0", [128, 512], F32)] * 2
            uc = [sb(sbk, "uc0", [128, 512], F32)] * 2
            ones1b = sb(sbk, "ones1b", [1, 8], F32)
            for dst, src, k in ((bgT_s, bgT, "bgT"), (buT_s, buT, "buT"), (bdn, b_down, "bdn"), (g2b_s, g2b, "g2b"),
                                (iota_s, iotaj, "iota"), (ltf, ltstrict, "ltf")):
                S.add("sp", lambda e, dst=dst, src=src: e.dma_start(out=dst[:], in_=src), writes=[k], dma=True)
            S.add("dve", lambda e: e.memset(ones1b[:], 1.0), writes=["ones1b"])
            S.add("dve", lambda e: e.memset(onesb[:], 1.0), writes=["onesb"])
            S.add("dve", lambda e: e.tensor_copy(out=lts[:], in_=ltf[:]), reads=["ltf"], writes=["lts"])
            S.add("dve", lambda e: e.tensor_scalar(out=buT_s[:], in0=buT_s[:], scalar1=1.0, scalar2=None, op0=ALU.add),
                  reads=["buT"], writes=["buT"])
            wsl = [wa[:, s * 8192:(s + 1) * 8192].rearrange("p (k f) -> p k f", k=8) for s in range(3)]
            wsrc = []
            for e_ in range(NE):
                wsrc += [w_gate[e_], w_up[e_], w_down[e_]]

            def wload(mi):
                s = mi % 3
                S.add("pool", lambda e, mi=mi, s=s: e.dma_start(out=wsl[s], in_=wsrc[mi].rearrange("(k p) f -> p k f", p=128)),
                      writes=[("ws", s)], dma=True, cost=14.0)
            if ne_run > 0:
                wload(0); wload(1); wload(2)
            for ti in range(NT):
                i = ti % 2
                S.add("dve", lambda e, ti=ti: e.scalar_tensor_tensor(out=h2[:, ti, :], in0=x1[:, ti, :],
                                                                     scalar=rstd2[:, ti:ti + 1], in1=g2b_s[:],
                                                                     op0=ALU.mult, op1=ALU.mult),
                      reads=[("x1", ti), "g2b", ("rstd2", ti)], writes=[("h2", ti)])
                S.add("dve", lambda e, ti=ti: e.tensor_scalar(out=Mb[:, ti, :], in0=G[:, ti, :], scalar1=0.0, scalar2=None,
                                                              op0=ALU.is_gt),
                      reads=[("G", ti)], writes=[("Mb", ti)])
                S.add("dve", lambda e, ti=ti: e.tensor_copy(out=Ghl[:, ti, :, 0], in_=G[:, ti, :]),
                      reads=[("G", ti)], writes=[("Ghl", ti)])
                S.add("dve", lambda e, ti=ti, i=i: e.tensor_tensor(out=Gr[i][:], in0=G[:, ti, :], in1=Ghl[:, ti, :, 0],
                                                                   op=ALU.subtract),
                      reads=[("G", ti), ("Ghl", ti)], writes=[("Gr", i)])
                S.add("dve", lambda e, ti=ti, i=i: e.tensor_copy(out=Ghl[:, ti, :, 1], in_=Gr[i][:]),
                      reads=[("Gr", i)], writes=[("Ghl", ti)])
                b = nb()
                pb = banks[b]
                S.add("pe", lambda e, ti=ti, pb=pb: e.transpose(out=pb[0:NE, 0:128], in_=G[:, ti, :], identity=identf[:]),
                      reads=[("G", ti)], writes=[("bank", b)])
                S.add("act", lambda e, i=i, pb=pb: e.activation(out=GTs[i][:], in_=pb[0:NE, 0:128], func=AF.Copy),
                      reads=[("bank", b)], writes=[("GTs", i)])
                for hf in range(2):
                    b = nb()
                    pb = banks[b]
                    S.add("pe", lambda e, i=i, hf=hf, pb=pb: e.matmul(pb[:, :], lhsT=GTs[i][:], rhs=bdn[:, hf * 512:(hf + 1) * 512],
                                                                      start=True, stop=True),
                          reads=[("GTs", i), "bdn"], writes=[("bank", b)])
                    S.add("dve", lambda e, ti=ti, hf=hf, pb=pb: e.tensor_tensor(
                        out=x1[:, ti, hf * 512:(hf + 1) * 512], in0=pb[:, :], in1=x1[:, ti, hf * 512:(hf + 1) * 512], op=ALU.add),
                        reads=[("bank", b), ("x1", ti)], writes=[("x1", ti)])
            for ti in range(NT):
                i = ti % 2
                prev = list(range(ti % 4, ti, 4))
                b = nb()
                pb = banks[b]

                def rk(e, ti=ti, prev=prev, pb=pb):
                    ins = e.matmul(pb[:, 0:NE], lhsT=lts[:], rhs=Mb[:, ti, :], start=True, stop=(not prev))
                    for tj in prev:
                        ins = e.matmul(pb[:, 0:NE], lhsT=onesb[:], rhs=Mb[:, tj, :], start=False, stop=(tj == prev[-1]))
                    return ins
                S.add("pe", rk, reads=[("Mb", tj) for tj in prev + [ti]] + ["lts", "onesb"], writes=[("bank", b)])
                S.add("dve", lambda e, ti=ti, i=i: e.tensor_scalar(out=Mf[i][:], in0=G[:, ti, :], scalar1=0.0, scalar2=None,
                                                                   op0=ALU.is_gt),
                      reads=[("G", ti)], writes=[("Mf", i)])
                S.add("dve", lambda e, ti=ti, i=i, pb=pb: e.scalar_tensor_tensor(out=slot[:, ti, :], in0=pb[:, 0:NE], scalar=1.0,
                                                                                 in1=Mf[i][:], op0=ALU.add, op1=ALU.mult),
                      reads=[("bank", b), ("Mf", i)], writes=[("slot", ti)])
                S.add("dve", lambda e, ti=ti: e.tensor_scalar(out=slot[:, ti, :], in0=slot[:, ti, :], scalar1=-1.0, scalar2=None,
                                                              op0=ALU.add),
                      reads=[("slot", ti)], writes=[("slot", ti)])
            H2 = [("h2", ti) for ti in range(NT)]
            for e_ in range(ne_run):
                sG, sU, sD = 0, 1, 2
                pi = e_ % 2
                Pe, PTe, gse = P[0], PT[pi], gsel[pi]
                for ti in range(NT):
                    S.add("dve", lambda e, ti=ti, e_=e_, Pe=Pe: e.tensor_scalar(out=Pe[:, ti, :], in0=iota_s[:],
                                                                               scalar1=slot[:, ti, e_:e_ + 1], scalar2=None,
                                                                               op0=ALU.is_equal),
                          reads=["iota", ("slot", ti)], writes=[("P", 0, ti % 4)], cost=0.2)
                for kc in range(8):
                    b = nb()
                    pb = banks[b]

                    def ga(e, kc=kc, pb=pb, Pe=Pe):
                        for g in range(4):
                            for r in range(4):
                                ins = e.matmul(pb[:, g * 128:(g + 1) * 128], lhsT=h2[:, 4 * r + g, kc * 128:(kc + 1) * 128],
                                               rhs=Pe[:, 4 * r + g, :], start=(r == 0), stop=(r == 3), skip_group_check=True)
                        return ins
                    S.add("pe", ga, reads=H2 + [("P", 0, g) for g in range(4)], writes=[("bank", b)], cost=1.6)
                    if True:
                        S.add("act", lambda e, kc=kc, pb=pb: e.activation(out=xTs[:, kc, :], in_=pb[:, :], func=AF.Copy),
                              reads=[("bank", b)], writes=[("xTs", kc)])
                    else:
                        S.add("dve", lambda e, kc=kc, pb=pb: e.tensor_copy(out=xTs[:, kc, :], in_=pb[:, :]),
                              reads=[("bank", b)], writes=[("xTs", kc)])
                for g in range(4):
                    b = nb()
                    pbb = banks[b][:].bitcast(BF16)

                    def trp(e, g=g, pbb=pbb, Pe=Pe):
                        for r in range(4):
                            ins = e.transpose(out=pbb[:, r * 128:(r + 1) * 128], in_=Pe[:, 4 * r + g, :], identity=identb[:])
                        return ins
                    S.add("pe", trp, reads=[("P", 0, g)], writes=[("bank", b)])
                    S.add("act", lambda e, g=g, pbb=pbb, PTe=PTe: e.activation(out=PTe[:, g, :], in_=pbb[:, 0:512], func=AF.Copy),
                          reads=[("bank", b)], writes=[("PT", pi, g)])
                b = nb()
                pbg = banks[b]

                def gs(e, pbg=pbg, Pe=Pe, e_=e_):
                    for g in range(4):
                        for r in range(4):
                            ins = e.matmul(pbg[:, 2 * g:2 * g + 2], lhsT=Pe[:, 4 * r + g, :], rhs=Ghl[:, 4 * r + g, e_, :],
                                           start=(r == 0), stop=(r == 3), skip_group_check=True)
                    return ins
                S.add("pe", gs, reads=[("P", 0, g) for g in range(4)] + [("Ghl", ti) for ti in range(NT)], writes=[("bank", b)])
                S.add("dve", lambda e, pbg=pbg, gse=gse: e.tensor_reduce(out=gse[:], in_=pbg[:, 0:8].rearrange("p (g two) -> p g two", two=2),
                                                                        axis=AX.X, op=ALU.add),
                      reads=[("bank", b)], writes=[("gsel", pi)])
                for fc in range(8):
                    j = 0
                    bg_, bu_ = nb(), nb()
                    pg, pu_ = banks[bg_], banks[bu_]

                    def mmg(e, fc=fc, pg=pg):
                        for k in range(8):
                            ins = e.matmul(pg[:, :], lhsT=wsl[sG][:, k, fc * 128:(fc + 1) * 128], rhs=xTs[:, k, :],
                                           start=(k == 0), stop=(k == 7))
                        return ins

                    def mmu(e, fc=fc, pu_=pu_):
                        for k in range(8):
                            ins = e.matmul(pu_[:, :], lhsT=wsl[sU][:, k, fc * 128:(fc + 1) * 128], rhs=xTs[:, k, :],
                                           start=(k == 0), stop=(k == 7))
                        return ins
                    XT = [("xTs", k) for k in range(8)]
                    S.add("pe", mmg, reads=[("ws", sG)] + XT, writes=[("bank", bg_)], cost=2.5)
                    S.add("pe", mmu, reads=[("ws", sU)] + XT, writes=[("bank", bu_)], cost=2.5)
                    S.add("dve", lambda e, fc=fc, e_=e_, pg=pg, j=j: e.tensor_scalar(
                        out=gc[j][:], in0=pg[:, :], scalar1=bgT_s[:, e_, fc:fc + 1], scalar2=7.0, op0=ALU.add, op1=ALU.min),
                        reads=[("bank", bg_), "bgT"], writes=[("gc", j)], cost=0.6)
                    S.add("act", lambda e, j=j: e.activation(out=sg[j][:], in_=gc[j][:], func=AF.Sigmoid, scale=1.702),
                          reads=[("gc", j)], writes=[("sg", j)], cost=0.6)
                    S.add("act", lambda e, fc=fc, e_=e_, pu_=pu_, j=j: e.activation(out=uc[j][:], in_=pu_[:, :], func=AF.Identity,
                                                                                    bias=buT_s[:, e_, fc:fc + 1]),
                          reads=[("bank", bu_), "buT"], writes=[("uc", j)], cost=0.7)
                    S.add("dve", lambda e, j=j: e.tensor_scalar(out=uc[j][:], in0=uc[j][:], scalar1=-6.0, scalar2=8.0,
                                                                op0=ALU.max, op1=ALU.min),
                          reads=[("uc", j)], writes=[("uc", j)], cost=0.6)
                    S.add("dve", lambda e, j=j: e.tensor_tensor(out=gc[j][:], in0=gc[j][:], in1=sg[j][:], op=ALU.mult),
                          reads=[("gc", j), ("sg", j)], writes=[("gc", j)], cost=0.6)
                    S.add("dve", lambda e, j=j, fc=fc: e.tensor_tensor(out=aT[:, fc, :], in0=gc[j][:], in1=uc[j][:], op=ALU.mult),
                          reads=[("gc", j), ("uc", j)], writes=[("aT", fc)], cost=0.6)
                if e_ + 1 < ne_run:
                    wload(3 * (e_ + 1)); wload(3 * (e_ + 1) + 1)
                AT = [("aT", k) for k in range(8)]
                for g in range(4):
                    yi = g % 2
                    for hf in range(2):
                        b = nb()
                        pb = banks[b]

                        def mmd(e, g=g, hf=hf, pb=pb):
                            for k in range(8):
                                ins = e.matmul(pb[:, :], lhsT=aT[:, k, g * 128:(g + 1) * 128],
                                               rhs=wsl[sD][:, k, hf * 512:(hf + 1) * 512], start=(k == 0), stop=(k == 7))
                            return ins
                        S.add("pe", mmd, reads=AT + [("ws", sD)], writes=[("bank", b)], cost=2.5)
                        S.add("act", lambda e, g=g, hf=hf, pb=pb, yi=yi, gse=gse: e.activation(
                            out=ysc[yi][:, hf * 512:(hf + 1) * 512], in_=pb[:, :], func=AF.Copy, scale=gse[:, g:g + 1]),
                            reads=[("bank", b), ("gsel", pi)], writes=[("ysc", yi)], cost=0.6)
                    for r in range(4):
                        ti = 4 * r + g
                        for hf in range(2):
                            b = nb()
                            pb = banks[b]
                            S.add("pe", lambda e, g=g, r=r, hf=hf, pb=pb, yi=yi, PTe=PTe: e.matmul(
                                pb[:, :], lhsT=PTe[:, g, r * 128:(r + 1) * 128], rhs=ysc[yi][:, hf * 512:(hf + 1) * 512],
                                start=True, stop=True),
                                reads=[("PT", pi, g), ("ysc", yi)], writes=[("bank", b)], cost=0.32)
                            S.add("dve", lambda e, ti=ti, hf=hf, pb=pb: e.tensor_tensor(
                                out=x1[:, ti, hf * 512:(hf + 1) * 512], in0=pb[:, :], in1=x1[:, ti, hf * 512:(hf + 1) * 512],
                                op=ALU.add),
                                reads=[("bank", b), ("x1", ti)], writes=[("x1", ti)], cost=0.6)
                if e_ + 1 < ne_run:
                    wload(3 * (e_ + 1) + 2)
            for ti in range(NT):
                S.add("act", lambda e, ti=ti: e.activation(out=h2[:, ti, :], in_=x1[:, ti, :], func=AF.Square,
                                                           accum_out=rstd2[:, ti:ti + 1]),
                      reads=[("x1", ti)], writes=[("h2", ti), ("rstd2", ti)])
                S.add("act", lambda e, ti=ti: e.activation(out=rstd2[:, ti:ti + 1], in_=rstd2[:, ti:ti + 1], func=AF.Ln,
                                                           scale=1.0 / D, bias=EPS),
                      reads=[("rstd2", ti)], writes=[("rstd2", ti)])
                S.add("act", lambda e, ti=ti: e.activation(out=rstd2[:, ti:ti + 1], in_=rstd2[:, ti:ti + 1], func=AF.Exp,
                                                           scale=-0.5),
                      reads=[("rstd2", ti)], writes=[("rstd2", ti)])
                S.add("dve", lambda e, ti=ti: e.scalar_tensor_tensor(out=x1[:, ti, :], in0=x1[:, ti, :],
                                                                     scalar=rstd2[:, ti:ti + 1], in1=gfb_s[:],
                                                                     op0=ALU.mult, op1=ALU.mult),
                      reads=[("x1", ti), ("rstd2", ti), "gfb"], writes=[("x1", ti)])
                S.add("sp", lambda e, ti=ti: e.dma_start(out=out[ti * 128:(ti + 1) * 128, :], in_=x1[:, ti, :]),
                      reads=[("x1", ti)], dma=True)
            dm = dummies()
            dpb = banks[0]
            dm["pe"] = lambda e: e.matmul(dpb[0:1, 0:1], lhsT=ones1b[0:1, 0:1], rhs=ones1b[0:1, 0:1], start=True, stop=True,
                                          skip_group_check=True)
            S.emit(dm)
    return nc


_NC = None


def _prep(inputs):
    f = lambda a: np.ascontiguousarray(np.asarray(a, dtype=np.float32))
    x = f(inputs["x"])
    rep = lambda v, n=128: f(np.broadcast_to(np.asarray(v, np.float32).reshape(1, -1), (n, np.asarray(v).size)))
    col = lambda v: f(np.asarray(v, np.float32).reshape(8, 128).T)
    common = {
        "w_in": f(inputs["w_in"][0]), "w_out": f(inputs["w_out"][0]), "pool_w": f(inputs["pool_w"][0]),
        "g1c": col(inputs["norm1_g"][0]), "g2c": col(inputs["norm2_g"][0]), "gfb": rep(inputs["normf_g"]),
        "convT": f(np.asarray(inputs["conv_w"][0], np.float32).T.reshape(8, 128, 4).transpose(1, 0, 2)),
        "igb": f(np.tile(np.asarray(inputs["ig_b"][0], np.float32), 32).reshape(128, 1)),
        "fgbn": f(np.tile(np.asarray(inputs["fg_b"][0], np.float32), 32).reshape(128, 1)),
        "hngb": rep(inputs["head_norm_g"][0]),
        "pscale": f(np.asarray(inputs["pool_scale"][0], np.float32).reshape(4, 128).T),
        "w_router": f(inputs["w_router"][0]), "brb": rep(inputs["b_router"][0]),
        "w_gate": f(inputs["w_gate"][0]), "w_up": f(inputs["w_up"][0]), "w_down": f(inputs["w_down"][0]),
        "bgT": f(np.asarray(inputs["b_gate"][0], np.float32).reshape(NE, 8, 128).transpose(2, 0, 1)),
        "buT": f(np.asarray(inputs["b_up"][0], np.float32).reshape(NE, 8, 128).transpose(2, 0, 1)),
        "b_down": f(inputs["b_down"][0]),
        "ident": np.eye(128, dtype=np.float32),
        "maskrl": np.triu(np.ones((128, 128), np.float32)),
        "g2b": rep(inputs["norm2_g"][0]),
        "iotaj": np.ascontiguousarray(np.broadcast_to(np.arange(128, dtype=np.float32)[None, :], (128, 128))),
        "ltstrict": np.triu(np.ones((128, 128), np.float32), 1),
    }
    rm = np.ones((128, 128), np.float32)
    rm[:, 0] = 0.0
    common["resetm"] = rm
    corr_even = np.ones((128, 4, 16), np.float32)
    for g, w in enumerate((2, 4, 8, 16)):
        t = np.arange(16)
        corr_even[:, g, :] = (w / np.minimum(t + 1, w)).astype(np.float32)[None, :]
    maps = []
    for c in range(8):
        b, half = c // 2, c % 2
        m = dict(common)
        m["xo"] = f(x[b, half * TOK:(half + 1) * TOK])
        m["xp"] = f(x[b, 0:TOK]) if half else np.zeros((TOK, D), np.float32)
        m["flag"] = np.full((128, 1), float(half), np.float32)
        m["corr"] = np.ones((128, 4, 16), np.float32) if half else corr_even
        maps.append(m)
    return maps


def kernel(**inputs):
    global _NC
    debug = bool(os.environ.get("KDEBUG"))
    nc = build(debug)
    maps = _prep(inputs)
    res = run_bass_kernel_spmd(nc, maps, core_ids=list(range(8)))
    outp = np.zeros((4, 4096, D), np.float32)
    for c in range(8):
        outp[c // 2, (c % 2) * TOK:(c % 2 + 1) * TOK] = res.results[c]["out"]
    return outp
```
